# Optimizing a Trainium2 kernel written in Bass

```python
import math
import jax, jax.numpy as jnp
from jax import lax
import numpy as np

D_MODEL = 1024
BATCH = 16
SEQ = 4096
DEPTH = 2

GLA_HEADS = 4
GLA_DK = 64
GLA_DV = 128
GLA_GATE_RANK = 16
GLA_GATE_NORM = 16.0
GLA_CHUNK = 64
DSA_HEADS = 8
DSA_DH = 64
IDX_HEADS = 8
IDX_DIM = 64
TOPK_MAX = 256
Q_BLOCK = 128
REL_BUCKETS = 32
REL_MAX_DIST = 128
D_FF = 4 * D_MODEL
PLE_DIM = 256
EPS = 1e-6

GLA_QK = GLA_HEADS * GLA_DK
GLA_V = GLA_HEADS * GLA_DV
DSA_W = DSA_HEADS * DSA_DH
IDX_Q = IDX_HEADS * IDX_DIM
SPLITS = (GLA_QK, GLA_QK, GLA_V, GLA_V, GLA_GATE_RANK, DSA_W, DSA_W, DSA_W,
          IDX_Q, IDX_DIM, IDX_HEADS, D_MODEL, D_MODEL)
N_IN = sum(SPLITS)
SPLIT_POINTS = tuple(int(v) for v in np.cumsum(SPLITS)[:-1])

kernel_name = "hybrid_gla_dsa_griffin_block"


def rms_norm(x, gain):
    xf = x.astype(jnp.float32)
    y = xf * lax.rsqrt(jnp.mean(xf * xf, axis=-1, keepdims=True) + EPS)
    return (y * gain.astype(jnp.float32)).astype(x.dtype)


def rel_bucket(dist):
    max_exact = REL_BUCKETS // 2
    d = jnp.maximum(dist, 1).astype(jnp.float32)
    large = max_exact + (jnp.log(d / max_exact) / math.log(REL_MAX_DIST / max_exact)
                         * (REL_BUCKETS - max_exact)).astype(jnp.int32)
    large = jnp.minimum(large, REL_BUCKETS - 1)
    return jnp.where(dist < max_exact, dist, large)


def gla_chunked(q, k, v, g_log):
    B, S, H, _ = q.shape
    dv = v.shape[-1]
    n = S // GLA_CHUNK

    def to_chunks(a):
        return a.reshape(B, n, GLA_CHUNK, H, a.shape[-1]).transpose(1, 0, 3, 2, 4).astype(jnp.float32)

    causal = jnp.tril(jnp.ones((GLA_CHUNK, GLA_CHUNK), dtype=bool))

    def step(state, inp):
        qb, kb, vb, gb = inp
        b = jnp.cumsum(gb, axis=2)
        inter = jnp.einsum('bhcd,bhde->bhce', qb * jnp.exp(b), state)
        diff = b[:, :, :, None, :] - b[:, :, None, :, :]
        decay = jnp.exp(jnp.where(causal[:, :, None], diff, -jnp.inf))
        attn = jnp.einsum('bhid,bhjd,bhijd->bhij', qb, kb, decay)
        intra = jnp.einsum('bhij,bhje->bhie', attn, vb)
        b_last = b[:, :, -1:, :]
        state = state * jnp.exp(b_last[:, :, 0, :])[..., None] + jnp.einsum(
            'bhcd,bhce->bhde', kb * jnp.exp(b_last - b), vb)
        return state, inter + intra

    state0 = jnp.zeros((B, H, q.shape[-1], dv), jnp.float32)
    _, out = lax.scan(step, state0, (to_chunks(q), to_chunks(k), to_chunks(v), to_chunks(g_log)))
    return out.transpose(1, 0, 3, 2, 4).reshape(B, S, H, dv).astype(q.dtype)


def dsa_attention(q, k, v, qi, ki, wi, rel_bias):
    B, S, H, dh = q.shape
    nb = S // Q_BLOCK
    topk = min(TOPK_MAX, S // 4)
    key_pos = jnp.arange(S, dtype=jnp.int32)
    idx_scale = (IDX_HEADS ** -0.5) * (IDX_DIM ** -0.5)
    ki32 = ki.astype(jnp.float32)

    def blocks(a):
        return a.reshape((B, nb, Q_BLOCK) + a.shape[2:]).swapaxes(0, 1)

    def one_block(inp):
        qb, qib, wib, blk = inp
        qpos = blk * Q_BLOCK + jnp.arange(Q_BLOCK, dtype=jnp.int32)
        visible = key_pos[None, :] <= qpos[:, None]
        rel = jax.nn.relu(jnp.einsum('bqhd,bsd->bqhs', qib.astype(jnp.float32), ki32))
        score = jnp.einsum('bqh,bqhs->bqs', wib.astype(jnp.float32), rel) * idx_scale
        score = jnp.where(visible[None], score, -jnp.inf)
        _, idx = lax.top_k(score, topk)
        k_sel = jax.vmap(lambda kk, ii: kk[ii])(k, idx)
        v_sel = jax.vmap(lambda vv, ii: vv[ii])(v, idx)
        dist = qpos[None, :, None] - idx
        valid = dist >= 0
        bias = rel_bias[rel_bucket(jnp.maximum(dist, 0))].astype(jnp.float32)
        logits = jnp.einsum('bqhd,bqkhd->bqhk', qb.astype(jnp.float32), k_sel.astype(jnp.float32)) \
            * (DSA_DH ** -0.5) + bias.transpose(0, 1, 3, 2)
        logits = jnp.where(valid[:, :, None, :], logits, -jnp.inf)
        probs = jax.nn.softmax(logits, axis=-1)
        out = jnp.einsum('bqhk,bqkhd->bqhd', probs, v_sel.astype(jnp.float32))
        return out.astype(q.dtype)

    outs = lax.map(one_block, (blocks(q), blocks(qi), blocks(wi), jnp.arange(nb, dtype=jnp.int32)))
    return outs.swapaxes(0, 1).reshape(B, S, H, dh)


def setup_inputs(seed: int = 0) -> dict:
    key = jax.random.key(seed)
    ks = jax.random.split(key, 20)
    f32 = jnp.float32

    def w(k, shape, fan_in):
        return jax.random.normal(k, shape, f32) * (fan_in ** -0.5)

    def gain(k, n):
        return 1.0 + 0.05 * jax.random.normal(k, (DEPTH, n), f32)

    return {
        "x": jax.random.normal(ks[0], (BATCH, SEQ, D_MODEL), f32),
        "p": jax.random.normal(ks[1], (DEPTH, BATCH, SEQ, PLE_DIM), f32),
        "rel_bias": 0.5 * jax.random.normal(ks[2], (REL_BUCKETS, DSA_HEADS), f32),
        "ln_mix_pre": gain(ks[3], D_MODEL),
        "ln_mix_post": gain(ks[4], D_MODEL),
        "ln_mlp_pre": gain(ks[5], D_MODEL),
        "ln_mlp_post": gain(ks[6], D_MODEL),
        "ln_ple_post": gain(ks[7], D_MODEL),
        "w_in": w(ks[8], (DEPTH, D_MODEL, N_IN), D_MODEL),
        "gla_gate_w2": w(ks[9], (DEPTH, GLA_GATE_RANK, GLA_QK), GLA_GATE_RANK),
        "gla_gate_b": 0.1 * jax.random.normal(ks[10], (DEPTH, GLA_QK), f32),
        "gla_norm": gain(ks[11], GLA_DV),
        "w_branch_a": w(ks[12], (DEPTH, GLA_V, D_MODEL), GLA_V),
        "w_branch_b": w(ks[13], (DEPTH, DSA_W, D_MODEL), DSA_W),
        "w_out": w(ks[14], (DEPTH, D_MODEL, D_MODEL), D_MODEL),
        "w_mlp_in": w(ks[15], (DEPTH, D_MODEL, D_FF), D_MODEL),
        "w_mlp_out": w(ks[16], (DEPTH, D_FF, D_MODEL), D_FF),
        "w_ple": w(ks[17], (DEPTH, PLE_DIM, D_MODEL), PLE_DIM),
        "w_ple_gate": w(ks[18], (DEPTH, D_MODEL, D_MODEL), D_MODEL),
    }


def reference(x, p, rel_bias, ln_mix_pre, ln_mix_post, ln_mlp_pre, ln_mlp_post, ln_ple_post,
              w_in, gla_gate_w2, gla_gate_b, gla_norm, w_branch_a, w_branch_b, w_out,
              w_mlp_in, w_mlp_out, w_ple, w_ple_gate):
    B, S, _ = x.shape
    h = x
    for i in range(DEPTH):
        u = rms_norm(h, ln_mix_pre[i])
        z = u @ w_in[i]
        (gq, gk, gv, gg, glr, dq, dk, dv, iq, ik, iw, gate_a, gate_b) = jnp.split(
            z, SPLIT_POINTS, axis=-1)

        g_log = jax.nn.log_sigmoid((glr @ gla_gate_w2[i] + gla_gate_b[i]).astype(jnp.float32)) / GLA_GATE_NORM
        o_a = gla_chunked(gq.reshape(B, S, GLA_HEADS, GLA_DK) * (GLA_DK ** -0.5),
                          gk.reshape(B, S, GLA_HEADS, GLA_DK),
                          gv.reshape(B, S, GLA_HEADS, GLA_DV),
                          g_log.reshape(B, S, GLA_HEADS, GLA_DK))
        o_a = rms_norm(o_a, gla_norm[i]) * jax.nn.silu(gg.reshape(B, S, GLA_HEADS, GLA_DV))
        y_a = o_a.reshape(B, S, GLA_V) @ w_branch_a[i]

        o_b = dsa_attention(dq.reshape(B, S, DSA_HEADS, DSA_DH),
                            dk.reshape(B, S, DSA_HEADS, DSA_DH),
                            dv.reshape(B, S, DSA_HEADS, DSA_DH),
                            iq.reshape(B, S, IDX_HEADS, IDX_DIM), ik, iw, rel_bias)
        y_b = o_b.reshape(B, S, DSA_W) @ w_branch_b[i]

        mixed = jax.nn.sigmoid(gate_a) * y_a + jax.nn.sigmoid(gate_b) * y_b
        h = h + rms_norm(mixed @ w_out[i], ln_mix_post[i])

        um = rms_norm(h, ln_mlp_pre[i])
        f = jnp.square(jax.nn.relu(um @ w_mlp_in[i])) @ w_mlp_out[i]
        h = h + rms_norm(f, ln_mlp_post[i])

        e = (p[i] @ w_ple[i]) * jax.nn.sigmoid(h @ w_ple_gate[i])
        h = h + rms_norm(e, ln_ple_post[i])
    return h
```

```python
import numpy as np
from contextlib import ExitStack
import concourse.bass as bass
import concourse.mybir as mybir
from concourse.bass_utils import run_bass_kernel_spmd

F32 = mybir.dt.float32
BF16 = mybir.dt.bfloat16
AF = mybir.ActivationFunctionType
ALU = mybir.AluOpType
AX = mybir.AxisListType

DM = 1024; DEPTH = 2; NIN = 5720; DFF = 4096; PLE = 256
O_GQ, O_GK, O_GV, O_GG, O_GLR, O_DQ, O_DK, O_DV, O_IQ, O_IK, O_IW, O_GA, O_GB = (
    0, 256, 512, 1024, 1536, 1552, 2064, 2576, 3088, 3600, 3664, 3672, 4696)
EPS = 1e-6
NIT = 16
ARENA = 52000
NEG = -30000.0
ACT_BISECT = False
C_ID, C_TRI, C_LNEG, C_UNEG, C_CNEG, C_POW2, C_ONESM, C_ONES = 0, 128, 256, 384, 512, 640, 672, 800
NCONST = 928


class Buf:
    __slots__ = ("name", "w", "r")

    def __init__(self, name):
        self.name = name; self.w = {}; self.r = {}


class Prog:
    ENGS = ("tensor", "scalar", "vector", "gpsimd", "sync")

    def __init__(self, nc, es):
        self.nc = nc; self.es = es
        self.q = {e: [] for e in self.ENGS}
        self.cnt = {e: 0 for e in self.ENGS}
        self.seen = {e: {} for e in self.ENGS}
        self.sem = {}; self.val = {}
        for e in self.ENGS:
            self.sem["E:" + e] = es.enter_context(nc.semaphore("pe_" + e)); self.val["E:" + e] = 0
        self.nb = 0; self.free = []; self.free_sw = []; self.bufsem = {}

    def buf(self, name):
        self.nb += 1
        return Buf(f"{name}_{self.nb}")

    def _waits(self, eng, R, W, is_dma=False):
        me = "E:" + eng
        skip_me = (eng == "tensor") and not is_dma
        need = {}
        for b in R:
            for k, v in b.w.items():
                if k == me and skip_me: continue
                if v > need.get(k, 0): need[k] = v
        for b in W:
            for k, v in b.w.items():
                if k == me and skip_me: continue
                if v > need.get(k, 0): need[k] = v
            for k, v in b.r.items():
                if k == me and skip_me: continue
                if v > need.get(k, 0): need[k] = v
        out = []; seen = self.seen[eng]
        for k, v in need.items():
            if seen.get(k, 0) >= v: continue
            seen[k] = v; out.append((k, v))
        return out

    def _mark(self, k, v, R, W):
        for b in W:
            b.w[k] = v; b.r = {}
        for b in R:
            b.r[k] = v

    def op(self, eng, fn, R=(), W=()):
        waits = self._waits(eng, R, W)
        k = "E:" + eng
        self.val[k] += 1
        self.q[eng].append((waits, fn, k))
        self._mark(k, self.val[k], R, W)

    def dma(self, eng, out, in_, R=(), W=(), sb=None):
        waits = self._waits(eng, R, W, is_dma=True)
        k = self.bufsem.get(sb.name)
        if k is None:
            pool = self.free_sw if eng == "gpsimd" else self.free
            if pool:
                k = pool.pop()
            else:
                k = f"D{'s' if eng == 'gpsimd' else 'h'}:{len(self.sem)}"
                self.sem[k] = self.es.enter_context(self.nc.semaphore(f"dsem{len(self.sem)}")); self.val[k] = 0
            self.bufsem[sb.name] = k
        self.val[k] += 16
        self.q[eng].append((waits, lambda h: h.dma_start(out=out, in_=in_), k))
        self._mark(k, self.val[k], R, W)

    def barrier(self):
        for e in self.ENGS:
            waits = []; seen = self.seen[e]
            for k, v in self.val.items():
                if k == "E:" + e or v == 0 or seen.get(k, 0) >= v: continue
                seen[k] = v; waits.append((k, v))
            self.q[e].append((waits, None, None))
        for k in self.bufsem.values():
            (self.free_sw if k.startswith("Ds") else self.free).append(k)
        self.bufsem = {}

    def replay(self, eng, h):
        for waits, fn, k in self.q[eng]:
            for sk, v in waits:
                h.wait_ge(self.sem[sk], v)
            if fn is None: continue
            ins = fn(h)
            ins.then_inc(self.sem[k], 16 if k[0] == "D" else 1)


class Arena:
    def __init__(self, ap, n):
        self.ap = ap; self.n = n; self.off = 0

    def mark(self): return self.off

    def reset(self, m): self.off = m

    def f32(self, cols, parts=128):
        o = self.off; self.off += cols
        assert self.off <= self.n, f"arena overflow {self.off}"
        return self.ap[0:parts, o:o + cols]

    def bf(self, cols, parts=128):
        n = (cols + 1) // 2
        o = self.off; self.off += n
        assert self.off <= self.n, f"arena overflow {self.off}"
        return self.ap[0:parts, o:o + n].bitcast(BF16)[:, 0:cols]


class Rot:
    def __init__(self, items):
        self.items = items; self.i = 0

    def next(self):
        it = self.items[self.i % len(self.items)]; self.i += 1
        return it


def build(cfg):
    S = cfg["S"]; NSEQ = cfg["NSEQ"]; T = S * NSEQ; TOPK = min(256, S // 4); NB = S // 128
    dbg = cfg.get("dbg", ()); phases = cfg.get("phases", "ABCDEF"); layers = cfg.get("layers", DEPTH)
    nc = bass.Bass("TRN2", target_bir_lowering=False)

    def din(name, shape, dt=F32):
        return nc.dram_tensor(name, list(shape), dt, kind="ExternalInput").ap()

    DB = {}

    def dscr(name, shape, dt):
        kind = "ExternalOutput" if name in dbg else "Internal"
        ap = nc.dram_tensor(name, list(shape), dt, kind=kind).ap()
        return ap

    x_d = din("x", [T, DM]); p_d = din("p", [DEPTH, T, PLE])
    win_d = din("w_in", [DEPTH, DM, NIN]); w2_d = din("gw2", [DEPTH, 16, 256]); gb_d = din("ggb", [DEPTH, 1, 256])
    gn_d = din("gnorm", [DEPTH, 128, 1]); wa_d = din("w_a", [DEPTH, 512, DM]); wb_d = din("w_b", [DEPTH, 512, DM])
    wo_d = din("w_o", [DEPTH, DM, DM]); w1_d = din("w_1", [DEPTH, DM, DFF]); w2m_d = din("w_2", [DEPTH, DFF, DM])
    wple_d = din("w_ple", [DEPTH, PLE, DM]); wpg_d = din("w_pg", [DEPTH, DM, DM])
    gains_d = din("gains", [DEPTH, 5, 128, DM]); cst_d = din("consts", [128, NCONST])
    bias_d = din("biasT", [128, 2048]); cfar_d = din("cfar", [128, 8])
    out_d = nc.dram_tensor("out", [T, DM], F32, kind="ExternalOutput").ap()

    gqT = dscr("gqT", [256, T], F32); gkT = dscr("gkT", [256, T], F32); gk = dscr("gk", [T, 256], F32)
    gv = dscr("gv", [T, 512], BF16); sgT = dscr("sgT", [512, T], BF16); glrT = dscr("glrT", [16, T], F32)
    dqT = dscr("dqT", [512, T], BF16); dkT = dscr("dkT", [512, T], BF16); dvx = dscr("dvx", [T, 520], BF16)
    iqT = dscr("iqT", [512, T], BF16); ikT = dscr("ikT", [64, T], BF16); iw = dscr("iw", [T, 8], F32)
    sgaT = dscr("sgaT", [DM, T], BF16); sgbT = dscr("sgbT", [DM, T], BF16)
    oaT = dscr("oaT", [512, T], BF16); obT = dscr("obT", [512, T], BF16)
    hA = dscr("hA", [T, DM], F32); hB = dscr("hB", [T, DM], F32); hC = dscr("hC", [T, DM], F32)
    SCR = dict(gqT=gqT, gkT=gkT, gk=gk, gv=gv, sgT=sgT, glrT=glrT, dqT=dqT, dkT=dkT, dvx=dvx, iqT=iqT, ikT=ikT,
               iw=iw, sgaT=sgaT, sgbT=sgbT, oaT=oaT, obT=obT, hA=hA, hB=hB, hC=hC)

    with ExitStack() as es:
        arena_t = es.enter_context(nc.sbuf_tensor("arena", [128, ARENA], F32))
        A = Arena(arena_t[:, :], ARENA)
        PS = [es.enter_context(nc.psum_tensor(f"ps{i}", [128, 512], F32)) for i in range(8)]
        P = Prog(nc, es)
        for n_ in list(SCR) + ["x", "p", "out"]:
            DB[n_] = P.buf("dram_" + n_)
        PSB = [P.buf(f"psum{i}") for i in range(8)]
        psf = [PS[i][:, :] for i in range(8)]
        psb = [PS[i][:, :].bitcast(BF16) for i in range(8)]

        def mm(out, lhsT, rhs, start, stop, R, W):
            P.op("tensor", lambda h: h.matmul(out, lhsT=lhsT, rhs=rhs, start=start, stop=stop), R, W)

        def tr(out, in_, ident, R, W):
            P.op("tensor", lambda h: h.transpose(out, in_, ident), R, W)

        def act(out, in_, func, R, W, scale=None, bias=None, accum=None):
            kw = {}
            if scale is not None: kw["scale"] = scale
            if bias is not None: kw["bias"] = bias
            if accum is not None: kw["accum_out"] = accum
            P.op("scalar", lambda h: h.activation(out=out, in_=in_, func=func, **kw), R, W)

        def ts(eng, out, in0, s1, s2, op0, op1, R, W, accum=None):
            kw = {}
            if op1 is not None: kw["op1"] = op1
            if accum is not None: kw["accum_out"] = accum
            P.op(eng, lambda h: h.tensor_scalar(out=out, in0=in0, scalar1=s1, scalar2=s2, op0=op0, **kw), R, W)

        def tt(eng, out, in0, in1, op, R, W):
            P.op(eng, lambda h: h.tensor_tensor(out=out, in0=in0, in1=in1, op=op), R, W)

        def stt(out, in0, scalar, in1, op0, op1, R, W):
            P.op("vector", lambda h: h.scalar_tensor_tensor(out=out, in0=in0, scalar=scalar, in1=in1, op0=op0, op1=op1), R, W)

        def cp(eng, out, in_, R, W):
            if eng == "scalar":
                act(out, in_, AF.Copy, R, W)
            else:
                P.op(eng, lambda h: h.tensor_copy(out=out, in_=in_), R, W)

        def red(out, in_, op, R, W):
            P.op("vector", lambda h: h.tensor_reduce(out=out, in_=in_, axis=AX.X, op=op), R, W)

        def recip(out, in_, R, W):
            P.op("vector", lambda h: h.reciprocal(out=out, in_=in_), R, W)

        def mset(eng, ap, val, W):
            P.op(eng, lambda h: h.memset(ap, val), (), W)

        def dma(out, in_, R, W, sb, eng="sync"):
            P.dma(eng, out, in_, R, W, sb)

        def rot_f32(name, n, cols, parts=128):
            return Rot([(A.f32(cols, parts), P.buf(f"{name}{j}")) for j in range(n)])

        def rot_bf(name, n, cols, parts=128):
            return Rot([(A.bf(cols, parts), P.buf(f"{name}{j}")) for j in range(n)])

        cst = A.f32(NCONST); bC = P.buf("cst")
        dma(cst, cst_d[:, :], (), [bC], bC)
        cfar = A.f32(8); bCF = P.buf("cfar")
        dma(cfar, cfar_d[:, :], (), [bCF], bCF)
        ident_bf = A.bf(128); biasT_bf = A.bf(2048); tri4 = A.f32(512)
        m_tmp = A.mark()
        btmp = A.f32(2048); bBT = P.buf("btmp")
        dma(btmp, bias_d[:, :], (), [bBT], bBT)
        cp("vector", ident_bf, cst[:, C_ID:C_ID + 128], [bC], [bC])
        cp("vector", biasT_bf, btmp, [bBT], [bC])
        for i in range(4):
            cp("vector", tri4[:, i * 128:(i + 1) * 128], cst[:, C_TRI:C_TRI + 128], [bC], [bC])
        A.reset(m_tmp)
        m0 = A.mark()
        ident_f = cst[:, C_ID:C_ID + 128]

        def load_w_bf(dst3, src2, kchunks, ncols, bW):
            for kc in range(kchunks):
                for c0 in range(0, ncols, 2048):
                    c1 = min(ncols, c0 + 2048)
                    dma(dst3[:, kc, c0:c1], src2[kc * 128:(kc + 1) * 128, c0:c1], (), [bW], bW, eng="gpsimd")

        class Norm:
            def __init__(self, nslots=4, njunk=4):
                self.st = rot_f32("nst", nslots, 4)
                self.junks = rot_bf("njunk", njunk, 1024)

            def rstd(self, srcs, R):
                st, bs = self.st.next()
                self.last_st = st
                col = 0
                for s_ap in srcs:
                    n = s_ap.shape[-1]
                    jk, bj = self.junks.next()
                    act(jk[:, 0:n], s_ap, AF.Square, R, [bj, bs], accum=st[:, col:col + 1])
                    col += 1
                if len(srcs) == 2:
                    tt("vector", st[:, 0:1], st[:, 0:1], st[:, 1:2], ALU.add, [bs], [bs])
                act(st[:, 2:3], st[:, 0:1], AF.Sqrt, [bs], [bs], scale=1.0 / DM, bias=EPS)
                recip(st[:, 3:4], st[:, 2:3], [bs], [bs])
                return st[:, 3:4], bs

        def phase_A(l, h_src, b_src):
            P.barrier(); A.reset(m0)
            G = 512; NT = 4
            Wt = A.bf(8 * NIN).rearrange("p (k n) -> p k n", k=8); bW = P.buf("Win")
            load_w_bf(Wt, win_d[l], 8, NIN, bW)
            gbc = A.f32(DM); bG = P.buf("gAin")
            dma(gbc, gains_d[l, 0], (), [bG], bG)
            nrm = Norm()
            hs = Rot([(A.f32(NT * DM).rearrange("p (i d) -> p i d", i=NT), P.buf(f"hA{j}")) for j in range(2)])
            ub = rot_bf("ubf", 2, DM)
            uT = Rot([(A.bf(8 * G).rearrange("p (k t) -> p k t", k=8), P.buf(f"uT{j}")) for j in range(2)])
            stf = rot_f32("stf", 4, 512); stb = rot_bf("stb", 6, 512)
            dvs = Rot([(A.bf(520).rearrange("p (h e) -> p h e", h=8), P.buf(f"dvs{j}")) for j in range(2)])
            for ap_, b_ in dvs.items:
                mset("vector", ap_, 1.0, [b_])
            pt = Rot([(psb[0], PSB[0]), (psb[1], PSB[1])])
            pm = Rot([(psf[i], PSB[i]) for i in range(2, 8)])
            FM = []
            for i in range(2): FM.append((O_GQ + 128 * i, 128, "gqT", 128 * i, "sc_f"))
            for i in range(2): FM.append((O_GK + 128 * i, 128, "gkT", 128 * i, "cp_f"))
            for i in range(4): FM.append((O_GG + 128 * i, 128, "sgT", 128 * i, "silu"))
            FM.append((O_GLR, 16, "glrT", 0, "cp_f"))
            for i in range(4): FM.append((O_DQ + 128 * i, 128, "dqT", 128 * i, "sc_b"))
            for i in range(4): FM.append((O_DK + 128 * i, 128, "dkT", 128 * i, "cp_b"))
            for i in range(4): FM.append((O_IQ + 128 * i, 128, "iqT", 128 * i, "cp_b"))
            FM.append((O_IK, 64, "ikT", 0, "cp_b"))
            for i in range(8): FM.append((O_GA + 128 * i, 128, "sgaT", 128 * i, "sig"))
            for i in range(8): FM.append((O_GB + 128 * i, 128, "sgbT", 128 * i, "sig"))
            ngrp = T // G
            loaded = {}

            def load(g):
                hb, bh = hs.next()
                dma(hb, h_src[g * G:(g + 1) * G, :].rearrange("(i p) d -> p i d", p=128), [b_src], [bh], bh)
                loaded[g] = (hb, bh)

            load(0)
            for g in range(ngrp):
                if g + 1 < ngrp: load(g + 1)
                hb, bh = loaded.pop(g)
                uTt, buT = uT.next()
                for i in range(NT):
                    rs, brs = nrm.rstd([hb[:, i, :]], [bh])
                    rs_dbg = (nrm.last_st, brs)
                    u, bu = ub.next()
                    stt(u, hb[:, i, :], rs, gbc, ALU.mult, ALU.mult, [bh, brs, bG], [bu])
                    ptile, bpt = pt.next()
                    for kc in range(8):
                        tr(ptile[:, kc * 128:(kc + 1) * 128], u[:, kc * 128:(kc + 1) * 128], ident_bf, [bu, bC], [bpt])
                    cp("vector" if i % 2 == 0 else "scalar", uTt[:, :, i * 128:(i + 1) * 128],
                       ptile.rearrange("p (k t) -> p k t", k=8), [bpt], [buT])
                if cfg.get("dumpA") and g == 0:
                    d1 = nc.dram_tensor("dbg_st", [128, 4], F32, kind="ExternalOutput").ap()
                    d2 = nc.dram_tensor("dbg_u", [128, DM], BF16, kind="ExternalOutput").ap()
                    d3 = nc.dram_tensor("dbg_uT", [128, 8 * G], BF16, kind="ExternalOutput").ap()
                    d4 = nc.dram_tensor("dbg_W", [128, 512], BF16, kind="ExternalOutput").ap()
                    d5 = nc.dram_tensor("dbg_g", [128, DM], F32, kind="ExternalOutput").ap()
                    bdd = P.buf("dbgd")
                    dma(d1, rs_dbg[0], [rs_dbg[1]], [bdd], rs_dbg[1])
                    dma(d2, u, [bu], [bdd], bu)
                    dma(d3, uTt.rearrange("p k t -> p (k t)"), [buT], [bdd], buT)
                    dma(d4, Wt[:, 0, 0:512], [bW], [bdd], bW)
                    dma(d5, gbc, [bG], [bdd], bG)
                for (c0, m, dst, r0, kind) in FM:
                    pp, bp = pm.next()
                    for kc in range(8):
                        mm(pp[0:m, :], Wt[:, kc, c0:c0 + m], uTt[:, kc, :], kc == 0, kc == 7, [bW, buT], [bp])
                    if kind in ("sc_f", "cp_f"):
                        sg_, bs_ = stf.next()
                    else:
                        sg_, bs_ = stb.next()
                    if kind in ("sc_f", "sc_b"):
                        ts("vector", sg_[0:m, :], pp[0:m, :], 0.125, None, ALU.mult, None, [bp], [bs_])
                    elif kind in ("cp_f", "cp_b"):
                        cp("vector", sg_[0:m, :], pp[0:m, :], [bp], [bs_])
                    elif kind == "silu":
                        act(sg_[0:m, :], pp[0:m, :], AF.Silu, [bp], [bs_])
                    else:
                        act(sg_[0:m, :], pp[0:m, :], AF.Sigmoid, [bp], [bs_])
                    dma(SCR[dst][r0:r0 + m, g * G:(g + 1) * G], sg_[0:m, :], [bs_], [DB[dst]], bs_)
                for i in range(NT):
                    t0 = g * G + i * 128
                    lh = lambda kc: uTt[:, kc, i * 128:(i + 1) * 128]
                    pp, bp = pm.next()
                    for kc in range(8):
                        mm(pp[:, 0:256], lh(kc), Wt[:, kc, O_GK:O_GK + 256], kc == 0, kc == 7, [bW, buT], [bp])
                    sg_, bs_ = stf.next()
                    cp("vector", sg_[:, 0:256], pp[:, 0:256], [bp], [bs_])
                    dma(gk[t0:t0 + 128, :], sg_[:, 0:256], [bs_], [DB["gk"]], bs_)
                    pp, bp = pm.next()
                    for kc in range(8):
                        mm(pp[:, 0:8], lh(kc), Wt[:, kc, O_IW:O_IW + 8], kc == 0, kc == 7, [bW, buT], [bp])
                    sg_, bs_ = stf.next()
                    cp("vector", sg_[:, 0:8], pp[:, 0:8], [bp], [bs_])
                    dma(iw[t0:t0 + 128, :], sg_[:, 0:8], [bs_], [DB["iw"]], bs_)
                    pp, bp = pm.next()
                    for kc in range(8):
                        mm(pp, lh(kc), Wt[:, kc, O_GV:O_GV + 512], kc == 0, kc == 7, [bW, buT], [bp])
                    sg_, bs_ = stb.next()
                    cp("scalar", sg_, pp, [bp], [bs_])
                    dma(gv[t0:t0 + 128, :], sg_, [bs_], [DB["gv"]], bs_)
                    pp, bp = pm.next()
                    for kc in range(8):
                        mm(pp, lh(kc), Wt[:, kc, O_DV:O_DV + 512], kc == 0, kc == 7, [bW, buT], [bp])
                    dv_, bdv = dvs.next()
                    cp("vector", dv_[:, :, 0:64], pp.rearrange("p (h e) -> p h e", h=8), [bp], [bdv])
                    dma(dvx[t0:t0 + 128, :], dv_.rearrange("p h e -> p (h e)"), [bdv], [DB["dvx"]], bdv)

        def phase_B(l):
            P.barrier(); A.reset(m0)
            w2p = A.f32(256); gno = A.f32(1); bK = P.buf("Bconst")
            mset("vector", w2p, 0.0, [bK])
            dma(w2p[0:16, :], w2_d[l], (), [bK], bK); dma(w2p[32:33, :], gb_d[l], (), [bK], bK); dma(gno, gn_d[l], (), [bK], bK)
            Lneg = cst[:, C_LNEG:C_LNEG + 128]; Uneg = cst[:, C_UNEG:C_UNEG + 128]
            onesm = cst[:, C_ONESM:C_ONESM + 128]

            def padded(name, n, cols, dt_bf, view=None):
                items = []
                for j_ in range(n):
                    ap_ = A.bf(cols) if dt_bf else A.f32(cols)
                    b_ = P.buf(f"{name}{j_}")
                    mset("vector", ap_, 0.0, [b_])
                    items.append((ap_, b_))
                return Rot(items)

            qT2 = padded("qT2", 2, 512, False); kT2 = padded("kT2", 2, 512, False)
            kk = rot_f32("kk", 2, 256); vv = rot_bf("vv", 4, 512)
            lr = padded("lr", 2, 128, False)
            for ap_, b_ in lr.items:
                mset("vector", ap_[32:33, :], 1.0, [b_])
            sg = Rot([(A.bf(512).rearrange("p (c t) -> p c t", c=4), P.buf(f"sgB{j}")) for j in range(4)])
            ee = rot_f32("ee", 2, 256)
            ll = padded("ll", 2, 320, False)
            ebT = rot_f32("ebT", 4, 512); enbT = rot_f32("enbT", 2, 512); ecs = rot_f32("ecs", 2, 256)
            qt = padded("qt", 4, 512, True); kt = padded("kt", 4, 512, True)
            kh = padded("kh", 4, 320, True)
            att = rot_bf("att", 2, 512); sq = rot_f32("sq", 2, 512); sd = rot_f32("sd", 2, 512)
            o1 = rot_f32("o1", 2, 512)
            oas = Rot([(A.bf(512).rearrange("p (c t) -> p c t", c=4), P.buf(f"oas{j}")) for j in range(2)])
            Sst = [(A.f32(512), P.buf(f"S{s}")) for s in range(NSEQ)]
            Sbf = [padded(f"Sb{s}", 2, 512, True) for s in range(NSEQ)]
            cur_sbf = {}
            for s in range(NSEQ):
                mset("vector", Sst[s][0], 0.0, [Sst[s][1]])
                cur_sbf[s] = Sbf[s].next()
            px, ppb, ppc, patt, ppo, ppS, ppm = [(psf[i], PSB[i]) for i in range(7)]

            def stage_a(n, s):
                t0 = s * S + n * 128
                q2, bq2 = qT2.next(); k2, bk2 = kT2.next(); kk_, bkk = kk.next(); vv_, bvv = vv.next()
                lr_, blr = lr.next(); sg_, bsg = sg.next()
                dma(q2[0:64, :].rearrange("p (h t) -> p h t", h=4), gqT.rearrange("(h d) t -> d h t", d=64)[:, :, t0:t0 + 128],
                    [DB["gqT"]], [bq2], bq2)
                dma(k2[0:64, :].rearrange("p (h t) -> p h t", h=4), gkT.rearrange("(h d) t -> d h t", d=64)[:, :, t0:t0 + 128],
                    [DB["gkT"]], [bk2], bk2)
                dma(kk_, gk[t0:t0 + 128, :], [DB["gk"]], [bkk], bkk)
                dma(vv_, gv[t0:t0 + 128, :], [DB["gv"]], [bvv], bvv)
                dma(lr_[0:16, :], glrT[:, t0:t0 + 128], [DB["glrT"]], [blr], blr)
                dma(sg_, sgT.rearrange("(c p) t -> p c t", p=128)[:, :, t0:t0 + 128], [DB["sgT"]], [bsg], bsg)
                mm(px[0][:, 0:256], lr_, w2p, True, True, [blr, bK], [px[1]])
                e_, be = ee.next(); l_, bl = ll.next()
                act(e_, px[0][:, 0:256], AF.Exp, [px[1]], [be], scale=-1.0)
                act(l_[:, 0:256], e_, AF.Ln, [be], [bl], bias=1.0)
                for hh in range(4):
                    mm(ppb[0][:, hh * 128:(hh + 1) * 128], l_[:, hh * 64:hh * 64 + 128], Lneg, True, True, [bl, bC], [ppb[1]])
                mm(ppc[0][:, 0:256], Uneg, l_[:, 0:256], True, True, [bl, bC], [ppc[1]])
                eb, beb = ebT.next(); enb, benb = enbT.next(); ec, bec = ecs.next()
                act(eb[0:64, :], ppb[0][0:64, :], AF.Exp, [ppb[1]], [beb])
                act(enb[0:64, :], ppb[0][0:64, :], AF.Exp, [ppb[1]], [benb], scale=-1.0)
                act(ec, ppc[0][:, 0:256], AF.Exp, [ppc[1]], [bec])
                qt_, bqt = qt.next(); kt_, bkt = kt.next(); kh_, bkh = kh.next()
                tt("vector", qt_[0:64, :], q2[0:64, :], eb[0:64, :], ALU.mult, [bq2, beb], [bqt])
                tt("vector", kt_[0:64, :], k2[0:64, :], enb[0:64, :], ALU.mult, [bk2, benb], [bkt])
                tt("gpsimd", kh_[:, 0:256], kk_, ec, ALU.mult, [bkk, bec], [bkh])
                return (n, s, t0, qt_, bqt, kt_, bkt, kh_, bkh, vv_, bvv, sg_, bsg, eb, beb)

            def stage_b(ctx):
                n, s, t0, qt_, bqt, kt_, bkt, kh_, bkh, vv_, bvv, sg_, bsg, eb, beb = ctx
                for hh in range(4):
                    mm(patt[0][:, hh * 128:(hh + 1) * 128], kt_[:, hh * 128:(hh + 1) * 128], qt_[:, hh * 128:(hh + 1) * 128],
                       True, True, [bkt, bqt], [patt[1]])
                at_, bat = att.next()
                tt("vector", at_, patt[0], tri4, ALU.mult, [patt[1], bC], [bat])
                sb_, bsb = cur_sbf[s]
                for hh in range(4):
                    mm(ppo[0][:, hh * 128:(hh + 1) * 128], vv_[:, hh * 128:(hh + 1) * 128], at_[:, hh * 128:(hh + 1) * 128],
                       True, False, [bvv, bat], [ppo[1]])
                    mm(ppo[0][:, hh * 128:(hh + 1) * 128], sb_[:, hh * 128:(hh + 1) * 128], qt_[:, hh * 128:(hh + 1) * 128],
                       False, True, [bsb, bqt], [ppo[1]])
                for hh in range(4):
                    mm(ppS[0][:, hh * 128:(hh + 1) * 128], kh_[:, hh * 64:hh * 64 + 128], vv_[:, hh * 128:(hh + 1) * 128],
                       True, True, [bkh, bvv], [ppS[1]])
                S_, bS = Sst[s]
                for hh in range(4):
                    cs_ = slice(hh * 128, (hh + 1) * 128)
                    stt(S_[0:64, cs_], S_[0:64, cs_], eb[0:64, hh * 128 + 127:hh * 128 + 128], ppS[0][0:64, cs_],
                        ALU.mult, ALU.add, [bS, beb, ppS[1]], [bS])
                sb_, bsb = Sbf[s].next()
                cp("gpsimd", sb_[0:64, :], S_[0:64, :], [bS], [bsb])
                cur_sbf[s] = (sb_, bsb)
                sq_, bsq = sq.next(); sd_, bsd = sd.next(); o1_, bo1 = o1.next(); oa_, boa = oas.next()
                act(sq_, ppo[0], AF.Square, [ppo[1]], [bsq])
                mm(ppm[0], onesm, sq_, True, True, [bC, bsq], [ppm[1]])
                act(sd_, ppm[0], AF.Sqrt, [ppm[1]], [bsd], bias=EPS)
                recip(sd_, sd_, [bsd], [bsd])
                stt(o1_, ppo[0], gno[:, 0:1], sd_, ALU.mult, ALU.mult, [ppo[1], bK, bsd], [bo1])
                tt("gpsimd", oa_.rearrange("p c t -> p (c t)"), o1_, sg_.rearrange("p c t -> p (c t)"), ALU.mult,
                   [bo1, bsg], [boa])
                dma(oaT.rearrange("(c p) t -> p c t", p=128)[:, :, t0:t0 + 128], oa_, [boa], [DB["oaT"]], boa)

            chunks = [(n, s) for n in range(NB) for s in range(NSEQ)]
            pend = [stage_a(*chunks[0])]
            if len(chunks) > 1: pend.append(stage_a(*chunks[1]))
            for ci in range(len(chunks)):
                if ci + 2 < len(chunks): pend.append(stage_a(*chunks[ci + 2]))
                stage_b(pend.pop(0))

        def phase_C(l):
            P.barrier(); A.reset(m0)
            dk_s = A.bf(8 * S).rearrange("p (c t) -> p c t", c=8); bdk = P.buf("dk_s")
            dv_s = A.bf(NB * 520).rearrange("p (k e) -> p k e", k=NB); bdv = P.buf("dv_s")
            ik2 = A.bf(S); bik = P.buf("ik2")
            mset("vector", dk_s[64:128], 0.0, [bdk]); mset("vector", ik2[64:128, :], 0.0, [bik])
            acc = A.f32(S); bacc = P.buf("acc")
            junk = A.bf(S); bjunk = P.buf("junkC")
            madd = rot_bf("madd", 2, S)
            rr = rot_f32("rr", 3, 512); PT = rot_bf("PT", 3, 512)
            iqb = Rot([(A.bf(1024).rearrange("p (c t) -> p c t", c=8), P.buf(f"iqb{j}")) for j in range(2)])
            dqb = Rot([(A.bf(1024).rearrange("p (c t) -> p c t", c=8), P.buf(f"dqb{j}")) for j in range(3)])
            for ap_, b_ in iqb.items + dqb.items:
                mset("vector", ap_[64:128], 0.0, [b_])
            iwb = rot_f32("iwb", 2, 8)
            ob = rot_bf("ob", 2, 512)
            obs = Rot([(A.bf(512).rearrange("p (c t) -> p c t", c=4), P.buf(f"obs{j}")) for j in range(2)])
            stat = rot_f32("statC", 2, 8 + 2 * NIT)
            recs = rot_f32("recC", 2, 8)
            statA = rot_f32("statA", 2, 2 * NIT + 4)
            junkA = A.bf(S); bjunkA = P.buf("junkA")
            blk_count = [0]
            cneg = cst[:, C_CNEG:C_CNEG + 128]; pow2 = cst[:, C_POW2:C_POW2 + NIT]
            pi = Rot([(psf[0], PSB[0]), (psf[1], PSB[1])])
            pl = Rot([(psf[2], PSB[2]), (psf[3], PSB[3]), (psf[4], PSB[4])])
            po = [(psf[5], PSB[5]), (psf[6], PSB[6])]
            ptr = (psb[7], PSB[7])
            cur_seq = {"s1": -1, "s2": -1}

            def stage1(s, j):
                s0 = s * S
                if cur_seq["s1"] != s:
                    cur_seq["s1"] = s
                    dma(ik2[0:64, :], ikT[:, s0:s0 + S], [DB["ikT"]], [bik], bik)
                t0 = s0 + j * 128; nk = j + 1; NK = nk * 128
                iq_, biq = iqb.next(); dq_, bdq = dqb.next(); iw_, biw = iwb.next()
                dma(iq_[0:64], iqT.rearrange("(h d) t -> d h t", d=64)[:, :, t0:t0 + 128], [DB["iqT"]], [biq], biq)
                dma(dq_[0:64], dqT.rearrange("(h d) t -> d h t", d=64)[:, :, t0:t0 + 128], [DB["dqT"]], [bdq], bdq)
                dma(iw_, iw[t0:t0 + 128, :], [DB["iw"]], [biw], biw)
                for hh in range(8):
                    for c4 in range(0, NK, 512):
                        w = min(512, NK - c4)
                        pp, bp = pi.next()
                        mm(pp[:, 0:w], iq_[:, hh, :], ik2[:, c4:c4 + w], True, True, [biq, bik], [bp])
                        r_, br = rr.next()
                        act(r_[:, 0:w], pp[:, 0:w], AF.Relu, [bp], [br])
                        if hh == 0:
                            ts("vector", acc[:, c4:c4 + w], r_[:, 0:w], iw_[:, 0:1], None, ALU.mult, None, [br, biw], [bacc])
                        else:
                            stt(acc[:, c4:c4 + w], r_[:, 0:w], iw_[:, hh:hh + 1], acc[:, c4:c4 + w], ALU.mult, ALU.add,
                                [br, biw, bacc], [bacc])
                st, bst = stat.next()
                lo = st[:, 0:1]; hi = st[:, 1:2]; w0 = st[:, 2:3]; mid = st[:, 3:4]; cnt = st[:, 4:5]; step = st[:, 5:6]
                wtab = st[:, 8:8 + NIT]; twt = st[:, 8 + NIT:8 + 2 * NIT]
                red(hi, acc[:, 0:NK], ALU.max, [bacc], [bst])
                red(lo, acc[:, 0:NK], ALU.min, [bacc], [bst])
                tt("vector", w0, hi, lo, ALU.subtract, [bst], [bst])
                ts("vector", wtab, pow2, w0, None, ALU.mult, None, [bC, bst], [bst])
                tt("vector", acc[:, j * 128:(j + 1) * 128], acc[:, j * 128:(j + 1) * 128], cneg, ALU.add, [bacc, bC], [bacc])
                ma, bma = madd.next()
                rc, brc = recs.next()
                deferred = []
                use_act = ACT_BISECT and (blk_count[0] % 2 == 1) and nk >= 3
                blk_count[0] += 1
                if not use_act:
                    if NK > TOPK:
                        ts("vector", twt, wtab, 2.0, None, ALU.mult, None, [bst], [bst])
                        tt("vector", mid, lo, wtab[:, 0:1], ALU.add, [bst], [bst])
                        for k in range(NIT):
                            ts("vector", junk[:, 0:NK], acc[:, 0:NK], mid, None, ALU.is_ge, ALU.add, [bacc, bst], [bjunk, bst], accum=cnt)
                            if k < NIT - 1:
                                stt(step, cnt, TOPK - 0.5, twt[:, k + 1:k + 2], ALU.is_ge, ALU.mult, [bst], [bst])
                                stt(mid, step, wtab[:, k + 1:k + 2], mid, ALU.subtract, ALU.add, [bst], [bst])
                            else:
                                stt(step, cnt, TOPK - 0.5, wtab[:, k:k + 1], ALU.is_ge, ALU.mult, [bst], [bst])
                                stt(lo, mid, wtab[:, k:k + 1], step, ALU.subtract, ALU.add, [bst], [bst])
                    ts("vector", ma[:, 0:NK], acc[:, 0:NK], lo, NEG, ALU.is_lt, ALU.mult, [bacc, bst], [bma])
                else:
                    sa, bsa = statA.next()
                    nwt = sa[:, 0:NIT]; hwt = sa[:, NIT:2 * NIT]; nmid = sa[:, 2 * NIT:2 * NIT + 1]
                    sgs = sa[:, 2 * NIT + 1:2 * NIT + 2]; sfl = sa[:, 2 * NIT + 2:2 * NIT + 3]; stp = sa[:, 2 * NIT + 3:2 * NIT + 4]
                    ts("vector", nwt, wtab, -1.0, None, ALU.mult, None, [bst], [bsa])
                    ts("vector", hwt, wtab, 0.5, None, ALU.mult, None, [bst], [bsa])

                    def it(k, lo=lo, NK=NK, nwt=nwt, hwt=hwt, nmid=nmid, sgs=sgs, sfl=sfl, stp=stp, bst=bst, bsa=bsa):
                        act(nmid, lo, AF.Identity, [bst, bsa], [bsa], scale=-1.0, bias=nwt[:, k:k + 1])
                        act(junkA[:, 0:NK], acc[:, 0:NK], AF.Sign, [bacc, bsa], [bjunkA, bsa], bias=nmid, accum=sgs)
                        act(sfl, sgs, AF.Sign, [bsa], [bsa], bias=float(NK - 2 * TOPK + 1))
                        act(stp, sfl, AF.Identity, [bsa], [bsa], scale=hwt[:, k:k + 1], bias=hwt[:, k:k + 1])
                        act(lo, lo, AF.Identity, [bst, bsa], [bst], bias=stp)
                    for k in range(NIT):
                        deferred.append(lambda k=k: it(k))
                    deferred.append(lambda lo=lo, NK=NK, ma=ma, bma=bma, bst=bst:
                                    ts("vector", ma[:, 0:NK], acc[:, 0:NK], lo, NEG, ALU.is_lt, ALU.mult, [bacc, bst], [bma]))
                return (s, j, dq_, bdq, ma, bma, rc, brc, deferred)

            def stage2(ctx, inter):
                s, j, dq_, bdq, ma, bma, rec, bst, _d = ctx
                per_head = (len(inter) + 7) // 8
                s0 = s * S; t0 = s0 + j * 128; nk = j + 1
                if cur_seq["s2"] != s:
                    cur_seq["s2"] = s
                    dma(dk_s[0:64], dkT.rearrange("(h d) t -> d h t", d=64)[:, :, s0:s0 + S], [DB["dkT"]], [bdk], bdk)
                    dma(dv_s, dvx[s0:s0 + S, :].rearrange("(k p) e -> p k e", p=128), [DB["dvx"]], [bdv], bdv)
                for hh in range(8):
                    pob, bpo = po[hh // 4]
                    pcol = (hh % 4) * 65
                    for c4 in range(0, nk, 4):
                        kbs = list(range(c4, min(c4 + 4, nk)))
                        pp, bp = pl.next()
                        for idx, kb in enumerate(kbs):
                            sl = pp[:, idx * 128:(idx + 1) * 128]
                            near = kb >= j - 1
                            mm(sl, dk_s[:, hh, kb * 128:(kb + 1) * 128], dq_[:, hh, :], True, False, [bdk, bdq], [bp])
                            mm(sl, ma[:, kb * 128:(kb + 1) * 128], ident_bf, False, not near, [bma, bC], [bp])
                            if near:
                                bi = hh * 2 + (0 if kb == j else 1)
                                mm(sl, ident_bf, biasT_bf[:, bi * 128:(bi + 1) * 128], False, True, [bC], [bp])
                        nfar = sum(1 for kb in kbs if kb < j - 1)
                        pt_, bpt = PT.next()
                        if nfar > 0:
                            act(pt_[:, 0:nfar * 128], pp[:, 0:nfar * 128], AF.Exp, [bp, bCF], [bpt], bias=cfar[:, hh:hh + 1])
                        if nfar < len(kbs):
                            act(pt_[:, nfar * 128:len(kbs) * 128], pp[:, nfar * 128:len(kbs) * 128], AF.Exp, [bp], [bpt])
                        for idx, kb in enumerate(kbs):
                            mm(pob[:, pcol:pcol + 65], pt_[:, idx * 128:(idx + 1) * 128], dv_s[:, kb, hh * 65:(hh + 1) * 65],
                               kb == 0, kb == j, [bpt, bdv], [bpo])
                    for _ in range(per_head):
                        if inter: inter.pop(0)()
                while inter: inter.pop(0)()
                for half in range(2):
                    pob, bpo = po[half]
                    recip(rec[:, half * 4:(half + 1) * 4].rearrange("p (h o) -> p h o", o=1),
                          pob[:, 0:260].rearrange("p (h e) -> p h e", e=65)[:, :, 64:65], [bpo], [bst])
                ob_, bob = ob.next()
                for hh in range(8):
                    pob, bpo = po[hh // 4]
                    pcol = (hh % 4) * 65
                    if hh % 2 == 0:
                        ts("vector", ob_[:, hh * 64:(hh + 1) * 64], pob[:, pcol:pcol + 64], rec[:, hh:hh + 1], None, ALU.mult, None,
                           [bpo, bst], [bob])
                    else:
                        act(ob_[:, hh * 64:(hh + 1) * 64], pob[:, pcol:pcol + 64], AF.Copy, [bpo, bst], [bob], scale=rec[:, hh:hh + 1])
                for c in range(4):
                    tr(ptr[0][:, c * 128:(c + 1) * 128], ob_[:, c * 128:(c + 1) * 128], ident_bf, [bob, bC], [ptr[1]])
                os_, bos = obs.next()
                cp("scalar", os_, ptr[0][:, 0:512].rearrange("p (c t) -> p c t", c=4), [ptr[1]], [bos])
                dma(obT.rearrange("(c p) t -> p c t", p=128)[:, :, t0:t0 + 128], os_, [bos], [DB["obT"]], bos)

            blocks = [(s, j) for s in range(NSEQ) for j in range(NB)]
            pend = stage1(*blocks[0])
            while pend[-1]: pend[-1].pop(0)()
            for bi_ in range(len(blocks)):
                nxt = stage1(*blocks[bi_ + 1]) if bi_ + 1 < len(blocks) else None
                stage2(pend, nxt[-1] if nxt is not None else [])
                pend = nxt

        def phase_D(l, h_src, b_src, h_dst, b_dst):
            P.barrier(); A.reset(m0)
            G = 512; NT = 4
            Wa = A.bf(4 * DM).rearrange("p (k n) -> p k n", k=4); Wb = A.bf(4 * DM).rearrange("p (k n) -> p k n", k=4)
            Wo = A.bf(8 * DM).rearrange("p (k n) -> p k n", k=8); bW = P.buf("WD")
            load_w_bf(Wa, wa_d[l], 4, DM, bW); load_w_bf(Wb, wb_d[l], 4, DM, bW); load_w_bf(Wo, wo_d[l], 8, DM, bW)
            gbc = A.f32(DM); bG = P.buf("gD")
            dma(gbc, gains_d[l, 1], (), [bG], bG)
            nrm = Norm()
            oa = Rot([(A.bf(4 * G).rearrange("p (c t) -> p c t", c=4), P.buf(f"oaD{j}")) for j in range(2)])
            obb = Rot([(A.bf(4 * G).rearrange("p (c t) -> p c t", c=4), P.buf(f"obD{j}")) for j in range(2)])
            sa = Rot([(A.bf(8 * G).rearrange("p (c t) -> p c t", c=8), P.buf(f"saD{j}")) for j in range(2)])
            sbb = Rot([(A.bf(8 * G).rearrange("p (c t) -> p c t", c=8), P.buf(f"sbD{j}")) for j in range(2)])
            hs = Rot([(A.f32(NT * DM).rearrange("p (i d) -> p i d", i=NT), P.buf(f"hD{j}")) for j in range(2)])
            m1 = rot_f32("m1", 2, G); m2 = rot_f32("m2", 2, G)
            mxT = Rot([(A.bf(8 * G).rearrange("p (k t) -> p k t", k=8), P.buf(f"mxT{j}")) for j in range(2)])
            tmp = rot_f32("tmpD", 2, DM)
            pm = Rot([(psf[i], PSB[i]) for i in range(8)])
            ngrp = T // G
            loaded = {}

            def load(g):
                sl = slice(g * G, (g + 1) * G)
                a_, ba = oa.next(); b_, bb = obb.next(); sa_, bsa = sa.next(); sb_, bsb = sbb.next(); h_, bh = hs.next()
                dma(a_, oaT.rearrange("(c p) t -> p c t", p=128)[:, :, sl], [DB["oaT"]], [ba], ba)
                dma(b_, obT.rearrange("(c p) t -> p c t", p=128)[:, :, sl], [DB["obT"]], [bb], bb)
                dma(sa_, sgaT.rearrange("(c p) t -> p c t", p=128)[:, :, sl], [DB["sgaT"]], [bsa], bsa)
                dma(sb_, sgbT.rearrange("(c p) t -> p c t", p=128)[:, :, sl], [DB["sgbT"]], [bsb], bsb)
                dma(h_, h_src[sl, :].rearrange("(i p) d -> p i d", p=128), [b_src], [bh], bh)
                loaded[g] = (a_, ba, b_, bb, sa_, bsa, sb_, bsb, h_, bh)

            load(0)
            for g in range(ngrp):
                if g + 1 < ngrp: load(g + 1)
                a_, ba, b_, bb, sa_, bsa, sb_, bsb, h_, bh = loaded.pop(g)
                mx, bmx = mxT.next()
                for ncb in range(8):
                    pa, bpa = pm.next(); pb, bpb = pm.next()
                    for kc in range(4):
                        mm(pa, Wa[:, kc, ncb * 128:(ncb + 1) * 128], a_[:, kc, :], kc == 0, kc == 3, [bW, ba], [bpa])
                    for kc in range(4):
                        mm(pb, Wb[:, kc, ncb * 128:(ncb + 1) * 128], b_[:, kc, :], kc == 0, kc == 3, [bW, bb], [bpb])
                    m1_, bm1 = m1.next(); m2_, bm2 = m2.next()
                    tt("vector", m1_, pa, sa_[:, ncb, :], ALU.mult, [bpa, bsa], [bm1])
                    tt("vector", m2_, pb, sb_[:, ncb, :], ALU.mult, [bpb, bsb], [bm2])
                    tt("gpsimd", mx[:, ncb, :], m1_, m2_, ALU.add, [bm1, bm2], [bmx])
                for i in range(NT):
                    t0 = g * G + i * 128
                    halves = []
                    for half in range(2):
                        pp, bp = pm.next()
                        for kc in range(8):
                            mm(pp, mx[:, kc, i * 128:(i + 1) * 128], Wo[:, kc, half * 512:(half + 1) * 512], kc == 0, kc == 7,
                               [bmx, bW], [bp])
                        halves.append((pp, bp))
                    rs, brs = nrm.rstd([halves[0][0], halves[1][0]], [halves[0][1], halves[1][1]])
                    tm, btm = tmp.next()
                    for half in range(2):
                        stt(tm[:, half * 512:(half + 1) * 512], halves[half][0], rs, gbc[:, half * 512:(half + 1) * 512],
                            ALU.mult, ALU.mult, [halves[half][1], brs, bG], [btm])
                    tt("gpsimd", h_[:, i, :], h_[:, i, :], tm, ALU.add, [bh, btm], [bh])
                    dma(h_dst[t0:t0 + 128, :], h_[:, i, :], [bh], [b_dst], bh)

        def phase_E(l, h_src, b_src, h_dst, b_dst):
            P.barrier(); A.reset(m0)
            G = 256; NT = 2
            W1 = A.bf(8 * DFF).rearrange("p (k n) -> p k n", k=8); W2 = A.bf(32 * DM).rearrange("p (k n) -> p k n", k=32)
            bW = P.buf("WE")
            load_w_bf(W1, w1_d[l], 8, DFF, bW); load_w_bf(W2, w2m_d[l], 32, DM, bW)
            g1 = A.f32(DM); g2 = A.f32(DM); bG = P.buf("gE")
            dma(g1, gains_d[l, 2], (), [bG], bG); dma(g2, gains_d[l, 3], (), [bG], bG)
            nrm = Norm(njunk=2)
            hs = Rot([(A.f32(NT * DM).rearrange("p (i d) -> p i d", i=NT), P.buf(f"hE{j}")) for j in range(2)])
            ub = rot_bf("ubE", 2, DM)
            uT = Rot([(A.bf(8 * G).rearrange("p (k t) -> p k t", k=8), P.buf(f"uTE{j}")) for j in range(2)])
            a1 = Rot([(A.bf(32 * G).rearrange("p (k t) -> p k t", k=32), P.buf(f"a1E{j}")) for j in range(1)])
            rl = rot_f32("rlE", 3, G)
            tmp = rot_f32("tmpE", 1, DM)
            pt = Rot([(psb[0], PSB[0]), (psb[1], PSB[1])])
            pm = Rot([(psf[i], PSB[i]) for i in range(2, 8)])
            ngrp = T // G
            loaded = {}

            def load(g):
                h_, bh = hs.next()
                dma(h_, h_src[g * G:(g + 1) * G, :].rearrange("(i p) d -> p i d", p=128), [b_src], [bh], bh)
                loaded[g] = (h_, bh)

            load(0)
            for g in range(ngrp):
                if g + 1 < ngrp: load(g + 1)
                h_, bh = loaded.pop(g)
                uTt, buT = uT.next()
                for i in range(NT):
                    rs, brs = nrm.rstd([h_[:, i, :]], [bh])
                    u, bu = ub.next()
                    stt(u, h_[:, i, :], rs, g1, ALU.mult, ALU.mult, [bh, brs, bG], [bu])
                    ptile, bpt = pt.next()
                    for kc in range(8):
                        tr(ptile[:, kc * 128:(kc + 1) * 128], u[:, kc * 128:(kc + 1) * 128], ident_bf, [bu, bC], [bpt])
                    cp("vector" if i % 2 == 0 else "scalar", uTt[:, :, i * 128:(i + 1) * 128],
                       ptile.rearrange("p (k t) -> p k t", k=8), [bpt], [buT])
                a1_, ba1 = a1.next()
                for fc in range(32):
                    pp, bp = pm.next()
                    for kc in range(8):
                        mm(pp[:, 0:G], W1[:, kc, fc * 128:(fc + 1) * 128], uTt[:, kc, :], kc == 0, kc == 7, [bW, buT], [bp])
                    r_, br = rl.next()
                    act(r_, pp[:, 0:G], AF.Relu, [bp], [br])
                    tt("vector" if fc % 2 == 0 else "gpsimd", a1_[:, fc, :], r_, r_, ALU.mult, [br], [ba1])
                for i in range(NT):
                    t0 = g * G + i * 128
                    halves = []
                    for half in range(2):
                        pp, bp = pm.next()
                        for fc in range(32):
                            mm(pp, a1_[:, fc, i * 128:(i + 1) * 128], W2[:, fc, half * 512:(half + 1) * 512], fc == 0, fc == 31,
                               [ba1, bW], [bp])
                        halves.append((pp, bp))
                    rs, brs = nrm.rstd([halves[0][0], halves[1][0]], [halves[0][1], halves[1][1]])
                    tm, btm = tmp.next()
                    for half in range(2):
                        stt(tm[:, half * 512:(half + 1) * 512], halves[half][0], rs, g2[:, half * 512:(half + 1) * 512],
                            ALU.mult, ALU.mult, [halves[half][1], brs, bG], [btm])
                    tt("gpsimd", h_[:, i, :], h_[:, i, :], tm, ALU.add, [bh, btm], [bh])
                    dma(h_dst[t0:t0 + 128, :], h_[:, i, :], [bh], [b_dst], bh)

        def phase_F(l, h_src, b_src, h_dst, b_dst):
            P.barrier(); A.reset(m0)
            Wp = A.bf(2 * DM).rearrange("p (k n) -> p k n", k=2); Wg = A.bf(8 * DM).rearrange("p (k n) -> p k n", k=8)
            bW = P.buf("WF")
            load_w_bf(Wp, wple_d[l], 2, DM, bW); load_w_bf(Wg, wpg_d[l], 8, DM, bW)
            g5 = A.f32(DM); bG = P.buf("gF")
            dma(g5, gains_d[l, 4], (), [bG], bG)
            nrm = Norm()
            hs = rot_f32("hF", 3, DM); ps_ = rot_f32("pF", 3, PLE)
            hb = rot_bf("hbF", 2, DM); pb = rot_bf("pbF", 2, PLE)
            hT = Rot([(A.bf(8 * 128).rearrange("p (k t) -> p k t", k=8), P.buf(f"hTF{j}")) for j in range(2)])
            pT = Rot([(A.bf(2 * 128).rearrange("p (k t) -> p k t", k=2), P.buf(f"pTF{j}")) for j in range(2)])
            sg = rot_f32("sgF", 2, DM); ee = rot_f32("eF", 2, DM); tmp = rot_f32("tmpF", 2, DM)
            pt = Rot([(psb[0], PSB[0]), (psb[1], PSB[1])])
            pm = Rot([(psf[i], PSB[i]) for i in range(2, 8)])
            ntile = T // 128
            loaded = {}

            def load(i):
                h_, bh = hs.next(); p_, bp = ps_.next()
                dma(h_, h_src[i * 128:(i + 1) * 128, :], [b_src], [bh], bh)
                dma(p_, p_d[l, i * 128:(i + 1) * 128, :], [DB["p"]], [bp], bp)
                loaded[i] = (h_, bh, p_, bp)

            load(0)
            for i in range(ntile):
                if i + 1 < ntile: load(i + 1)
                h_, bh, p_, bpp = loaded.pop(i)
                hb_, bhb = hb.next(); pb_, bpb = pb.next()
                cp("vector", hb_, h_, [bh], [bhb]); cp("gpsimd", pb_, p_, [bpp], [bpb])
                ptile, bpt = pt.next()
                for kc in range(8):
                    tr(ptile[:, kc * 128:(kc + 1) * 128], hb_[:, kc * 128:(kc + 1) * 128], ident_bf, [bhb, bC], [bpt])
                hT_, bhT = hT.next()
                cp("scalar", hT_, ptile.rearrange("p (k t) -> p k t", k=8), [bpt], [bhT])
                ptile, bpt = pt.next()
                for kc in range(2):
                    tr(ptile[:, kc * 128:(kc + 1) * 128], pb_[:, kc * 128:(kc + 1) * 128], ident_bf, [bpb, bC], [bpt])
                pT_, bpT = pT.next()
                cp("vector", pT_, ptile[:, 0:256].rearrange("p (k t) -> p k t", k=2), [bpt], [bpT])
                sg_, bsg = sg.next(); e_, be = ee.next()
                for half in range(2):
                    cs = slice(half * 512, (half + 1) * 512)
                    pg, bpg = pm.next()
                    for kc in range(8):
                        mm(pg, hT_[:, kc, :], Wg[:, kc, cs], kc == 0, kc == 7, [bhT, bW], [bpg])
                    act(sg_[:, cs], pg, AF.Sigmoid, [bpg], [bsg])
                    pe, bpe = pm.next()
                    for kc in range(2):
                        mm(pe, pT_[:, kc, :], Wp[:, kc, cs], kc == 0, kc == 1, [bpT, bW], [bpe])
                    tt("vector", e_[:, cs], pe, sg_[:, cs], ALU.mult, [bpe, bsg], [be])
                rs, brs = nrm.rstd([e_], [be])
                tm, btm = tmp.next()
                stt(tm, e_, rs, g5, ALU.mult, ALU.mult, [be, brs, bG], [btm])
                tt("gpsimd", h_, h_, tm, ALU.add, [bh, btm], [bh])
                dma(h_dst[i * 128:(i + 1) * 128, :], h_, [bh], [b_dst], bh)

        cur, bcur = x_d, DB["x"]
        for l in range(layers):
            last = (l == layers - 1)
            if "A" in phases: phase_A(l, cur, bcur)
            if "B" in phases: phase_B(l)
            if "C" in phases: phase_C(l)
            if "D" in phases: phase_D(l, cur, bcur, hA, DB["hA"])
            if "E" in phases: phase_E(l, hA, DB["hA"], hB, DB["hB"])
            if "F" in phases:
                dst, bdst = (out_d, DB["out"]) if last else (hC, DB["hC"])
                phase_F(l, hB, DB["hB"], dst, bdst)
                cur, bcur = dst, bdst
        P.barrier()

        with nc.Block() as block:
            @block.tensor
            def _(h): P.replay("tensor", h)

            @block.scalar
            def _(h): P.replay("scalar", h)

            @block.vector
            def _(h): P.replay("vector", h)

            @block.gpsimd
            def _(h): P.replay("gpsimd", h)

            @block.sync
            def _(h): P.replay("sync", h)
    return nc


def _bucket_table():
    d = np.arange(256)
    dd = np.maximum(d, 1).astype(np.float32)
    large = 16 + (np.log(dd / np.float32(16)) / np.float32(np.log(8.0)) * np.float32(16)).astype(np.int32)
    large = np.minimum(large, 31)
    return np.where(d < 16, d, large).astype(np.int64)


def make_consts():
    c = np.zeros((128, NCONST), np.float32)
    i = np.arange(128)
    c[:, C_ID:C_ID + 128] = np.eye(128, dtype=np.float32)
    c[:, C_TRI:C_TRI + 128] = (i[:, None] <= i[None, :]).astype(np.float32)
    c[:, C_LNEG:C_LNEG + 128] = (i[:, None] <= i[None, :]).astype(np.float32) * (-1.0 / 16.0)
    c[:, C_UNEG:C_UNEG + 128] = (i[:, None] > i[None, :]).astype(np.float32) * (-1.0 / 16.0)
    c[:, C_CNEG:C_CNEG + 128] = np.where(i[None, :] <= i[:, None], 0.0, -1e30).astype(np.float32)
    c[:, C_POW2:C_POW2 + 32] = (0.5 ** np.arange(1, 33)).astype(np.float32)[None, :]
    c[:, C_ONESM:C_ONESM + 128] = 1.0 / 128.0
    c[:, C_ONES:C_ONES + 128] = 1.0
    return c


def make_bias_tiles(rel_bias):
    bt = _bucket_table()
    s = np.arange(128)[:, None]; t = np.arange(128)[None, :]
    out = np.zeros((128, 2048), np.float32)
    for h in range(8):
        for o in range(2):
            d = t - s + o * 128
            idx = bt[np.clip(d, 0, 255)]
            out[:, (h * 2 + o) * 128:(h * 2 + o + 1) * 128] = rel_bias[idx, h]
    cfar = np.broadcast_to(rel_bias[31, :][None, :], (128, 8)).astype(np.float32).copy()
    return out, cfar


def make_in_maps(inputs, S, NSEQ, ncores):
    f = lambda a: np.ascontiguousarray(np.asarray(a, dtype=np.float32))
    x = f(inputs["x"]); p = f(inputs["p"])
    gains = np.stack([f(inputs[k]) for k in ("ln_mix_pre", "ln_mix_post", "ln_mlp_pre", "ln_mlp_post", "ln_ple_post")], axis=1)
    gains = np.ascontiguousarray(np.broadcast_to(gains[:, :, None, :], (DEPTH, 5, 128, DM)))
    biasT, cfar = make_bias_tiles(f(inputs["rel_bias"]))
    shared = dict(
        w_in=f(inputs["w_in"]), gw2=f(inputs["gla_gate_w2"]), ggb=f(inputs["gla_gate_b"]).reshape(DEPTH, 1, 256),
        gnorm=f(inputs["gla_norm"]).reshape(DEPTH, 128, 1), w_a=f(inputs["w_branch_a"]), w_b=f(inputs["w_branch_b"]),
        w_o=f(inputs["w_out"]), w_1=f(inputs["w_mlp_in"]), w_2=f(inputs["w_mlp_out"]), w_ple=f(inputs["w_ple"]),
        w_pg=f(inputs["w_ple_gate"]), gains=gains, consts=make_consts(), biasT=biasT, cfar=cfar)
    maps = []
    for c in range(ncores):
        m = dict(shared)
        m["x"] = np.ascontiguousarray(x[c * NSEQ:(c + 1) * NSEQ, :S].reshape(NSEQ * S, DM))
        m["p"] = np.ascontiguousarray(p[:, c * NSEQ:(c + 1) * NSEQ, :S].reshape(DEPTH, NSEQ * S, PLE))
        maps.append(m)
    return maps


_NC_CACHE = {}


def kernel(**inputs):
    B, S, _ = inputs["x"].shape
    ncores = 8; NSEQ = B // ncores
    key = (S, NSEQ)
    if key not in _NC_CACHE:
        _NC_CACHE[key] = build(dict(S=S, NSEQ=NSEQ))
    nc = _NC_CACHE[key]
    maps = make_in_maps(inputs, S, NSEQ, ncores)
    res = run_bass_kernel_spmd(nc, maps, core_ids=list(range(ncores)))
    out = np.stack([r["out"].reshape(NSEQ, S, DM) for r in res.results], axis=0).reshape(B, S, DM)
    return out.astype(np.float32)
```

```python
import numpy as np
from contextlib import ExitStack
import concourse.bass as bass
import concourse.mybir as mybir
from concourse.bass_utils import run_bass_kernel_spmd

F32 = mybir.dt.float32
BF16 = mybir.dt.bfloat16
AF = mybir.ActivationFunctionType
ALU = mybir.AluOpType
AX = mybir.AxisListType

DM = 1024; DEPTH = 2; NIN = 5720; DFF = 4096; PLE = 256
O_GQ, O_GK, O_GV, O_GG, O_GLR, O_DQ, O_DK, O_DV, O_IQ, O_IK, O_IW, O_GA, O_GB = (
    0, 256, 512, 1024, 1536, 1552, 2064, 2576, 3088, 3600, 3664, 3672, 4696)
EPS = 1e-6
NIT = 16
ARENA = 52000
NEG = -30000.0
ACT_BISECT = False
C_ID, C_TRI, C_LNEG, C_UNEG, C_CNEG, C_POW2, C_ONESM, C_ONES = 0, 128, 256, 384, 512, 640, 672, 800
NCONST = 928


class Buf:
    __slots__ = ("name", "w", "r")

    def __init__(self, name):
        self.name = name; self.w = {}; self.r = {}


class Prog:
    ENGS = ("tensor", "scalar", "vector", "gpsimd", "sync")

    def __init__(self, nc, es):
        self.nc = nc; self.es = es
        self.q = {e: [] for e in self.ENGS}
        self.cnt = {e: 0 for e in self.ENGS}
        self.seen = {e: {} for e in self.ENGS}
        self.sem = {}; self.val = {}
        for e in self.ENGS:
            self.sem["E:" + e] = es.enter_context(nc.semaphore("pe_" + e)); self.val["E:" + e] = 0
        self.nb = 0; self.free = []; self.free_sw = []; self.bufsem = {}

    def buf(self, name):
        self.nb += 1
        return Buf(f"{name}_{self.nb}")

    def _waits(self, eng, R, W, is_dma=False):
        me = "E:" + eng
        skip_me = (eng == "tensor") and not is_dma
        need = {}
        for b in R:
            for k, v in b.w.items():
                if k == me and skip_me: continue
                if v > need.get(k, 0): need[k] = v
        for b in W:
            for k, v in b.w.items():
                if k == me and skip_me: continue
                if v > need.get(k, 0): need[k] = v
            for k, v in b.r.items():
                if k == me and skip_me: continue
                if v > need.get(k, 0): need[k] = v
        out = []; seen = self.seen[eng]
        for k, v in need.items():
            if seen.get(k, 0) >= v: continue
            seen[k] = v; out.append((k, v))
        return out

    def _mark(self, k, v, R, W):
        for b in W:
            b.w[k] = v; b.r = {}
        for b in R:
            b.r[k] = v

    def op(self, eng, fn, R=(), W=()):
        waits = self._waits(eng, R, W)
        k = "E:" + eng
        self.val[k] += 1
        self.q[eng].append((waits, fn, k))
        self._mark(k, self.val[k], R, W)

    def dma(self, eng, out, in_, R=(), W=(), sb=None):
        waits = self._waits(eng, R, W, is_dma=True)
        k = self.bufsem.get(sb.name)
        if k is None:
            pool = self.free_sw if eng == "gpsimd" else self.free
            if pool:
                k = pool.pop()
            else:
                k = f"D{'s' if eng == 'gpsimd' else 'h'}:{len(self.sem)}"
                self.sem[k] = self.es.enter_context(self.nc.semaphore(f"dsem{len(self.sem)}")); self.val[k] = 0
            self.bufsem[sb.name] = k
        self.val[k] += 16
        self.q[eng].append((waits, lambda h: h.dma_start(out=out, in_=in_), k))
        self._mark(k, self.val[k], R, W)

    def barrier(self):
        for e in self.ENGS:
            waits = []; seen = self.seen[e]
            for k, v in self.val.items():
                if k == "E:" + e or v == 0 or seen.get(k, 0) >= v: continue
                seen[k] = v; waits.append((k, v))
            self.q[e].append((waits, None, None))
        for k in self.bufsem.values():
            (self.free_sw if k.startswith("Ds") else self.free).append(k)
        self.bufsem = {}

    def replay(self, eng, h):
        for waits, fn, k in self.q[eng]:
            for sk, v in waits:
                h.wait_ge(self.sem[sk], v)
            if fn is None: continue
            ins = fn(h)
            ins.then_inc(self.sem[k], 16 if k[0] == "D" else 1)


class Arena:
    def __init__(self, ap, n):
        self.ap = ap; self.n = n; self.off = 0

    def mark(self): return self.off

    def reset(self, m): self.off = m

    def f32(self, cols, parts=128):
        o = self.off; self.off += cols
        assert self.off <= self.n, f"arena overflow {self.off}"
        return self.ap[0:parts, o:o + cols]

    def bf(self, cols, parts=128):
        n = (cols + 1) // 2
        o = self.off; self.off += n
        assert self.off <= self.n, f"arena overflow {self.off}"
        return self.ap[0:parts, o:o + n].bitcast(BF16)[:, 0:cols]


class Rot:
    def __init__(self, items):
        self.items = items; self.i = 0

    def next(self):
        it = self.items[self.i % len(self.items)]; self.i += 1
        return it


def build(cfg):
    S = cfg["S"]; NSEQ = cfg["NSEQ"]; T = S * NSEQ; TOPK = min(256, S // 4); NB = S // 128
    dbg = cfg.get("dbg", ()); phases = cfg.get("phases", "ABCDEF"); layers = cfg.get("layers", DEPTH)
    nc = bass.Bass("TRN2", target_bir_lowering=False)

    def din(name, shape, dt=F32):
        return nc.dram_tensor(name, list(shape), dt, kind="ExternalInput").ap()

    DB = {}

    def dscr(name, shape, dt):
        kind = "ExternalOutput" if name in dbg else "Internal"
        ap = nc.dram_tensor(name, list(shape), dt, kind=kind).ap()
        return ap

    x_d = din("x", [T, DM]); p_d = din("p", [DEPTH, T, PLE])
    win_d = din("w_in", [DEPTH, DM, NIN]); w2_d = din("gw2", [DEPTH, 16, 256]); gb_d = din("ggb", [DEPTH, 1, 256])
    gn_d = din("gnorm", [DEPTH, 128, 1]); wa_d = din("w_a", [DEPTH, 512, DM]); wb_d = din("w_b", [DEPTH, 512, DM])
    wo_d = din("w_o", [DEPTH, DM, DM]); w1_d = din("w_1", [DEPTH, DM, DFF]); w2m_d = din("w_2", [DEPTH, DFF, DM])
    wple_d = din("w_ple", [DEPTH, PLE, DM]); wpg_d = din("w_pg", [DEPTH, DM, DM])
    gains_d = din("gains", [DEPTH, 5, 128, DM]); cst_d = din("consts", [128, NCONST])
    bias_d = din("biasT", [128, 2048]); cfar_d = din("cfar", [128, 8])
    out_d = nc.dram_tensor("out", [T, DM], F32, kind="ExternalOutput").ap()

    gqT = dscr("gqT", [256, T], F32); gkT = dscr("gkT", [256, T], F32); gk = dscr("gk", [T, 256], F32)
    gv = dscr("gv", [T, 512], BF16); sgT = dscr("sgT", [512, T], BF16); glrT = dscr("glrT", [16, T], F32)
    dqT = dscr("dqT", [512, T], BF16); dkT = dscr("dkT", [512, T], BF16); dvx = dscr("dvx", [T, 520], BF16)
    iqT = dscr("iqT", [512, T], BF16); ikT = dscr("ikT", [64, T], BF16); iw = dscr("iw", [T, 8], F32)
    sgaT = dscr("sgaT", [DM, T], BF16); sgbT = dscr("sgbT", [DM, T], BF16)
    oaT = dscr("oaT", [512, T], BF16); obT = dscr("obT", [512, T], BF16)
    hA = dscr("hA", [T, DM], F32); hB = dscr("hB", [T, DM], F32); hC = dscr("hC", [T, DM], F32)
    SCR = dict(gqT=gqT, gkT=gkT, gk=gk, gv=gv, sgT=sgT, glrT=glrT, dqT=dqT, dkT=dkT, dvx=dvx, iqT=iqT, ikT=ikT,
               iw=iw, sgaT=sgaT, sgbT=sgbT, oaT=oaT, obT=obT, hA=hA, hB=hB, hC=hC)

    with ExitStack() as es:
        arena_t = es.enter_context(nc.sbuf_tensor("arena", [128, ARENA], F32))
        A = Arena(arena_t[:, :], ARENA)
        PS = [es.enter_context(nc.psum_tensor(f"ps{i}", [128, 512], F32)) for i in range(8)]
        P = Prog(nc, es)
        for n_ in list(SCR) + ["x", "p", "out"]:
            DB[n_] = P.buf("dram_" + n_)
        PSB = [P.buf(f"psum{i}") for i in range(8)]
        psf = [PS[i][:, :] for i in range(8)]
        psb = [PS[i][:, :].bitcast(BF16) for i in range(8)]

        def mm(out, lhsT, rhs, start, stop, R, W):
            P.op("tensor", lambda h: h.matmul(out, lhsT=lhsT, rhs=rhs, start=start, stop=stop), R, W)

        def tr(out, in_, ident, R, W):
            P.op("tensor", lambda h: h.transpose(out, in_, ident), R, W)

        def act(out, in_, func, R, W, scale=None, bias=None, accum=None):
            kw = {}
            if scale is not None: kw["scale"] = scale
            if bias is not None: kw["bias"] = bias
            if accum is not None: kw["accum_out"] = accum
            P.op("scalar", lambda h: h.activation(out=out, in_=in_, func=func, **kw), R, W)

        def ts(eng, out, in0, s1, s2, op0, op1, R, W, accum=None):
            kw = {}
            if op1 is not None: kw["op1"] = op1
            if accum is not None: kw["accum_out"] = accum
            P.op(eng, lambda h: h.tensor_scalar(out=out, in0=in0, scalar1=s1, scalar2=s2, op0=op0, **kw), R, W)

        def tt(eng, out, in0, in1, op, R, W):
            P.op(eng, lambda h: h.tensor_tensor(out=out, in0=in0, in1=in1, op=op), R, W)

        def stt(out, in0, scalar, in1, op0, op1, R, W):
            P.op("vector", lambda h: h.scalar_tensor_tensor(out=out, in0=in0, scalar=scalar, in1=in1, op0=op0, op1=op1), R, W)

        def cp(eng, out, in_, R, W):
            if eng == "scalar":
                act(out, in_, AF.Copy, R, W)
            else:
                P.op(eng, lambda h: h.tensor_copy(out=out, in_=in_), R, W)

        def red(out, in_, op, R, W):
            P.op("vector", lambda h: h.tensor_reduce(out=out, in_=in_, axis=AX.X, op=op), R, W)

        def recip(out, in_, R, W):
            P.op("vector", lambda h: h.reciprocal(out=out, in_=in_), R, W)

        def mset(eng, ap, val, W):
            P.op(eng, lambda h: h.memset(ap, val), (), W)

        def dma(out, in_, R, W, sb, eng="sync"):
            P.dma(eng, out, in_, R, W, sb)

        def rot_f32(name, n, cols, parts=128):
            return Rot([(A.f32(cols, parts), P.buf(f"{name}{j}")) for j in range(n)])

        def rot_bf(name, n, cols, parts=128):
            return Rot([(A.bf(cols, parts), P.buf(f"{name}{j}")) for j in range(n)])

        cst = A.f32(NCONST); bC = P.buf("cst")
        dma(cst, cst_d[:, :], (), [bC], bC)
        cfar = A.f32(8); bCF = P.buf("cfar")
        dma(cfar, cfar_d[:, :], (), [bCF], bCF)
        ident_bf = A.bf(128); biasT_bf = A.bf(2048); tri4 = A.f32(512)
        m_tmp = A.mark()
        btmp = A.f32(2048); bBT = P.buf("btmp")
        dma(btmp, bias_d[:, :], (), [bBT], bBT)
        cp("vector", ident_bf, cst[:, C_ID:C_ID + 128], [bC], [bC])
        cp("vector", biasT_bf, btmp, [bBT], [bC])
        for i in range(4):
            cp("vector", tri4[:, i * 128:(i + 1) * 128], cst[:, C_TRI:C_TRI + 128], [bC], [bC])
        A.reset(m_tmp)
        m0 = A.mark()
        ident_f = cst[:, C_ID:C_ID + 128]

        def load_w_bf(dst3, src2, kchunks, ncols, bW):
            for kc in range(kchunks):
                for c0 in range(0, ncols, 2048):
                    c1 = min(ncols, c0 + 2048)
                    dma(dst3[:, kc, c0:c1], src2[kc * 128:(kc + 1) * 128, c0:c1], (), [bW], bW, eng="gpsimd")

        class Norm:
            def __init__(self, nslots=4, njunk=4):
                self.st = rot_f32("nst", nslots, 4)
                self.junks = rot_bf("njunk", njunk, 1024)

            def rstd(self, srcs, R):
                st, bs = self.st.next()
                self.last_st = st
                col = 0
                for s_ap in srcs:
                    n = s_ap.shape[-1]
                    jk, bj = self.junks.next()
                    act(jk[:, 0:n], s_ap, AF.Square, R, [bj, bs], accum=st[:, col:col + 1])
                    col += 1
                if len(srcs) == 2:
                    tt("vector", st[:, 0:1], st[:, 0:1], st[:, 1:2], ALU.add, [bs], [bs])
                act(st[:, 2:3], st[:, 0:1], AF.Sqrt, [bs], [bs], scale=1.0 / DM, bias=EPS)
                recip(st[:, 3:4], st[:, 2:3], [bs], [bs])
                return st[:, 3:4], bs

        def phase_A(l, h_src, b_src):
            P.barrier(); A.reset(m0)
            G = 512; NT = 4
            Wt = A.bf(8 * NIN).rearrange("p (k n) -> p k n", k=8); bW = P.buf("Win")
            load_w_bf(Wt, win_d[l], 8, NIN, bW)
            gbc = A.f32(DM); bG = P.buf("gAin")
            dma(gbc, gains_d[l, 0], (), [bG], bG)
            nrm = Norm()
            hs = Rot([(A.f32(NT * DM).rearrange("p (i d) -> p i d", i=NT), P.buf(f"hA{j}")) for j in range(2)])
            ub = rot_bf("ubf", 2, DM)
            uT = Rot([(A.bf(8 * G).rearrange("p (k t) -> p k t", k=8), P.buf(f"uT{j}")) for j in range(2)])
            stf = rot_f32("stf", 4, 512); stb = rot_bf("stb", 6, 512)
            dvs = Rot([(A.bf(520).rearrange("p (h e) -> p h e", h=8), P.buf(f"dvs{j}")) for j in range(2)])
            for ap_, b_ in dvs.items:
                mset("vector", ap_, 1.0, [b_])
            pt = Rot([(psb[0], PSB[0]), (psb[1], PSB[1])])
            pm = Rot([(psf[i], PSB[i]) for i in range(2, 8)])
            FM = []
            for i in range(2): FM.append((O_GQ + 128 * i, 128, "gqT", 128 * i, "sc_f"))
            for i in range(2): FM.append((O_GK + 128 * i, 128, "gkT", 128 * i, "cp_f"))
            for i in range(4): FM.append((O_GG + 128 * i, 128, "sgT", 128 * i, "silu"))
            FM.append((O_GLR, 16, "glrT", 0, "cp_f"))
            for i in range(4): FM.append((O_DQ + 128 * i, 128, "dqT", 128 * i, "sc_b"))
            for i in range(4): FM.append((O_DK + 128 * i, 128, "dkT", 128 * i, "cp_b"))
            for i in range(4): FM.append((O_IQ + 128 * i, 128, "iqT", 128 * i, "cp_b"))
            FM.append((O_IK, 64, "ikT", 0, "cp_b"))
            for i in range(8): FM.append((O_GA + 128 * i, 128, "sgaT", 128 * i, "sig"))
            for i in range(8): FM.append((O_GB + 128 * i, 128, "sgbT", 128 * i, "sig"))
            ngrp = T // G
            loaded = {}

            def load(g):
                hb, bh = hs.next()
                dma(hb, h_src[g * G:(g + 1) * G, :].rearrange("(i p) d -> p i d", p=128), [b_src], [bh], bh)
                loaded[g] = (hb, bh)

            load(0)
            for g in range(ngrp):
                if g + 1 < ngrp: load(g + 1)
                hb, bh = loaded.pop(g)
                uTt, buT = uT.next()
                for i in range(NT):
                    rs, brs = nrm.rstd([hb[:, i, :]], [bh])
                    rs_dbg = (nrm.last_st, brs)
                    u, bu = ub.next()
                    stt(u, hb[:, i, :], rs, gbc, ALU.mult, ALU.mult, [bh, brs, bG], [bu])
                    ptile, bpt = pt.next()
                    for kc in range(8):
                        tr(ptile[:, kc * 128:(kc + 1) * 128], u[:, kc * 128:(kc + 1) * 128], ident_bf, [bu, bC], [bpt])
                    cp("vector" if i % 2 == 0 else "scalar", uTt[:, :, i * 128:(i + 1) * 128],
                       ptile.rearrange("p (k t) -> p k t", k=8), [bpt], [buT])
                if cfg.get("dumpA") and g == 0:
                    d1 = nc.dram_tensor("dbg_st", [128, 4], F32, kind="ExternalOutput").ap()
                    d2 = nc.dram_tensor("dbg_u", [128, DM], BF16, kind="ExternalOutput").ap()
                    d3 = nc.dram_tensor("dbg_uT", [128, 8 * G], BF16, kind="ExternalOutput").ap()
                    d4 = nc.dram_tensor("dbg_W", [128, 512], BF16, kind="ExternalOutput").ap()
                    d5 = nc.dram_tensor("dbg_g", [128, DM], F32, kind="ExternalOutput").ap()
                    bdd = P.buf("dbgd")
                    dma(d1, rs_dbg[0], [rs_dbg[1]], [bdd], rs_dbg[1])
                    dma(d2, u, [bu], [bdd], bu)
                    dma(d3, uTt.rearrange("p k t -> p (k t)"), [buT], [bdd], buT)
                    dma(d4, Wt[:, 0, 0:512], [bW], [bdd], bW)
                    dma(d5, gbc, [bG], [bdd], bG)
                for (c0, m, dst, r0, kind) in FM:
                    pp, bp = pm.next()
                    for kc in range(8):
                        mm(pp[0:m, :], Wt[:, kc, c0:c0 + m], uTt[:, kc, :], kc == 0, kc == 7, [bW, buT], [bp])
                    if kind in ("sc_f", "cp_f"):
                        sg_, bs_ = stf.next()
                    else:
                        sg_, bs_ = stb.next()
                    if kind in ("sc_f", "sc_b"):
                        ts("vector", sg_[0:m, :], pp[0:m, :], 0.125, None, ALU.mult, None, [bp], [bs_])
                    elif kind in ("cp_f", "cp_b"):
                        cp("vector", sg_[0:m, :], pp[0:m, :], [bp], [bs_])
                    elif kind == "silu":
                        act(sg_[0:m, :], pp[0:m, :], AF.Silu, [bp], [bs_])
                    else:
                        act(sg_[0:m, :], pp[0:m, :], AF.Sigmoid, [bp], [bs_])
                    dma(SCR[dst][r0:r0 + m, g * G:(g + 1) * G], sg_[0:m, :], [bs_], [DB[dst]], bs_)
                for i in range(NT):
                    t0 = g * G + i * 128
                    lh = lambda kc: uTt[:, kc, i * 128:(i + 1) * 128]
                    pp, bp = pm.next()
                    for kc in range(8):
                        mm(pp[:, 0:256], lh(kc), Wt[:, kc, O_GK:O_GK + 256], kc == 0, kc == 7, [bW, buT], [bp])
                    sg_, bs_ = stf.next()
                    cp("vector", sg_[:, 0:256], pp[:, 0:256], [bp], [bs_])
                    dma(gk[t0:t0 + 128, :], sg_[:, 0:256], [bs_], [DB["gk"]], bs_)
                    pp, bp = pm.next()
                    for kc in range(8):
                        mm(pp[:, 0:8], lh(kc), Wt[:, kc, O_IW:O_IW + 8], kc == 0, kc == 7, [bW, buT], [bp])
                    sg_, bs_ = stf.next()
                    cp("vector", sg_[:, 0:8], pp[:, 0:8], [bp], [bs_])
                    dma(iw[t0:t0 + 128, :], sg_[:, 0:8], [bs_], [DB["iw"]], bs_)
                    pp, bp = pm.next()
                    for kc in range(8):
                        mm(pp, lh(kc), Wt[:, kc, O_GV:O_GV + 512], kc == 0, kc == 7, [bW, buT], [bp])
                    sg_, bs_ = stb.next()
                    cp("scalar", sg_, pp, [bp], [bs_])
                    dma(gv[t0:t0 + 128, :], sg_, [bs_], [DB["gv"]], bs_)
                    pp, bp = pm.next()
                    for kc in range(8):
                        mm(pp, lh(kc), Wt[:, kc, O_DV:O_DV + 512], kc == 0, kc == 7, [bW, buT], [bp])
                    dv_, bdv = dvs.next()
                    cp("vector", dv_[:, :, 0:64], pp.rearrange("p (h e) -> p h e", h=8), [bp], [bdv])
                    dma(dvx[t0:t0 + 128, :], dv_.rearrange("p h e -> p (h e)"), [bdv], [DB["dvx"]], bdv)

        def phase_B(l):
            P.barrier(); A.reset(m0)
            w2p = A.f32(256); gno = A.f32(1); bK = P.buf("Bconst")
            mset("vector", w2p, 0.0, [bK])
            dma(w2p[0:16, :], w2_d[l], (), [bK], bK); dma(w2p[32:33, :], gb_d[l], (), [bK], bK); dma(gno, gn_d[l], (), [bK], bK)
            Lneg = cst[:, C_LNEG:C_LNEG + 128]; Uneg = cst[:, C_UNEG:C_UNEG + 128]
            onesm = cst[:, C_ONESM:C_ONESM + 128]

            def padded(name, n, cols, dt_bf, view=None):
                items = []
                for j_ in range(n):
                    ap_ = A.bf(cols) if dt_bf else A.f32(cols)
                    b_ = P.buf(f"{name}{j_}")
                    mset("vector", ap_, 0.0, [b_])
                    items.append((ap_, b_))
                return Rot(items)

            qT2 = padded("qT2", 2, 512, False); kT2 = padded("kT2", 2, 512, False)
            kk = rot_f32("kk", 2, 256); vv = rot_bf("vv", 6, 512)
            lr = padded("lr", 2, 128, False)
            for ap_, b_ in lr.items:
                mset("vector", ap_[32:33, :], 1.0, [b_])
            sg = Rot([(A.bf(512).rearrange("p (c t) -> p c t", c=4), P.buf(f"sgB{j}")) for j in range(6)])
            ee = rot_f32("ee", 2, 256)
            ll = padded("ll", 2, 320, False)
            ebT = rot_f32("ebT", 6, 512); enbT = rot_f32("enbT", 2, 512); ecs = rot_f32("ecs", 2, 256)
            qt = padded("qt", 6, 512, True); kt = padded("kt", 6, 512, True)
            kh = padded("kh", 6, 320, True)
            att = rot_bf("att", 4, 512); sq = rot_f32("sq", 4, 512); sd = rot_f32("sd", 4, 512)
            o1 = rot_f32("o1", 4, 512)
            oas = Rot([(A.bf(512).rearrange("p (c t) -> p c t", c=4), P.buf(f"oas{j}")) for j in range(4)])
            Sst = [(A.f32(512), P.buf(f"S{s}")) for s in range(NSEQ)]
            Sbf = [padded(f"Sb{s}", 2, 512, True) for s in range(NSEQ)]
            cur_sbf = {}
            for s in range(NSEQ):
                mset("vector", Sst[s][0], 0.0, [Sst[s][1]])
                cur_sbf[s] = Sbf[s].next()
            px = (psf[0], PSB[0]); ppc = (psf[0][:, 256:512], PSB[0]); ppb = (psf[1], PSB[1])

            def stage_a(n, s):
                t0 = s * S + n * 128
                q2, bq2 = qT2.next(); k2, bk2 = kT2.next(); kk_, bkk = kk.next(); vv_, bvv = vv.next()
                lr_, blr = lr.next(); sg_, bsg = sg.next()
                dma(q2[0:64, :].rearrange("p (h t) -> p h t", h=4), gqT.rearrange("(h d) t -> d h t", d=64)[:, :, t0:t0 + 128],
                    [DB["gqT"]], [bq2], bq2)
                dma(k2[0:64, :].rearrange("p (h t) -> p h t", h=4), gkT.rearrange("(h d) t -> d h t", d=64)[:, :, t0:t0 + 128],
                    [DB["gkT"]], [bk2], bk2)
                dma(kk_, gk[t0:t0 + 128, :], [DB["gk"]], [bkk], bkk)
                dma(vv_, gv[t0:t0 + 128, :], [DB["gv"]], [bvv], bvv)
                dma(lr_[0:16, :], glrT[:, t0:t0 + 128], [DB["glrT"]], [blr], blr)
                dma(sg_, sgT.rearrange("(c p) t -> p c t", p=128)[:, :, t0:t0 + 128], [DB["sgT"]], [bsg], bsg)
                mm(px[0][:, 0:256], lr_, w2p, True, True, [blr, bK], [px[1]])
                e_, be = ee.next(); l_, bl = ll.next()
                act(e_, px[0][:, 0:256], AF.Exp, [px[1]], [be], scale=-1.0)
                act(l_[:, 0:256], e_, AF.Ln, [be], [bl], bias=1.0)
                for hh in range(4):
                    mm(ppb[0][:, hh * 128:(hh + 1) * 128], l_[:, hh * 64:hh * 64 + 128], Lneg, True, True, [bl, bC], [ppb[1]])
                mm(ppc[0][:, 0:256], Uneg, l_[:, 0:256], True, True, [bl, bC], [ppc[1]])
                eb, beb = ebT.next(); enb, benb = enbT.next(); ec, bec = ecs.next()
                act(eb[0:64, :], ppb[0][0:64, :], AF.Exp, [ppb[1]], [beb])
                act(enb[0:64, :], ppb[0][0:64, :], AF.Exp, [ppb[1]], [benb], scale=-1.0)
                act(ec, ppc[0][:, 0:256], AF.Exp, [ppc[1]], [bec])
                qt_, bqt = qt.next(); kt_, bkt = kt.next(); kh_, bkh = kh.next()
                tt("vector", qt_[0:64, :], q2[0:64, :], eb[0:64, :], ALU.mult, [bq2, beb], [bqt])
                tt("vector", kt_[0:64, :], k2[0:64, :], enb[0:64, :], ALU.mult, [bk2, benb], [bkt])
                tt("gpsimd", kh_[:, 0:256], kk_, ec, ALU.mult, [bkk, bec], [bkh])
                return (n, s, t0, qt_, bqt, kt_, bkt, kh_, bkh, vv_, bvv, sg_, bsg, eb, beb)

            def stage_b_group(ctxs):
                ns = len(ctxs)
                bank = lambda s_, r_: (psf[2 + 3 * s_ + r_], PSB[2 + 3 * s_ + r_])
                ats = []
                for (n, s, t0, qt_, bqt, kt_, bkt, kh_, bkh, vv_, bvv, sg_, bsg, eb, beb) in ctxs:
                    patt = bank(s, 0)
                    for hh in range(4):
                        mm(patt[0][:, hh * 128:(hh + 1) * 128], kt_[:, hh * 128:(hh + 1) * 128], qt_[:, hh * 128:(hh + 1) * 128],
                           True, True, [bkt, bqt], [patt[1]])
                for (n, s, t0, qt_, bqt, kt_, bkt, kh_, bkh, vv_, bvv, sg_, bsg, eb, beb) in ctxs:
                    patt = bank(s, 0)
                    at_, bat = att.next()
                    tt("vector", at_, patt[0], tri4, ALU.mult, [patt[1], bC], [bat])
                    ats.append((at_, bat))
                for ci, (n, s, t0, qt_, bqt, kt_, bkt, kh_, bkh, vv_, bvv, sg_, bsg, eb, beb) in enumerate(ctxs):
                    ppo = bank(s, 1); at_, bat = ats[ci]
                    sb_, bsb = cur_sbf[s]
                    for hh in range(4):
                        mm(ppo[0][:, hh * 128:(hh + 1) * 128], vv_[:, hh * 128:(hh + 1) * 128], at_[:, hh * 128:(hh + 1) * 128],
                           True, False, [bvv, bat], [ppo[1]])
                        mm(ppo[0][:, hh * 128:(hh + 1) * 128], sb_[:, hh * 128:(hh + 1) * 128], qt_[:, hh * 128:(hh + 1) * 128],
                           False, True, [bsb, bqt], [ppo[1]])
                for (n, s, t0, qt_, bqt, kt_, bkt, kh_, bkh, vv_, bvv, sg_, bsg, eb, beb) in ctxs:
                    ppS = bank(s, 0)
                    for hh in range(4):
                        mm(ppS[0][:, hh * 128:(hh + 1) * 128], kh_[:, hh * 64:hh * 64 + 128], vv_[:, hh * 128:(hh + 1) * 128],
                           True, True, [bkh, bvv], [ppS[1]])
                sqs = []
                for (n, s, t0, qt_, bqt, kt_, bkt, kh_, bkh, vv_, bvv, sg_, bsg, eb, beb) in ctxs:
                    ppo = bank(s, 1)
                    sq_, bsq = sq.next()
                    act(sq_, ppo[0], AF.Square, [ppo[1]], [bsq])
                    sqs.append((sq_, bsq))
                for (n, s, t0, qt_, bqt, kt_, bkt, kh_, bkh, vv_, bvv, sg_, bsg, eb, beb) in ctxs:
                    ppS = bank(s, 0)
                    S_, bS = Sst[s]
                    for hh in range(4):
                        cs_ = slice(hh * 128, (hh + 1) * 128)
                        stt(S_[0:64, cs_], S_[0:64, cs_], eb[0:64, hh * 128 + 127:hh * 128 + 128], ppS[0][0:64, cs_],
                            ALU.mult, ALU.add, [bS, beb, ppS[1]], [bS])
                    sb_, bsb = Sbf[s].next()
                    cp("gpsimd", sb_[0:64, :], S_[0:64, :], [bS], [bsb])
                    cur_sbf[s] = (sb_, bsb)
                for ci, (n, s, t0, qt_, bqt, kt_, bkt, kh_, bkh, vv_, bvv, sg_, bsg, eb, beb) in enumerate(ctxs):
                    ppm = bank(s, 2); sq_, bsq = sqs[ci]
                    mm(ppm[0], onesm, sq_, True, True, [bC, bsq], [ppm[1]])
                for (n, s, t0, qt_, bqt, kt_, bkt, kh_, bkh, vv_, bvv, sg_, bsg, eb, beb) in ctxs:
                    ppo = bank(s, 1); ppm = bank(s, 2)
                    sd_, bsd = sd.next(); o1_, bo1 = o1.next(); oa_, boa = oas.next()
                    act(sd_, ppm[0], AF.Sqrt, [ppm[1]], [bsd], bias=EPS)
                    recip(sd_, sd_, [bsd], [bsd])
                    stt(o1_, ppo[0], gno[:, 0:1], sd_, ALU.mult, ALU.mult, [ppo[1], bK, bsd], [bo1])
                    tt("gpsimd", oa_.rearrange("p c t -> p (c t)"), o1_, sg_.rearrange("p c t -> p (c t)"), ALU.mult,
                       [bo1, bsg], [boa])
                    dma(oaT.rearrange("(c p) t -> p c t", p=128)[:, :, t0:t0 + 128], oa_, [boa], [DB["oaT"]], boa)

            pend = [stage_a(0, s) for s in range(NSEQ)]
            for n in range(NB):
                nxt = [stage_a(n + 1, s) for s in range(NSEQ)] if n + 1 < NB else None
                stage_b_group(pend)
                pend = nxt

        def phase_C(l):
            P.barrier(); A.reset(m0)
            dk_s = A.bf(8 * S).rearrange("p (c t) -> p c t", c=8); bdk = P.buf("dk_s")
            dv_s = A.bf(NB * 520).rearrange("p (k e) -> p k e", k=NB); bdv = P.buf("dv_s")
            ik2 = A.bf(S); bik = P.buf("ik2")
            mset("vector", dk_s[64:128], 0.0, [bdk]); mset("vector", ik2[64:128, :], 0.0, [bik])
            acc = A.f32(S); bacc = P.buf("acc")
            junk = A.bf(S); bjunk = P.buf("junkC")
            madd = rot_bf("madd", 2, S)
            rr = rot_f32("rr", 3, 512); PT = rot_bf("PT", 3, 512)
            iqb = Rot([(A.bf(1024).rearrange("p (c t) -> p c t", c=8), P.buf(f"iqb{j}")) for j in range(2)])
            dqb = Rot([(A.bf(1024).rearrange("p (c t) -> p c t", c=8), P.buf(f"dqb{j}")) for j in range(3)])
            for ap_, b_ in iqb.items + dqb.items:
                mset("vector", ap_[64:128], 0.0, [b_])
            iwb = rot_f32("iwb", 2, 8)
            ob = rot_bf("ob", 2, 512)
            obs = Rot([(A.bf(512).rearrange("p (c t) -> p c t", c=4), P.buf(f"obs{j}")) for j in range(2)])
            stat = rot_f32("statC", 2, 8 + 2 * NIT)
            recs = rot_f32("recC", 2, 8)
            statA = rot_f32("statA", 2, 2 * NIT + 4)
            junkA = A.bf(S); bjunkA = P.buf("junkA")
            blk_count = [0]
            cneg = cst[:, C_CNEG:C_CNEG + 128]; pow2 = cst[:, C_POW2:C_POW2 + NIT]
            pi = Rot([(psf[0], PSB[0]), (psf[1], PSB[1])])
            pl = Rot([(psf[2], PSB[2]), (psf[3], PSB[3]), (psf[4], PSB[4])])
            po = [(psf[5], PSB[5]), (psf[6], PSB[6])]
            ptr = (psb[7], PSB[7])
            cur_seq = {"s1": -1, "s2": -1}

            def stage1(s, j):
                s0 = s * S
                if cur_seq["s1"] != s:
                    cur_seq["s1"] = s
                    dma(ik2[0:64, :], ikT[:, s0:s0 + S], [DB["ikT"]], [bik], bik)
                t0 = s0 + j * 128; nk = j + 1; NK = nk * 128
                iq_, biq = iqb.next(); dq_, bdq = dqb.next(); iw_, biw = iwb.next()
                dma(iq_[0:64], iqT.rearrange("(h d) t -> d h t", d=64)[:, :, t0:t0 + 128], [DB["iqT"]], [biq], biq)
                dma(dq_[0:64], dqT.rearrange("(h d) t -> d h t", d=64)[:, :, t0:t0 + 128], [DB["dqT"]], [bdq], bdq)
                dma(iw_, iw[t0:t0 + 128, :], [DB["iw"]], [biw], biw)
                for hh in range(8):
                    for c4 in range(0, NK, 512):
                        w = min(512, NK - c4)
                        pp, bp = pi.next()
                        mm(pp[:, 0:w], iq_[:, hh, :], ik2[:, c4:c4 + w], True, True, [biq, bik], [bp])
                        r_, br = rr.next()
                        act(r_[:, 0:w], pp[:, 0:w], AF.Relu, [bp], [br])
                        if hh == 0:
                            ts("vector", acc[:, c4:c4 + w], r_[:, 0:w], iw_[:, 0:1], None, ALU.mult, None, [br, biw], [bacc])
                        else:
                            stt(acc[:, c4:c4 + w], r_[:, 0:w], iw_[:, hh:hh + 1], acc[:, c4:c4 + w], ALU.mult, ALU.add,
                                [br, biw, bacc], [bacc])
                st, bst = stat.next()
                lo = st[:, 0:1]; hi = st[:, 1:2]; w0 = st[:, 2:3]; mid = st[:, 3:4]; cnt = st[:, 4:5]; step = st[:, 5:6]
                wtab = st[:, 8:8 + NIT]; twt = st[:, 8 + NIT:8 + 2 * NIT]
                red(hi, acc[:, 0:NK], ALU.max, [bacc], [bst])
                red(lo, acc[:, 0:NK], ALU.min, [bacc], [bst])
                tt("vector", w0, hi, lo, ALU.subtract, [bst], [bst])
                ts("vector", wtab, pow2, w0, None, ALU.mult, None, [bC, bst], [bst])
                tt("vector", acc[:, j * 128:(j + 1) * 128], acc[:, j * 128:(j + 1) * 128], cneg, ALU.add, [bacc, bC], [bacc])
                ma, bma = madd.next()
                rc, brc = recs.next()
                deferred = []
                use_act = ACT_BISECT and (blk_count[0] % 2 == 1) and nk >= 3
                blk_count[0] += 1
                if not use_act:
                    if NK > TOPK:
                        ts("vector", twt, wtab, 2.0, None, ALU.mult, None, [bst], [bst])
                        tt("vector", mid, lo, wtab[:, 0:1], ALU.add, [bst], [bst])
                        for k in range(NIT):
                            ts("vector", junk[:, 0:NK], acc[:, 0:NK], mid, None, ALU.is_ge, ALU.add, [bacc, bst], [bjunk, bst], accum=cnt)
                            if k < NIT - 1:
                                stt(step, cnt, TOPK - 0.5, twt[:, k + 1:k + 2], ALU.is_ge, ALU.mult, [bst], [bst])
                                stt(mid, step, wtab[:, k + 1:k + 2], mid, ALU.subtract, ALU.add, [bst], [bst])
                            else:
                                stt(step, cnt, TOPK - 0.5, wtab[:, k:k + 1], ALU.is_ge, ALU.mult, [bst], [bst])
                                stt(lo, mid, wtab[:, k:k + 1], step, ALU.subtract, ALU.add, [bst], [bst])
                    ts("vector", ma[:, 0:NK], acc[:, 0:NK], lo, NEG, ALU.is_lt, ALU.mult, [bacc, bst], [bma])
                else:
                    sa, bsa = statA.next()
                    nwt = sa[:, 0:NIT]; hwt = sa[:, NIT:2 * NIT]; nmid = sa[:, 2 * NIT:2 * NIT + 1]
                    sgs = sa[:, 2 * NIT + 1:2 * NIT + 2]; sfl = sa[:, 2 * NIT + 2:2 * NIT + 3]; stp = sa[:, 2 * NIT + 3:2 * NIT + 4]
                    ts("vector", nwt, wtab, -1.0, None, ALU.mult, None, [bst], [bsa])
                    ts("vector", hwt, wtab, 0.5, None, ALU.mult, None, [bst], [bsa])

                    def it(k, lo=lo, NK=NK, nwt=nwt, hwt=hwt, nmid=nmid, sgs=sgs, sfl=sfl, stp=stp, bst=bst, bsa=bsa):
                        act(nmid, lo, AF.Identity, [bst, bsa], [bsa], scale=-1.0, bias=nwt[:, k:k + 1])
                        act(junkA[:, 0:NK], acc[:, 0:NK], AF.Sign, [bacc, bsa], [bjunkA, bsa], bias=nmid, accum=sgs)
                        act(sfl, sgs, AF.Sign, [bsa], [bsa], bias=float(NK - 2 * TOPK + 1))
                        act(stp, sfl, AF.Identity, [bsa], [bsa], scale=hwt[:, k:k + 1], bias=hwt[:, k:k + 1])
                        act(lo, lo, AF.Identity, [bst, bsa], [bst], bias=stp)
                    for k in range(NIT):
                        deferred.append(lambda k=k: it(k))
                    deferred.append(lambda lo=lo, NK=NK, ma=ma, bma=bma, bst=bst:
                                    ts("vector", ma[:, 0:NK], acc[:, 0:NK], lo, NEG, ALU.is_lt, ALU.mult, [bacc, bst], [bma]))
                return (s, j, dq_, bdq, ma, bma, rc, brc, deferred)

            def stage2(ctx, inter):
                s, j, dq_, bdq, ma, bma, rec, bst, _d = ctx
                per_head = (len(inter) + 7) // 8
                s0 = s * S; t0 = s0 + j * 128; nk = j + 1
                if cur_seq["s2"] != s:
                    cur_seq["s2"] = s
                    dma(dk_s[0:64], dkT.rearrange("(h d) t -> d h t", d=64)[:, :, s0:s0 + S], [DB["dkT"]], [bdk], bdk)
                    dma(dv_s, dvx[s0:s0 + S, :].rearrange("(k p) e -> p k e", p=128), [DB["dvx"]], [bdv], bdv)
                for hh in range(8):
                    pob, bpo = po[hh // 4]
                    pcol = (hh % 4) * 65
                    for c4 in range(0, nk, 4):
                        kbs = list(range(c4, min(c4 + 4, nk)))
                        pp, bp = pl.next()
                        for idx, kb in enumerate(kbs):
                            sl = pp[:, idx * 128:(idx + 1) * 128]
                            near = kb >= j - 1
                            mm(sl, dk_s[:, hh, kb * 128:(kb + 1) * 128], dq_[:, hh, :], True, False, [bdk, bdq], [bp])
                            mm(sl, ma[:, kb * 128:(kb + 1) * 128], ident_bf, False, not near, [bma, bC], [bp])
                            if near:
                                bi = hh * 2 + (0 if kb == j else 1)
                                mm(sl, ident_bf, biasT_bf[:, bi * 128:(bi + 1) * 128], False, True, [bC], [bp])
                        nfar = sum(1 for kb in kbs if kb < j - 1)
                        pt_, bpt = PT.next()
                        if nfar > 0:
                            act(pt_[:, 0:nfar * 128], pp[:, 0:nfar * 128], AF.Exp, [bp, bCF], [bpt], bias=cfar[:, hh:hh + 1])
                        if nfar < len(kbs):
                            act(pt_[:, nfar * 128:len(kbs) * 128], pp[:, nfar * 128:len(kbs) * 128], AF.Exp, [bp], [bpt])
                        for idx, kb in enumerate(kbs):
                            mm(pob[:, pcol:pcol + 65], pt_[:, idx * 128:(idx + 1) * 128], dv_s[:, kb, hh * 65:(hh + 1) * 65],
                               kb == 0, kb == j, [bpt, bdv], [bpo])
                    for _ in range(per_head):
                        if inter: inter.pop(0)()
                while inter: inter.pop(0)()
                for half in range(2):
                    pob, bpo = po[half]
                    recip(rec[:, half * 4:(half + 1) * 4].rearrange("p (h o) -> p h o", o=1),
                          pob[:, 0:260].rearrange("p (h e) -> p h e", e=65)[:, :, 64:65], [bpo], [bst])
                ob_, bob = ob.next()
                for hh in range(8):
                    pob, bpo = po[hh // 4]
                    pcol = (hh % 4) * 65
                    if hh % 2 == 0:
                        ts("vector", ob_[:, hh * 64:(hh + 1) * 64], pob[:, pcol:pcol + 64], rec[:, hh:hh + 1], None, ALU.mult, None,
                           [bpo, bst], [bob])
                    else:
                        act(ob_[:, hh * 64:(hh + 1) * 64], pob[:, pcol:pcol + 64], AF.Copy, [bpo, bst], [bob], scale=rec[:, hh:hh + 1])
                for c in range(4):
                    tr(ptr[0][:, c * 128:(c + 1) * 128], ob_[:, c * 128:(c + 1) * 128], ident_bf, [bob, bC], [ptr[1]])
                os_, bos = obs.next()
                cp("scalar", os_, ptr[0][:, 0:512].rearrange("p (c t) -> p c t", c=4), [ptr[1]], [bos])
                dma(obT.rearrange("(c p) t -> p c t", p=128)[:, :, t0:t0 + 128], os_, [bos], [DB["obT"]], bos)

            blocks = [(s, j) for s in range(NSEQ) for j in range(NB)]
            pend = stage1(*blocks[0])
            while pend[-1]: pend[-1].pop(0)()
            for bi_ in range(len(blocks)):
                nxt = stage1(*blocks[bi_ + 1]) if bi_ + 1 < len(blocks) else None
                stage2(pend, nxt[-1] if nxt is not None else [])
                pend = nxt

        def phase_D(l, h_src, b_src, h_dst, b_dst):
            P.barrier(); A.reset(m0)
            G = 512; NT = 4
            Wa = A.bf(4 * DM).rearrange("p (k n) -> p k n", k=4); Wb = A.bf(4 * DM).rearrange("p (k n) -> p k n", k=4)
            Wo = A.bf(8 * DM).rearrange("p (k n) -> p k n", k=8); bW = P.buf("WD")
            load_w_bf(Wa, wa_d[l], 4, DM, bW); load_w_bf(Wb, wb_d[l], 4, DM, bW); load_w_bf(Wo, wo_d[l], 8, DM, bW)
            gbc = A.f32(DM); bG = P.buf("gD")
            dma(gbc, gains_d[l, 1], (), [bG], bG)
            nrm = Norm()
            oa = Rot([(A.bf(4 * G).rearrange("p (c t) -> p c t", c=4), P.buf(f"oaD{j}")) for j in range(2)])
            obb = Rot([(A.bf(4 * G).rearrange("p (c t) -> p c t", c=4), P.buf(f"obD{j}")) for j in range(2)])
            sa = Rot([(A.bf(8 * G).rearrange("p (c t) -> p c t", c=8), P.buf(f"saD{j}")) for j in range(2)])
            sbb = Rot([(A.bf(8 * G).rearrange("p (c t) -> p c t", c=8), P.buf(f"sbD{j}")) for j in range(2)])
            hs = Rot([(A.f32(NT * DM).rearrange("p (i d) -> p i d", i=NT), P.buf(f"hD{j}")) for j in range(2)])
            m1 = rot_f32("m1", 2, G); m2 = rot_f32("m2", 2, G)
            mxT = Rot([(A.bf(8 * G).rearrange("p (k t) -> p k t", k=8), P.buf(f"mxT{j}")) for j in range(2)])
            tmp = rot_f32("tmpD", 2, DM)
            pm = Rot([(psf[i], PSB[i]) for i in range(8)])
            ngrp = T // G
            loaded = {}

            def load(g):
                sl = slice(g * G, (g + 1) * G)
                a_, ba = oa.next(); b_, bb = obb.next(); sa_, bsa = sa.next(); sb_, bsb = sbb.next(); h_, bh = hs.next()
                dma(a_, oaT.rearrange("(c p) t -> p c t", p=128)[:, :, sl], [DB["oaT"]], [ba], ba)
                dma(b_, obT.rearrange("(c p) t -> p c t", p=128)[:, :, sl], [DB["obT"]], [bb], bb)
                dma(sa_, sgaT.rearrange("(c p) t -> p c t", p=128)[:, :, sl], [DB["sgaT"]], [bsa], bsa)
                dma(sb_, sgbT.rearrange("(c p) t -> p c t", p=128)[:, :, sl], [DB["sgbT"]], [bsb], bsb)
                dma(h_, h_src[sl, :].rearrange("(i p) d -> p i d", p=128), [b_src], [bh], bh)
                loaded[g] = (a_, ba, b_, bb, sa_, bsa, sb_, bsb, h_, bh)

            load(0)
            for g in range(ngrp):
                if g + 1 < ngrp: load(g + 1)
                a_, ba, b_, bb, sa_, bsa, sb_, bsb, h_, bh = loaded.pop(g)
                mx, bmx = mxT.next()
                for ncb in range(8):
                    pa, bpa = pm.next(); pb, bpb = pm.next()
                    for kc in range(4):
                        mm(pa, Wa[:, kc, ncb * 128:(ncb + 1) * 128], a_[:, kc, :], kc == 0, kc == 3, [bW, ba], [bpa])
                    for kc in range(4):
                        mm(pb, Wb[:, kc, ncb * 128:(ncb + 1) * 128], b_[:, kc, :], kc == 0, kc == 3, [bW, bb], [bpb])
                    m1_, bm1 = m1.next(); m2_, bm2 = m2.next()
                    tt("vector", m1_, pa, sa_[:, ncb, :], ALU.mult, [bpa, bsa], [bm1])
                    tt("vector", m2_, pb, sb_[:, ncb, :], ALU.mult, [bpb, bsb], [bm2])
                    tt("gpsimd", mx[:, ncb, :], m1_, m2_, ALU.add, [bm1, bm2], [bmx])
                for i in range(NT):
                    t0 = g * G + i * 128
                    halves = []
                    for half in range(2):
                        pp, bp = pm.next()
                        for kc in range(8):
                            mm(pp, mx[:, kc, i * 128:(i + 1) * 128], Wo[:, kc, half * 512:(half + 1) * 512], kc == 0, kc == 7,
                               [bmx, bW], [bp])
                        halves.append((pp, bp))
                    rs, brs = nrm.rstd([halves[0][0], halves[1][0]], [halves[0][1], halves[1][1]])
                    tm, btm = tmp.next()
                    for half in range(2):
                        stt(tm[:, half * 512:(half + 1) * 512], halves[half][0], rs, gbc[:, half * 512:(half + 1) * 512],
                            ALU.mult, ALU.mult, [halves[half][1], brs, bG], [btm])
                    tt("gpsimd", h_[:, i, :], h_[:, i, :], tm, ALU.add, [bh, btm], [bh])
                    dma(h_dst[t0:t0 + 128, :], h_[:, i, :], [bh], [b_dst], bh)

        def phase_E(l, h_src, b_src, h_dst, b_dst):
            P.barrier(); A.reset(m0)
            G = 256; NT = 2
            W1 = A.bf(8 * DFF).rearrange("p (k n) -> p k n", k=8); W2 = A.bf(32 * DM).rearrange("p (k n) -> p k n", k=32)
            bW = P.buf("WE")
            load_w_bf(W1, w1_d[l], 8, DFF, bW); load_w_bf(W2, w2m_d[l], 32, DM, bW)
            g1 = A.f32(DM); g2 = A.f32(DM); bG = P.buf("gE")
            dma(g1, gains_d[l, 2], (), [bG], bG); dma(g2, gains_d[l, 3], (), [bG], bG)
            nrm = Norm(njunk=2)
            hs = Rot([(A.f32(NT * DM).rearrange("p (i d) -> p i d", i=NT), P.buf(f"hE{j}")) for j in range(2)])
            ub = rot_bf("ubE", 2, DM)
            uT = Rot([(A.bf(8 * G).rearrange("p (k t) -> p k t", k=8), P.buf(f"uTE{j}")) for j in range(2)])
            a1 = Rot([(A.bf(32 * G).rearrange("p (k t) -> p k t", k=32), P.buf(f"a1E{j}")) for j in range(1)])
            rl = rot_f32("rlE", 3, G)
            tmp = rot_f32("tmpE", 1, DM)
            pt = Rot([(psb[0], PSB[0]), (psb[1], PSB[1])])
            pm = Rot([(psf[i], PSB[i]) for i in range(2, 8)])
            ngrp = T // G
            loaded = {}

            def load(g):
                h_, bh = hs.next()
                dma(h_, h_src[g * G:(g + 1) * G, :].rearrange("(i p) d -> p i d", p=128), [b_src], [bh], bh)
                loaded[g] = (h_, bh)

            load(0)
            for g in range(ngrp):
                if g + 1 < ngrp: load(g + 1)
                h_, bh = loaded.pop(g)
                uTt, buT = uT.next()
                for i in range(NT):
                    rs, brs = nrm.rstd([h_[:, i, :]], [bh])
                    u, bu = ub.next()
                    stt(u, h_[:, i, :], rs, g1, ALU.mult, ALU.mult, [bh, brs, bG], [bu])
                    ptile, bpt = pt.next()
                    for kc in range(8):
                        tr(ptile[:, kc * 128:(kc + 1) * 128], u[:, kc * 128:(kc + 1) * 128], ident_bf, [bu, bC], [bpt])
                    cp("vector" if i % 2 == 0 else "scalar", uTt[:, :, i * 128:(i + 1) * 128],
                       ptile.rearrange("p (k t) -> p k t", k=8), [bpt], [buT])
                a1_, ba1 = a1.next()
                for fc in range(32):
                    pp, bp = pm.next()
                    for kc in range(8):
                        mm(pp[:, 0:G], W1[:, kc, fc * 128:(fc + 1) * 128], uTt[:, kc, :], kc == 0, kc == 7, [bW, buT], [bp])
                    r_, br = rl.next()
                    act(r_, pp[:, 0:G], AF.Relu, [bp], [br])
                    tt("vector" if fc % 2 == 0 else "gpsimd", a1_[:, fc, :], r_, r_, ALU.mult, [br], [ba1])
                for i in range(NT):
                    t0 = g * G + i * 128
                    halves = []
                    for half in range(2):
                        pp, bp = pm.next()
                        for fc in range(32):
                            mm(pp, a1_[:, fc, i * 128:(i + 1) * 128], W2[:, fc, half * 512:(half + 1) * 512], fc == 0, fc == 31,
                               [ba1, bW], [bp])
                        halves.append((pp, bp))
                    rs, brs = nrm.rstd([halves[0][0], halves[1][0]], [halves[0][1], halves[1][1]])
                    tm, btm = tmp.next()
                    for half in range(2):
                        stt(tm[:, half * 512:(half + 1) * 512], halves[half][0], rs, g2[:, half * 512:(half + 1) * 512],
                            ALU.mult, ALU.mult, [halves[half][1], brs, bG], [btm])
                    tt("gpsimd", h_[:, i, :], h_[:, i, :], tm, ALU.add, [bh, btm], [bh])
                    dma(h_dst[t0:t0 + 128, :], h_[:, i, :], [bh], [b_dst], bh)

        def phase_F(l, h_src, b_src, h_dst, b_dst):
            P.barrier(); A.reset(m0)
            Wp = A.bf(2 * DM).rearrange("p (k n) -> p k n", k=2); Wg = A.bf(8 * DM).rearrange("p (k n) -> p k n", k=8)
            bW = P.buf("WF")
            load_w_bf(Wp, wple_d[l], 2, DM, bW); load_w_bf(Wg, wpg_d[l], 8, DM, bW)
            g5 = A.f32(DM); bG = P.buf("gF")
            dma(g5, gains_d[l, 4], (), [bG], bG)
            nrm = Norm()
            hs = rot_f32("hF", 3, DM); ps_ = rot_f32("pF", 3, PLE)
            hb = rot_bf("hbF", 2, DM); pb = rot_bf("pbF", 2, PLE)
            hT = Rot([(A.bf(8 * 128).rearrange("p (k t) -> p k t", k=8), P.buf(f"hTF{j}")) for j in range(2)])
            pT = Rot([(A.bf(2 * 128).rearrange("p (k t) -> p k t", k=2), P.buf(f"pTF{j}")) for j in range(2)])
            sg = rot_f32("sgF", 2, DM); ee = rot_f32("eF", 2, DM); tmp = rot_f32("tmpF", 2, DM)
            pt = Rot([(psb[0], PSB[0]), (psb[1], PSB[1])])
            pm = Rot([(psf[i], PSB[i]) for i in range(2, 8)])
            ntile = T // 128
            loaded = {}

            def load(i):
                h_, bh = hs.next(); p_, bp = ps_.next()
                dma(h_, h_src[i * 128:(i + 1) * 128, :], [b_src], [bh], bh)
                dma(p_, p_d[l, i * 128:(i + 1) * 128, :], [DB["p"]], [bp], bp)
                loaded[i] = (h_, bh, p_, bp)

            load(0)
            for i in range(ntile):
                if i + 1 < ntile: load(i + 1)
                h_, bh, p_, bpp = loaded.pop(i)
                hb_, bhb = hb.next(); pb_, bpb = pb.next()
                cp("vector", hb_, h_, [bh], [bhb]); cp("gpsimd", pb_, p_, [bpp], [bpb])
                ptile, bpt = pt.next()
                for kc in range(8):
                    tr(ptile[:, kc * 128:(kc + 1) * 128], hb_[:, kc * 128:(kc + 1) * 128], ident_bf, [bhb, bC], [bpt])
                hT_, bhT = hT.next()
                cp("scalar", hT_, ptile.rearrange("p (k t) -> p k t", k=8), [bpt], [bhT])
                ptile, bpt = pt.next()
                for kc in range(2):
                    tr(ptile[:, kc * 128:(kc + 1) * 128], pb_[:, kc * 128:(kc + 1) * 128], ident_bf, [bpb, bC], [bpt])
                pT_, bpT = pT.next()
                cp("vector", pT_, ptile[:, 0:256].rearrange("p (k t) -> p k t", k=2), [bpt], [bpT])
                sg_, bsg = sg.next(); e_, be = ee.next()
                for half in range(2):
                    cs = slice(half * 512, (half + 1) * 512)
                    pg, bpg = pm.next()
                    for kc in range(8):
                        mm(pg, hT_[:, kc, :], Wg[:, kc, cs], kc == 0, kc == 7, [bhT, bW], [bpg])
                    act(sg_[:, cs], pg, AF.Sigmoid, [bpg], [bsg])
                    pe, bpe = pm.next()
                    for kc in range(2):
                        mm(pe, pT_[:, kc, :], Wp[:, kc, cs], kc == 0, kc == 1, [bpT, bW], [bpe])
                    tt("vector", e_[:, cs], pe, sg_[:, cs], ALU.mult, [bpe, bsg], [be])
                rs, brs = nrm.rstd([e_], [be])
                tm, btm = tmp.next()
                stt(tm, e_, rs, g5, ALU.mult, ALU.mult, [be, brs, bG], [btm])
                tt("gpsimd", h_, h_, tm, ALU.add, [bh, btm], [bh])
                dma(h_dst[i * 128:(i + 1) * 128, :], h_, [bh], [b_dst], bh)

        cur, bcur = x_d, DB["x"]
        for l in range(layers):
            last = (l == layers - 1)
            if "A" in phases: phase_A(l, cur, bcur)
            if "B" in phases: phase_B(l)
            if "C" in phases: phase_C(l)
            if "D" in phases: phase_D(l, cur, bcur, hA, DB["hA"])
            if "E" in phases: phase_E(l, hA, DB["hA"], hB, DB["hB"])
            if "F" in phases:
                dst, bdst = (out_d, DB["out"]) if last else (hC, DB["hC"])
                phase_F(l, hB, DB["hB"], dst, bdst)
                cur, bcur = dst, bdst
        P.barrier()

        with nc.Block() as block:
            @block.tensor
            def _(h): P.replay("tensor", h)

            @block.scalar
            def _(h): P.replay("scalar", h)

            @block.vector
            def _(h): P.replay("vector", h)

            @block.gpsimd
            def _(h): P.replay("gpsimd", h)

            @block.sync
            def _(h): P.replay("sync", h)
    return nc


def _bucket_table():
    d = np.arange(256)
    dd = np.maximum(d, 1).astype(np.float32)
    large = 16 + (np.log(dd / np.float32(16)) / np.float32(np.log(8.0)) * np.float32(16)).astype(np.int32)
    large = np.minimum(large, 31)
    return np.where(d < 16, d, large).astype(np.int64)


def make_consts():
    c = np.zeros((128, NCONST), np.float32)
    i = np.arange(128)
    c[:, C_ID:C_ID + 128] = np.eye(128, dtype=np.float32)
    c[:, C_TRI:C_TRI + 128] = (i[:, None] <= i[None, :]).astype(np.float32)
    c[:, C_LNEG:C_LNEG + 128] = (i[:, None] <= i[None, :]).astype(np.float32) * (-1.0 / 16.0)
    c[:, C_UNEG:C_UNEG + 128] = (i[:, None] > i[None, :]).astype(np.float32) * (-1.0 / 16.0)
    c[:, C_CNEG:C_CNEG + 128] = np.where(i[None, :] <= i[:, None], 0.0, -1e30).astype(np.float32)
    c[:, C_POW2:C_POW2 + 32] = (0.5 ** np.arange(1, 33)).astype(np.float32)[None, :]
    c[:, C_ONESM:C_ONESM + 128] = 1.0 / 128.0
    c[:, C_ONES:C_ONES + 128] = 1.0
    return c


def make_bias_tiles(rel_bias):
    bt = _bucket_table()
    s = np.arange(128)[:, None]; t = np.arange(128)[None, :]
    out = np.zeros((128, 2048), np.float32)
    for h in range(8):
        for o in range(2):
            d = t - s + o * 128
            idx = bt[np.clip(d, 0, 255)]
            out[:, (h * 2 + o) * 128:(h * 2 + o + 1) * 128] = rel_bias[idx, h]
    cfar = np.broadcast_to(rel_bias[31, :][None, :], (128, 8)).astype(np.float32).copy()
    return out, cfar


def make_in_maps(inputs, S, NSEQ, ncores):
    f = lambda a: np.ascontiguousarray(np.asarray(a, dtype=np.float32))
    x = f(inputs["x"]); p = f(inputs["p"])
    gains = np.stack([f(inputs[k]) for k in ("ln_mix_pre", "ln_mix_post", "ln_mlp_pre", "ln_mlp_post", "ln_ple_post")], axis=1)
    gains = np.ascontiguousarray(np.broadcast_to(gains[:, :, None, :], (DEPTH, 5, 128, DM)))
    biasT, cfar = make_bias_tiles(f(inputs["rel_bias"]))
    shared = dict(
        w_in=f(inputs["w_in"]), gw2=f(inputs["gla_gate_w2"]), ggb=f(inputs["gla_gate_b"]).reshape(DEPTH, 1, 256),
        gnorm=f(inputs["gla_norm"]).reshape(DEPTH, 128, 1), w_a=f(inputs["w_branch_a"]), w_b=f(inputs["w_branch_b"]),
        w_o=f(inputs["w_out"]), w_1=f(inputs["w_mlp_in"]), w_2=f(inputs["w_mlp_out"]), w_ple=f(inputs["w_ple"]),
        w_pg=f(inputs["w_ple_gate"]), gains=gains, consts=make_consts(), biasT=biasT, cfar=cfar)
    maps = []
    for c in range(ncores):
        m = dict(shared)
        m["x"] = np.ascontiguousarray(x[c * NSEQ:(c + 1) * NSEQ, :S].reshape(NSEQ * S, DM))
        m["p"] = np.ascontiguousarray(p[:, c * NSEQ:(c + 1) * NSEQ, :S].reshape(DEPTH, NSEQ * S, PLE))
        maps.append(m)
    return maps


_NC_CACHE = {}


def kernel(**inputs):
    B, S, _ = inputs["x"].shape
    ncores = 8; NSEQ = B // ncores
    key = (S, NSEQ)
    if key not in _NC_CACHE:
        _NC_CACHE[key] = build(dict(S=S, NSEQ=NSEQ))
    nc = _NC_CACHE[key]
    maps = make_in_maps(inputs, S, NSEQ, ncores)
    res = run_bass_kernel_spmd(nc, maps, core_ids=list(range(ncores)))
    out = np.stack([r["out"].reshape(NSEQ, S, DM) for r in res.results], axis=0).reshape(B, S, DM)
    return out.astype(np.float32)
```

```python
import numpy as np
from contextlib import ExitStack
import concourse.bass as bass
import concourse.mybir as mybir
from concourse.bass_utils import run_bass_kernel_spmd

F32 = mybir.dt.float32
BF16 = mybir.dt.bfloat16
AF = mybir.ActivationFunctionType
ALU = mybir.AluOpType
AX = mybir.AxisListType

DM = 1024; DEPTH = 2; NIN = 5720; DFF = 4096; PLE = 256
O_GQ, O_GK, O_GV, O_GG, O_GLR, O_DQ, O_DK, O_DV, O_IQ, O_IK, O_IW, O_GA, O_GB = (
    0, 256, 512, 1024, 1536, 1552, 2064, 2576, 3088, 3600, 3664, 3672, 4696)
EPS = 1e-6
NIT = 16
ARENA = 52000
NEG = -30000.0
ACT_BISECT = False
C_ID, C_TRI, C_LNEG, C_UNEG, C_CNEG, C_POW2, C_ONESM, C_ONES = 0, 128, 256, 384, 512, 640, 672, 800
NCONST = 928


class Buf:
    __slots__ = ("name", "w", "r")

    def __init__(self, name):
        self.name = name; self.w = {}; self.r = {}


class Prog:
    ENGS = ("tensor", "scalar", "vector", "gpsimd", "sync")

    def __init__(self, nc, es):
        self.nc = nc; self.es = es
        self.q = {e: [] for e in self.ENGS}
        self.cnt = {e: 0 for e in self.ENGS}
        self.seen = {e: {} for e in self.ENGS}
        self.sem = {}; self.val = {}
        for e in self.ENGS:
            self.sem["E:" + e] = es.enter_context(nc.semaphore("pe_" + e)); self.val["E:" + e] = 0
        self.nb = 0; self.free = []; self.free_sw = []; self.bufsem = {}

    def buf(self, name):
        self.nb += 1
        return Buf(f"{name}_{self.nb}")

    def _waits(self, eng, R, W, is_dma=False):
        me = "E:" + eng
        skip_me = (eng == "tensor") and not is_dma
        need = {}
        for b in R:
            for k, v in b.w.items():
                if k == me and skip_me: continue
                if v > need.get(k, 0): need[k] = v
        for b in W:
            for k, v in b.w.items():
                if k == me and skip_me: continue
                if v > need.get(k, 0): need[k] = v
            for k, v in b.r.items():
                if k == me and skip_me: continue
                if v > need.get(k, 0): need[k] = v
        out = []; seen = self.seen[eng]
        for k, v in need.items():
            if seen.get(k, 0) >= v: continue
            seen[k] = v; out.append((k, v))
        return out

    def _mark(self, k, v, R, W):
        for b in W:
            b.w[k] = v; b.r = {}
        for b in R:
            b.r[k] = v

    def op(self, eng, fn, R=(), W=()):
        waits = self._waits(eng, R, W)
        k = "E:" + eng
        self.val[k] += 1
        self.q[eng].append((waits, fn, k))
        self._mark(k, self.val[k], R, W)

    def dma(self, eng, out, in_, R=(), W=(), sb=None):
        waits = self._waits(eng, R, W, is_dma=True)
        k = self.bufsem.get(sb.name)
        if k is None:
            pool = self.free_sw if eng == "gpsimd" else self.free
            if pool:
                k = pool.pop()
            else:
                k = f"D{'s' if eng == 'gpsimd' else 'h'}:{len(self.sem)}"
                self.sem[k] = self.es.enter_context(self.nc.semaphore(f"dsem{len(self.sem)}")); self.val[k] = 0
            self.bufsem[sb.name] = k
        self.val[k] += 16
        self.q[eng].append((waits, lambda h: h.dma_start(out=out, in_=in_), k))
        self._mark(k, self.val[k], R, W)

    def barrier(self):
        for e in self.ENGS:
            waits = []; seen = self.seen[e]
            for k, v in self.val.items():
                if k == "E:" + e or v == 0 or seen.get(k, 0) >= v: continue
                seen[k] = v; waits.append((k, v))
            self.q[e].append((waits, None, None))
        for k in self.bufsem.values():
            (self.free_sw if k.startswith("Ds") else self.free).append(k)
        self.bufsem = {}

    def replay(self, eng, h):
        for waits, fn, k in self.q[eng]:
            for sk, v in waits:
                h.wait_ge(self.sem[sk], v)
            if fn is None: continue
            ins = fn(h)
            ins.then_inc(self.sem[k], 16 if k[0] == "D" else 1)


class Arena:
    def __init__(self, ap, n):
        self.ap = ap; self.n = n; self.off = 0

    def mark(self): return self.off

    def reset(self, m): self.off = m

    def f32(self, cols, parts=128):
        o = self.off; self.off += cols
        assert self.off <= self.n, f"arena overflow {self.off}"
        return self.ap[0:parts, o:o + cols]

    def bf(self, cols, parts=128):
        n = (cols + 1) // 2
        o = self.off; self.off += n
        assert self.off <= self.n, f"arena overflow {self.off}"
        return self.ap[0:parts, o:o + n].bitcast(BF16)[:, 0:cols]


class Rot:
    def __init__(self, items):
        self.items = items; self.i = 0

    def next(self):
        it = self.items[self.i % len(self.items)]; self.i += 1
        return it


def build(cfg):
    S = cfg["S"]; NSEQ = cfg["NSEQ"]; T = S * NSEQ; TOPK = min(256, S // 4); NB = S // 128
    dbg = cfg.get("dbg", ()); phases = cfg.get("phases", "ABCDEF"); layers = cfg.get("layers", DEPTH)
    nc = bass.Bass("TRN2", target_bir_lowering=False)

    def din(name, shape, dt=F32):
        return nc.dram_tensor(name, list(shape), dt, kind="ExternalInput").ap()

    DB = {}

    def dscr(name, shape, dt):
        kind = "ExternalOutput" if name in dbg else "Internal"
        ap = nc.dram_tensor(name, list(shape), dt, kind=kind).ap()
        return ap

    x_d = din("x", [T, DM]); p_d = din("p", [DEPTH, T, PLE])
    win_d = din("w_in", [DEPTH, DM, NIN]); w2_d = din("gw2", [DEPTH, 16, 256]); gb_d = din("ggb", [DEPTH, 1, 256])
    gn_d = din("gnorm", [DEPTH, 128, 1]); wa_d = din("w_a", [DEPTH, 512, DM]); wb_d = din("w_b", [DEPTH, 512, DM])
    wo_d = din("w_o", [DEPTH, DM, DM]); w1_d = din("w_1", [DEPTH, DM, DFF]); w2m_d = din("w_2", [DEPTH, DFF, DM])
    wple_d = din("w_ple", [DEPTH, PLE, DM]); wpg_d = din("w_pg", [DEPTH, DM, DM])
    gains_d = din("gains", [DEPTH, 5, 128, DM]); cst_d = din("consts", [128, NCONST])
    bias_d = din("biasT", [128, 2048]); cfar_d = din("cfar", [128, 8])
    out_d = nc.dram_tensor("out", [T, DM], F32, kind="ExternalOutput").ap()

    gqT = dscr("gqT", [256, T], F32); gkT = dscr("gkT", [256, T], F32); gk = dscr("gk", [T, 256], F32)
    gv = dscr("gv", [T, 512], BF16); sgT = dscr("sgT", [512, T], BF16); glrT = dscr("glrT", [16, T], F32)
    dqT = dscr("dqT", [512, T], BF16); dkT = dscr("dkT", [512, T], BF16); dvx = dscr("dvx", [T, 520], BF16)
    iqT = dscr("iqT", [512, T], BF16); ikT = dscr("ikT", [64, T], BF16); iw = dscr("iw", [T, 8], F32)
    sgaT = dscr("sgaT", [DM, T], BF16); sgbT = dscr("sgbT", [DM, T], BF16)
    oaT = dscr("oaT", [512, T], BF16); obT = dscr("obT", [512, T], BF16)
    hA = dscr("hA", [T, DM], F32); hB = dscr("hB", [T, DM], F32); hC = dscr("hC", [T, DM], F32)
    SCR = dict(gqT=gqT, gkT=gkT, gk=gk, gv=gv, sgT=sgT, glrT=glrT, dqT=dqT, dkT=dkT, dvx=dvx, iqT=iqT, ikT=ikT,
               iw=iw, sgaT=sgaT, sgbT=sgbT, oaT=oaT, obT=obT, hA=hA, hB=hB, hC=hC)

    with ExitStack() as es:
        arena_t = es.enter_context(nc.sbuf_tensor("arena", [128, ARENA], F32))
        A = Arena(arena_t[:, :], ARENA)
        PS = [es.enter_context(nc.psum_tensor(f"ps{i}", [128, 512], F32)) for i in range(8)]
        P = Prog(nc, es)
        for n_ in list(SCR) + ["x", "p", "out"]:
            DB[n_] = P.buf("dram_" + n_)
        PSB = [P.buf(f"psum{i}") for i in range(8)]
        psf = [PS[i][:, :] for i in range(8)]
        psb = [PS[i][:, :].bitcast(BF16) for i in range(8)]

        def mm(out, lhsT, rhs, start, stop, R, W):
            P.op("tensor", lambda h: h.matmul(out, lhsT=lhsT, rhs=rhs, start=start, stop=stop), R, W)

        def tr(out, in_, ident, R, W):
            P.op("tensor", lambda h: h.transpose(out, in_, ident), R, W)

        def act(out, in_, func, R, W, scale=None, bias=None, accum=None):
            kw = {}
            if scale is not None: kw["scale"] = scale
            if bias is not None: kw["bias"] = bias
            if accum is not None: kw["accum_out"] = accum
            P.op("scalar", lambda h: h.activation(out=out, in_=in_, func=func, **kw), R, W)

        def ts(eng, out, in0, s1, s2, op0, op1, R, W, accum=None):
            kw = {}
            if op1 is not None: kw["op1"] = op1
            if accum is not None: kw["accum_out"] = accum
            P.op(eng, lambda h: h.tensor_scalar(out=out, in0=in0, scalar1=s1, scalar2=s2, op0=op0, **kw), R, W)

        def tt(eng, out, in0, in1, op, R, W):
            P.op(eng, lambda h: h.tensor_tensor(out=out, in0=in0, in1=in1, op=op), R, W)

        def stt(out, in0, scalar, in1, op0, op1, R, W):
            P.op("vector", lambda h: h.scalar_tensor_tensor(out=out, in0=in0, scalar=scalar, in1=in1, op0=op0, op1=op1), R, W)

        def cp(eng, out, in_, R, W):
            if eng == "scalar":
                act(out, in_, AF.Copy, R, W)
            else:
                P.op(eng, lambda h: h.tensor_copy(out=out, in_=in_), R, W)

        def red(out, in_, op, R, W):
            P.op("vector", lambda h: h.tensor_reduce(out=out, in_=in_, axis=AX.X, op=op), R, W)

        def recip(out, in_, R, W):
            P.op("vector", lambda h: h.reciprocal(out=out, in_=in_), R, W)

        def mset(eng, ap, val, W):
            P.op(eng, lambda h: h.memset(ap, val), (), W)

        def dma(out, in_, R, W, sb, eng="sync"):
            P.dma(eng, out, in_, R, W, sb)

        def rot_f32(name, n, cols, parts=128):
            return Rot([(A.f32(cols, parts), P.buf(f"{name}{j}")) for j in range(n)])

        def rot_bf(name, n, cols, parts=128):
            return Rot([(A.bf(cols, parts), P.buf(f"{name}{j}")) for j in range(n)])

        cst = A.f32(NCONST); bC = P.buf("cst")
        dma(cst, cst_d[:, :], (), [bC], bC)
        cfar = A.f32(8); bCF = P.buf("cfar")
        dma(cfar, cfar_d[:, :], (), [bCF], bCF)
        ident_bf = A.bf(128); biasT_bf = A.bf(2048); tri4 = A.f32(512)
        m_tmp = A.mark()
        btmp = A.f32(2048); bBT = P.buf("btmp")
        dma(btmp, bias_d[:, :], (), [bBT], bBT)
        cp("vector", ident_bf, cst[:, C_ID:C_ID + 128], [bC], [bC])
        cp("vector", biasT_bf, btmp, [bBT], [bC])
        for i in range(4):
            cp("vector", tri4[:, i * 128:(i + 1) * 128], cst[:, C_TRI:C_TRI + 128], [bC], [bC])
        A.reset(m_tmp)
        m0 = A.mark()
        ident_f = cst[:, C_ID:C_ID + 128]

        def load_w_bf(dst3, src2, kchunks, ncols, bW):
            for kc in range(kchunks):
                for c0 in range(0, ncols, 2048):
                    c1 = min(ncols, c0 + 2048)
                    dma(dst3[:, kc, c0:c1], src2[kc * 128:(kc + 1) * 128, c0:c1], (), [bW], bW, eng="gpsimd")

        class Norm:
            def __init__(self, nslots=4, njunk=4):
                self.st = rot_f32("nst", nslots, 4)
                self.junks = rot_bf("njunk", njunk, 1024)

            def rstd(self, srcs, R):
                st, bs = self.st.next()
                self.last_st = st
                col = 0
                for s_ap in srcs:
                    n = s_ap.shape[-1]
                    jk, bj = self.junks.next()
                    act(jk[:, 0:n], s_ap, AF.Square, R, [bj, bs], accum=st[:, col:col + 1])
                    col += 1
                if len(srcs) == 2:
                    tt("vector", st[:, 0:1], st[:, 0:1], st[:, 1:2], ALU.add, [bs], [bs])
                act(st[:, 2:3], st[:, 0:1], AF.Sqrt, [bs], [bs], scale=1.0 / DM, bias=EPS)
                recip(st[:, 3:4], st[:, 2:3], [bs], [bs])
                return st[:, 3:4], bs

        def phase_A(l, h_src, b_src):
            P.barrier(); A.reset(m0)
            G = 512; NT = 4
            Wt = A.bf(8 * NIN).rearrange("p (k n) -> p k n", k=8); bW = P.buf("Win")
            load_w_bf(Wt, win_d[l], 8, NIN, bW)
            gbc = A.f32(DM); bG = P.buf("gAin")
            dma(gbc, gains_d[l, 0], (), [bG], bG)
            nrm = Norm()
            hs = Rot([(A.f32(NT * DM).rearrange("p (i d) -> p i d", i=NT), P.buf(f"hA{j}")) for j in range(2)])
            ub = rot_bf("ubf", 2, DM)
            uT = Rot([(A.bf(8 * G).rearrange("p (k t) -> p k t", k=8), P.buf(f"uT{j}")) for j in range(2)])
            stf = rot_f32("stf", 4, 512); stb = rot_bf("stb", 6, 512)
            dvs = Rot([(A.bf(520).rearrange("p (h e) -> p h e", h=8), P.buf(f"dvs{j}")) for j in range(2)])
            for ap_, b_ in dvs.items:
                mset("vector", ap_, 1.0, [b_])
            pt = Rot([(psb[0], PSB[0]), (psb[1], PSB[1])])
            pm = Rot([(psf[i], PSB[i]) for i in range(2, 8)])
            FM = []
            for i in range(2): FM.append((O_GQ + 128 * i, 128, "gqT", 128 * i, "sc_f"))
            for i in range(2): FM.append((O_GK + 128 * i, 128, "gkT", 128 * i, "cp_f"))
            for i in range(4): FM.append((O_GG + 128 * i, 128, "sgT", 128 * i, "silu"))
            FM.append((O_GLR, 16, "glrT", 0, "cp_f"))
            for i in range(4): FM.append((O_DQ + 128 * i, 128, "dqT", 128 * i, "sc_b"))
            for i in range(4): FM.append((O_DK + 128 * i, 128, "dkT", 128 * i, "cp_b"))
            for i in range(4): FM.append((O_IQ + 128 * i, 128, "iqT", 128 * i, "cp_b"))
            FM.append((O_IK, 64, "ikT", 0, "cp_b"))
            for i in range(8): FM.append((O_GA + 128 * i, 128, "sgaT", 128 * i, "sig"))
            for i in range(8): FM.append((O_GB + 128 * i, 128, "sgbT", 128 * i, "sig"))
            ngrp = T // G
            loaded = {}

            def load(g):
                hb, bh = hs.next()
                dma(hb, h_src[g * G:(g + 1) * G, :].rearrange("(i p) d -> p i d", p=128), [b_src], [bh], bh)
                loaded[g] = (hb, bh)

            load(0)
            for g in range(ngrp):
                if g + 1 < ngrp: load(g + 1)
                hb, bh = loaded.pop(g)
                uTt, buT = uT.next()
                for i in range(NT):
                    rs, brs = nrm.rstd([hb[:, i, :]], [bh])
                    rs_dbg = (nrm.last_st, brs)
                    u, bu = ub.next()
                    stt(u, hb[:, i, :], rs, gbc, ALU.mult, ALU.mult, [bh, brs, bG], [bu])
                    ptile, bpt = pt.next()
                    for kc in range(8):
                        tr(ptile[:, kc * 128:(kc + 1) * 128], u[:, kc * 128:(kc + 1) * 128], ident_bf, [bu, bC], [bpt])
                    cp("vector" if i % 2 == 0 else "scalar", uTt[:, :, i * 128:(i + 1) * 128],
                       ptile.rearrange("p (k t) -> p k t", k=8), [bpt], [buT])
                if cfg.get("dumpA") and g == 0:
                    d1 = nc.dram_tensor("dbg_st", [128, 4], F32, kind="ExternalOutput").ap()
                    d2 = nc.dram_tensor("dbg_u", [128, DM], BF16, kind="ExternalOutput").ap()
                    d3 = nc.dram_tensor("dbg_uT", [128, 8 * G], BF16, kind="ExternalOutput").ap()
                    d4 = nc.dram_tensor("dbg_W", [128, 512], BF16, kind="ExternalOutput").ap()
                    d5 = nc.dram_tensor("dbg_g", [128, DM], F32, kind="ExternalOutput").ap()
                    bdd = P.buf("dbgd")
                    dma(d1, rs_dbg[0], [rs_dbg[1]], [bdd], rs_dbg[1])
                    dma(d2, u, [bu], [bdd], bu)
                    dma(d3, uTt.rearrange("p k t -> p (k t)"), [buT], [bdd], buT)
                    dma(d4, Wt[:, 0, 0:512], [bW], [bdd], bW)
                    dma(d5, gbc, [bG], [bdd], bG)
                for (c0, m, dst, r0, kind) in FM:
                    pp, bp = pm.next()
                    for kc in range(8):
                        mm(pp[0:m, :], Wt[:, kc, c0:c0 + m], uTt[:, kc, :], kc == 0, kc == 7, [bW, buT], [bp])
                    if kind in ("sc_f", "cp_f"):
                        sg_, bs_ = stf.next()
                    else:
                        sg_, bs_ = stb.next()
                    if kind in ("sc_f", "sc_b"):
                        ts("vector", sg_[0:m, :], pp[0:m, :], 0.125, None, ALU.mult, None, [bp], [bs_])
                    elif kind in ("cp_f", "cp_b"):
                        cp("vector", sg_[0:m, :], pp[0:m, :], [bp], [bs_])
                    elif kind == "silu":
                        act(sg_[0:m, :], pp[0:m, :], AF.Silu, [bp], [bs_])
                    else:
                        act(sg_[0:m, :], pp[0:m, :], AF.Sigmoid, [bp], [bs_])
                    dma(SCR[dst][r0:r0 + m, g * G:(g + 1) * G], sg_[0:m, :], [bs_], [DB[dst]], bs_)
                for i in range(NT):
                    t0 = g * G + i * 128
                    lh = lambda kc: uTt[:, kc, i * 128:(i + 1) * 128]
                    pp, bp = pm.next()
                    for kc in range(8):
                        mm(pp[:, 0:256], lh(kc), Wt[:, kc, O_GK:O_GK + 256], kc == 0, kc == 7, [bW, buT], [bp])
                    sg_, bs_ = stf.next()
                    cp("vector", sg_[:, 0:256], pp[:, 0:256], [bp], [bs_])
                    dma(gk[t0:t0 + 128, :], sg_[:, 0:256], [bs_], [DB["gk"]], bs_)
                    pp, bp = pm.next()
                    for kc in range(8):
                        mm(pp[:, 0:8], lh(kc), Wt[:, kc, O_IW:O_IW + 8], kc == 0, kc == 7, [bW, buT], [bp])
                    sg_, bs_ = stf.next()
                    cp("vector", sg_[:, 0:8], pp[:, 0:8], [bp], [bs_])
                    dma(iw[t0:t0 + 128, :], sg_[:, 0:8], [bs_], [DB["iw"]], bs_)
                    pp, bp = pm.next()
                    for kc in range(8):
                        mm(pp, lh(kc), Wt[:, kc, O_GV:O_GV + 512], kc == 0, kc == 7, [bW, buT], [bp])
                    sg_, bs_ = stb.next()
                    cp("scalar", sg_, pp, [bp], [bs_])
                    dma(gv[t0:t0 + 128, :], sg_, [bs_], [DB["gv"]], bs_)
                    pp, bp = pm.next()
                    for kc in range(8):
                        mm(pp, lh(kc), Wt[:, kc, O_DV:O_DV + 512], kc == 0, kc == 7, [bW, buT], [bp])
                    dv_, bdv = dvs.next()
                    cp("vector", dv_[:, :, 0:64], pp.rearrange("p (h e) -> p h e", h=8), [bp], [bdv])
                    dma(dvx[t0:t0 + 128, :], dv_.rearrange("p h e -> p (h e)"), [bdv], [DB["dvx"]], bdv)

        def phase_B(l):
            P.barrier(); A.reset(m0)
            w2p = A.f32(256); gno = A.f32(1); bK = P.buf("Bconst")
            mset("vector", w2p, 0.0, [bK])
            dma(w2p[0:16, :], w2_d[l], (), [bK], bK); dma(w2p[32:33, :], gb_d[l], (), [bK], bK); dma(gno, gn_d[l], (), [bK], bK)
            Lneg = cst[:, C_LNEG:C_LNEG + 128]; Uneg = cst[:, C_UNEG:C_UNEG + 128]
            onesm = cst[:, C_ONESM:C_ONESM + 128]

            def padded(name, n, cols, dt_bf, view=None):
                items = []
                for j_ in range(n):
                    ap_ = A.bf(cols) if dt_bf else A.f32(cols)
                    b_ = P.buf(f"{name}{j_}")
                    mset("vector", ap_, 0.0, [b_])
                    items.append((ap_, b_))
                return Rot(items)

            qT2 = padded("qT2", 2, 512, False); kT2 = padded("kT2", 2, 512, False)
            kk = rot_f32("kk", 2, 256); vv = rot_bf("vv", 6, 512)
            lr = padded("lr", 2, 128, False)
            for ap_, b_ in lr.items:
                mset("vector", ap_[32:33, :], 1.0, [b_])
            sg = Rot([(A.bf(512).rearrange("p (c t) -> p c t", c=4), P.buf(f"sgB{j}")) for j in range(6)])
            ee = rot_f32("ee", 2, 256)
            ll = padded("ll", 2, 320, False)
            ebT = rot_f32("ebT", 6, 512); enbT = rot_f32("enbT", 2, 512); ecs = rot_f32("ecs", 2, 256)
            qt = padded("qt", 6, 512, True); kt = padded("kt", 6, 512, True)
            kh = padded("kh", 6, 320, True)
            att = rot_bf("att", 4, 512); sq = rot_f32("sq", 4, 512); sd = rot_f32("sd", 4, 512)
            o1 = rot_f32("o1", 4, 512)
            oas = Rot([(A.bf(512).rearrange("p (c t) -> p c t", c=4), P.buf(f"oas{j}")) for j in range(4)])
            Sst = [(A.f32(512), P.buf(f"S{s}")) for s in range(NSEQ)]
            Sbf = [padded(f"Sb{s}", 2, 512, True) for s in range(NSEQ)]
            cur_sbf = {}
            for s in range(NSEQ):
                mset("vector", Sst[s][0], 0.0, [Sst[s][1]])
                cur_sbf[s] = Sbf[s].next()
            px = (psf[0], PSB[0]); ppc = (psf[0][:, 256:512], PSB[0]); ppb = (psf[1], PSB[1])

            def stage_a(n, s):
                t0 = s * S + n * 128
                q2, bq2 = qT2.next(); k2, bk2 = kT2.next(); kk_, bkk = kk.next(); vv_, bvv = vv.next()
                lr_, blr = lr.next(); sg_, bsg = sg.next()
                dma(q2[0:64, :].rearrange("p (h t) -> p h t", h=4), gqT.rearrange("(h d) t -> d h t", d=64)[:, :, t0:t0 + 128],
                    [DB["gqT"]], [bq2], bq2)
                dma(k2[0:64, :].rearrange("p (h t) -> p h t", h=4), gkT.rearrange("(h d) t -> d h t", d=64)[:, :, t0:t0 + 128],
                    [DB["gkT"]], [bk2], bk2)
                dma(kk_, gk[t0:t0 + 128, :], [DB["gk"]], [bkk], bkk)
                dma(vv_, gv[t0:t0 + 128, :], [DB["gv"]], [bvv], bvv)
                dma(lr_[0:16, :], glrT[:, t0:t0 + 128], [DB["glrT"]], [blr], blr)
                dma(sg_, sgT.rearrange("(c p) t -> p c t", p=128)[:, :, t0:t0 + 128], [DB["sgT"]], [bsg], bsg)
                mm(px[0][:, 0:256], lr_, w2p, True, True, [blr, bK], [px[1]])
                e_, be = ee.next(); l_, bl = ll.next()
                act(e_, px[0][:, 0:256], AF.Exp, [px[1]], [be], scale=-1.0)
                act(l_[:, 0:256], e_, AF.Ln, [be], [bl], bias=1.0)
                for hh in range(4):
                    mm(ppb[0][:, hh * 128:(hh + 1) * 128], l_[:, hh * 64:hh * 64 + 128], Lneg, True, True, [bl, bC], [ppb[1]])
                mm(ppc[0][:, 0:256], Uneg, l_[:, 0:256], True, True, [bl, bC], [ppc[1]])
                eb, beb = ebT.next(); enb, benb = enbT.next(); ec, bec = ecs.next()
                act(eb[0:64, :], ppb[0][0:64, :], AF.Exp, [ppb[1]], [beb])
                act(enb[0:64, :], ppb[0][0:64, :], AF.Exp, [ppb[1]], [benb], scale=-1.0)
                act(ec, ppc[0][:, 0:256], AF.Exp, [ppc[1]], [bec])
                qt_, bqt = qt.next(); kt_, bkt = kt.next(); kh_, bkh = kh.next()
                tt("vector", qt_[0:64, :], q2[0:64, :], eb[0:64, :], ALU.mult, [bq2, beb], [bqt])
                tt("vector", kt_[0:64, :], k2[0:64, :], enb[0:64, :], ALU.mult, [bk2, benb], [bkt])
                tt("gpsimd", kh_[:, 0:256], kk_, ec, ALU.mult, [bkk, bec], [bkh])
                return (n, s, t0, qt_, bqt, kt_, bkt, kh_, bkh, vv_, bvv, sg_, bsg, eb, beb)

            def stage_b_group(ctxs):
                ns = len(ctxs)
                bank = lambda s_, r_: (psf[2 + 3 * s_ + r_], PSB[2 + 3 * s_ + r_])
                ats = []
                for (n, s, t0, qt_, bqt, kt_, bkt, kh_, bkh, vv_, bvv, sg_, bsg, eb, beb) in ctxs:
                    patt = bank(s, 0)
                    for hh in range(4):
                        mm(patt[0][:, hh * 128:(hh + 1) * 128], kt_[:, hh * 128:(hh + 1) * 128], qt_[:, hh * 128:(hh + 1) * 128],
                           True, True, [bkt, bqt], [patt[1]])
                for (n, s, t0, qt_, bqt, kt_, bkt, kh_, bkh, vv_, bvv, sg_, bsg, eb, beb) in ctxs:
                    patt = bank(s, 0)
                    at_, bat = att.next()
                    tt("vector", at_, patt[0], tri4, ALU.mult, [patt[1], bC], [bat])
                    ats.append((at_, bat))
                for ci, (n, s, t0, qt_, bqt, kt_, bkt, kh_, bkh, vv_, bvv, sg_, bsg, eb, beb) in enumerate(ctxs):
                    ppo = bank(s, 1); at_, bat = ats[ci]
                    sb_, bsb = cur_sbf[s]
                    for hh in range(4):
                        mm(ppo[0][:, hh * 128:(hh + 1) * 128], vv_[:, hh * 128:(hh + 1) * 128], at_[:, hh * 128:(hh + 1) * 128],
                           True, False, [bvv, bat], [ppo[1]])
                        mm(ppo[0][:, hh * 128:(hh + 1) * 128], sb_[:, hh * 128:(hh + 1) * 128], qt_[:, hh * 128:(hh + 1) * 128],
                           False, True, [bsb, bqt], [ppo[1]])
                for (n, s, t0, qt_, bqt, kt_, bkt, kh_, bkh, vv_, bvv, sg_, bsg, eb, beb) in ctxs:
                    ppS = bank(s, 0)
                    for hh in range(4):
                        mm(ppS[0][:, hh * 128:(hh + 1) * 128], kh_[:, hh * 64:hh * 64 + 128], vv_[:, hh * 128:(hh + 1) * 128],
                           True, True, [bkh, bvv], [ppS[1]])
                sqs = []
                for (n, s, t0, qt_, bqt, kt_, bkt, kh_, bkh, vv_, bvv, sg_, bsg, eb, beb) in ctxs:
                    ppo = bank(s, 1)
                    sq_, bsq = sq.next()
                    act(sq_, ppo[0], AF.Square, [ppo[1]], [bsq])
                    sqs.append((sq_, bsq))
                for (n, s, t0, qt_, bqt, kt_, bkt, kh_, bkh, vv_, bvv, sg_, bsg, eb, beb) in ctxs:
                    ppS = bank(s, 0)
                    S_, bS = Sst[s]
                    for hh in range(4):
                        cs_ = slice(hh * 128, (hh + 1) * 128)
                        stt(S_[0:64, cs_], S_[0:64, cs_], eb[0:64, hh * 128 + 127:hh * 128 + 128], ppS[0][0:64, cs_],
                            ALU.mult, ALU.add, [bS, beb, ppS[1]], [bS])
                    sb_, bsb = Sbf[s].next()
                    cp("gpsimd", sb_[0:64, :], S_[0:64, :], [bS], [bsb])
                    cur_sbf[s] = (sb_, bsb)
                for ci, (n, s, t0, qt_, bqt, kt_, bkt, kh_, bkh, vv_, bvv, sg_, bsg, eb, beb) in enumerate(ctxs):
                    ppm = bank(s, 2); sq_, bsq = sqs[ci]
                    mm(ppm[0], onesm, sq_, True, True, [bC, bsq], [ppm[1]])
                for (n, s, t0, qt_, bqt, kt_, bkt, kh_, bkh, vv_, bvv, sg_, bsg, eb, beb) in ctxs:
                    ppo = bank(s, 1); ppm = bank(s, 2)
                    sd_, bsd = sd.next(); o1_, bo1 = o1.next(); oa_, boa = oas.next()
                    act(sd_, ppm[0], AF.Sqrt, [ppm[1]], [bsd], bias=EPS)
                    recip(sd_, sd_, [bsd], [bsd])
                    stt(o1_, ppo[0], gno[:, 0:1], sd_, ALU.mult, ALU.mult, [ppo[1], bK, bsd], [bo1])
                    tt("gpsimd", oa_.rearrange("p c t -> p (c t)"), o1_, sg_.rearrange("p c t -> p (c t)"), ALU.mult,
                       [bo1, bsg], [boa])
                    dma(oaT.rearrange("(c p) t -> p c t", p=128)[:, :, t0:t0 + 128], oa_, [boa], [DB["oaT"]], boa)

            pend = [stage_a(0, s) for s in range(NSEQ)]
            for n in range(NB):
                nxt = [stage_a(n + 1, s) for s in range(NSEQ)] if n + 1 < NB else None
                stage_b_group(pend)
                pend = nxt

        def phase_C(l):
            P.barrier(); A.reset(m0)
            dk_s = A.bf(8 * S).rearrange("p (c t) -> p c t", c=8); bdk = P.buf("dk_s")
            dv_s = A.bf(NB * 520).rearrange("p (k e) -> p k e", k=NB); bdv = P.buf("dv_s")
            ik2 = A.bf(S); bik = P.buf("ik2")
            mset("vector", dk_s[64:128], 0.0, [bdk]); mset("vector", ik2[64:128, :], 0.0, [bik])
            acc = A.f32(S); bacc = P.buf("acc")
            junk = A.bf(S); bjunk = P.buf("junkC")
            madd = rot_bf("madd", 2, S)
            rr = rot_f32("rr", 3, 512); PT = rot_bf("PT", 3, 512)
            iqb = Rot([(A.bf(1024).rearrange("p (c t) -> p c t", c=8), P.buf(f"iqb{j}")) for j in range(2)])
            dqb = Rot([(A.bf(1024).rearrange("p (c t) -> p c t", c=8), P.buf(f"dqb{j}")) for j in range(3)])
            for ap_, b_ in iqb.items + dqb.items:
                mset("vector", ap_[64:128], 0.0, [b_])
            iwb = rot_f32("iwb", 2, 8)
            ob = rot_bf("ob", 2, 512)
            obs = Rot([(A.bf(512).rearrange("p (c t) -> p c t", c=4), P.buf(f"obs{j}")) for j in range(2)])
            stat = rot_f32("statC", 2, 8 + 2 * NIT)
            recs = rot_f32("recC", 2, 8)
            statA = rot_f32("statA", 2, 2 * NIT + 4)
            junkA = A.bf(S); bjunkA = P.buf("junkA")
            blk_count = [0]
            cneg = cst[:, C_CNEG:C_CNEG + 128]; pow2 = cst[:, C_POW2:C_POW2 + NIT]
            pi = Rot([(psf[0], PSB[0]), (psf[1], PSB[1])])
            pl = Rot([(psf[2], PSB[2]), (psf[3], PSB[3]), (psf[4], PSB[4])])
            po = [(psf[5], PSB[5]), (psf[6], PSB[6])]
            ptr = (psb[7], PSB[7])
            cur_seq = {"s1": -1, "s2": -1}

            def stage1(s, j):
                s0 = s * S
                if cur_seq["s1"] != s:
                    cur_seq["s1"] = s
                    dma(ik2[0:64, :], ikT[:, s0:s0 + S], [DB["ikT"]], [bik], bik)
                t0 = s0 + j * 128; nk = j + 1; NK = nk * 128
                iq_, biq = iqb.next(); dq_, bdq = dqb.next(); iw_, biw = iwb.next()
                dma(iq_[0:64], iqT.rearrange("(h d) t -> d h t", d=64)[:, :, t0:t0 + 128], [DB["iqT"]], [biq], biq)
                dma(dq_[0:64], dqT.rearrange("(h d) t -> d h t", d=64)[:, :, t0:t0 + 128], [DB["dqT"]], [bdq], bdq)
                dma(iw_, iw[t0:t0 + 128, :], [DB["iw"]], [biw], biw)
                for hh in range(8):
                    for c4 in range(0, NK, 512):
                        w = min(512, NK - c4)
                        pp, bp = pi.next()
                        mm(pp[:, 0:w], iq_[:, hh, :], ik2[:, c4:c4 + w], True, True, [biq, bik], [bp])
                        r_, br = rr.next()
                        act(r_[:, 0:w], pp[:, 0:w], AF.Relu, [bp], [br])
                        if hh == 0:
                            ts("vector", acc[:, c4:c4 + w], r_[:, 0:w], iw_[:, 0:1], None, ALU.mult, None, [br, biw], [bacc])
                        else:
                            stt(acc[:, c4:c4 + w], r_[:, 0:w], iw_[:, hh:hh + 1], acc[:, c4:c4 + w], ALU.mult, ALU.add,
                                [br, biw, bacc], [bacc])
                st, bst = stat.next()
                lo = st[:, 0:1]; hi = st[:, 1:2]; w0 = st[:, 2:3]; mid = st[:, 3:4]; cnt = st[:, 4:5]; step = st[:, 5:6]
                wtab = st[:, 8:8 + NIT]; twt = st[:, 8 + NIT:8 + 2 * NIT]
                red(hi, acc[:, 0:NK], ALU.max, [bacc], [bst])
                red(lo, acc[:, 0:NK], ALU.min, [bacc], [bst])
                tt("vector", w0, hi, lo, ALU.subtract, [bst], [bst])
                ts("vector", wtab, pow2, w0, None, ALU.mult, None, [bC, bst], [bst])
                tt("vector", acc[:, j * 128:(j + 1) * 128], acc[:, j * 128:(j + 1) * 128], cneg, ALU.add, [bacc, bC], [bacc])
                ma, bma = madd.next()
                rc, brc = recs.next()
                deferred = []
                use_act = ACT_BISECT and (blk_count[0] % 2 == 1) and nk >= 3
                blk_count[0] += 1
                if not use_act:
                    if NK > TOPK:
                        ts("vector", twt, wtab, 2.0, None, ALU.mult, None, [bst], [bst])
                        tt("vector", mid, lo, wtab[:, 0:1], ALU.add, [bst], [bst])
                        for k in range(NIT):
                            ts("vector", junk[:, 0:NK], acc[:, 0:NK], mid, None, ALU.is_ge, ALU.add, [bacc, bst], [bjunk, bst], accum=cnt)
                            if k < NIT - 1:
                                stt(step, cnt, TOPK - 0.5, twt[:, k + 1:k + 2], ALU.is_ge, ALU.mult, [bst], [bst])
                                stt(mid, step, wtab[:, k + 1:k + 2], mid, ALU.subtract, ALU.add, [bst], [bst])
                            else:
                                stt(step, cnt, TOPK - 0.5, wtab[:, k:k + 1], ALU.is_ge, ALU.mult, [bst], [bst])
                                stt(lo, mid, wtab[:, k:k + 1], step, ALU.subtract, ALU.add, [bst], [bst])
                    ts("vector", ma[:, 0:NK], acc[:, 0:NK], lo, NEG, ALU.is_lt, ALU.mult, [bacc, bst], [bma])
                else:
                    sa, bsa = statA.next()
                    nwt = sa[:, 0:NIT]; hwt = sa[:, NIT:2 * NIT]; nmid = sa[:, 2 * NIT:2 * NIT + 1]
                    sgs = sa[:, 2 * NIT + 1:2 * NIT + 2]; sfl = sa[:, 2 * NIT + 2:2 * NIT + 3]; stp = sa[:, 2 * NIT + 3:2 * NIT + 4]
                    ts("vector", nwt, wtab, -1.0, None, ALU.mult, None, [bst], [bsa])
                    ts("vector", hwt, wtab, 0.5, None, ALU.mult, None, [bst], [bsa])

                    def it(k, lo=lo, NK=NK, nwt=nwt, hwt=hwt, nmid=nmid, sgs=sgs, sfl=sfl, stp=stp, bst=bst, bsa=bsa):
                        act(nmid, lo, AF.Identity, [bst, bsa], [bsa], scale=-1.0, bias=nwt[:, k:k + 1])
                        act(junkA[:, 0:NK], acc[:, 0:NK], AF.Sign, [bacc, bsa], [bjunkA, bsa], bias=nmid, accum=sgs)
                        act(sfl, sgs, AF.Sign, [bsa], [bsa], bias=float(NK - 2 * TOPK + 1))
                        act(stp, sfl, AF.Identity, [bsa], [bsa], scale=hwt[:, k:k + 1], bias=hwt[:, k:k + 1])
                        act(lo, lo, AF.Identity, [bst, bsa], [bst], bias=stp)
                    for k in range(NIT):
                        deferred.append(lambda k=k: it(k))
                    deferred.append(lambda lo=lo, NK=NK, ma=ma, bma=bma, bst=bst:
                                    ts("vector", ma[:, 0:NK], acc[:, 0:NK], lo, NEG, ALU.is_lt, ALU.mult, [bacc, bst], [bma]))
                return (s, j, dq_, bdq, ma, bma, rc, brc, deferred)

            def stage2(ctx, inter):
                s, j, dq_, bdq, ma, bma, rec, bst, _d = ctx
                per_head = (len(inter) + 7) // 8
                s0 = s * S; t0 = s0 + j * 128; nk = j + 1
                if cur_seq["s2"] != s:
                    cur_seq["s2"] = s
                    dma(dk_s[0:64], dkT.rearrange("(h d) t -> d h t", d=64)[:, :, s0:s0 + S], [DB["dkT"]], [bdk], bdk)
                    dma(dv_s, dvx[s0:s0 + S, :].rearrange("(k p) e -> p k e", p=128), [DB["dvx"]], [bdv], bdv)
                for hh in range(8):
                    pob, bpo = po[hh // 4]
                    pcol = (hh % 4) * 65
                    for c4 in range(0, nk, 4):
                        kbs = list(range(c4, min(c4 + 4, nk)))
                        pp, bp = pl.next()
                        for idx, kb in enumerate(kbs):
                            sl = pp[:, idx * 128:(idx + 1) * 128]
                            near = kb >= j - 1
                            mm(sl, dk_s[:, hh, kb * 128:(kb + 1) * 128], dq_[:, hh, :], True, False, [bdk, bdq], [bp])
                            mm(sl, ma[:, kb * 128:(kb + 1) * 128], ident_bf, False, not near, [bma, bC], [bp])
                            if near:
                                bi = hh * 2 + (0 if kb == j else 1)
                                mm(sl, ident_bf, biasT_bf[:, bi * 128:(bi + 1) * 128], False, True, [bC], [bp])
                        nfar = sum(1 for kb in kbs if kb < j - 1)
                        pt_, bpt = PT.next()
                        if nfar > 0:
                            act(pt_[:, 0:nfar * 128], pp[:, 0:nfar * 128], AF.Exp, [bp, bCF], [bpt], bias=cfar[:, hh:hh + 1])
                        if nfar < len(kbs):
                            act(pt_[:, nfar * 128:len(kbs) * 128], pp[:, nfar * 128:len(kbs) * 128], AF.Exp, [bp], [bpt])
                        for idx, kb in enumerate(kbs):
                            mm(pob[:, pcol:pcol + 65], pt_[:, idx * 128:(idx + 1) * 128], dv_s[:, kb, hh * 65:(hh + 1) * 65],
                               kb == 0, kb == j, [bpt, bdv], [bpo])
                    for _ in range(per_head):
                        if inter: inter.pop(0)()
                while inter: inter.pop(0)()
                for half in range(2):
                    pob, bpo = po[half]
                    recip(rec[:, half * 4:(half + 1) * 4].rearrange("p (h o) -> p h o", o=1),
                          pob[:, 0:260].rearrange("p (h e) -> p h e", e=65)[:, :, 64:65], [bpo], [bst])
                ob_, bob = ob.next()
                for hh in range(8):
                    pob, bpo = po[hh // 4]
                    pcol = (hh % 4) * 65
                    if hh % 2 == 0:
                        ts("vector", ob_[:, hh * 64:(hh + 1) * 64], pob[:, pcol:pcol + 64], rec[:, hh:hh + 1], None, ALU.mult, None,
                           [bpo, bst], [bob])
                    else:
                        act(ob_[:, hh * 64:(hh + 1) * 64], pob[:, pcol:pcol + 64], AF.Copy, [bpo, bst], [bob], scale=rec[:, hh:hh + 1])
                for c in range(4):
                    tr(ptr[0][:, c * 128:(c + 1) * 128], ob_[:, c * 128:(c + 1) * 128], ident_bf, [bob, bC], [ptr[1]])
                os_, bos = obs.next()
                cp("scalar", os_, ptr[0][:, 0:512].rearrange("p (c t) -> p c t", c=4), [ptr[1]], [bos])
                dma(obT.rearrange("(c p) t -> p c t", p=128)[:, :, t0:t0 + 128], os_, [bos], [DB["obT"]], bos)

            blocks = [(s, j) for s in range(NSEQ) for j in range(NB)]
            pend = stage1(*blocks[0])
            while pend[-1]: pend[-1].pop(0)()
            for bi_ in range(len(blocks)):
                nxt = stage1(*blocks[bi_ + 1]) if bi_ + 1 < len(blocks) else None
                stage2(pend, nxt[-1] if nxt is not None else [])
                pend = nxt

        def phase_D(l, h_src, b_src, h_dst, b_dst):
            P.barrier(); A.reset(m0)
            G = 512; NT = 4
            Wa = A.bf(4 * DM).rearrange("p (k n) -> p k n", k=4); Wb = A.bf(4 * DM).rearrange("p (k n) -> p k n", k=4)
            Wo = A.bf(8 * DM).rearrange("p (k n) -> p k n", k=8); bW = P.buf("WD")
            load_w_bf(Wa, wa_d[l], 4, DM, bW); load_w_bf(Wb, wb_d[l], 4, DM, bW); load_w_bf(Wo, wo_d[l], 8, DM, bW)
            gbc = A.f32(DM); bG = P.buf("gD")
            dma(gbc, gains_d[l, 1], (), [bG], bG)
            nrm = Norm()
            oa = Rot([(A.bf(4 * G).rearrange("p (c t) -> p c t", c=4), P.buf(f"oaD{j}")) for j in range(2)])
            obb = Rot([(A.bf(4 * G).rearrange("p (c t) -> p c t", c=4), P.buf(f"obD{j}")) for j in range(2)])
            sa = Rot([(A.bf(8 * G).rearrange("p (c t) -> p c t", c=8), P.buf(f"saD{j}")) for j in range(2)])
            sbb = Rot([(A.bf(8 * G).rearrange("p (c t) -> p c t", c=8), P.buf(f"sbD{j}")) for j in range(2)])
            hs = Rot([(A.f32(NT * DM).rearrange("p (i d) -> p i d", i=NT), P.buf(f"hD{j}")) for j in range(3)])
            m1 = rot_f32("m1", 2, G); m2 = rot_f32("m2", 2, G)
            mxT = Rot([(A.bf(8 * G).rearrange("p (k t) -> p k t", k=8), P.buf(f"mxT{j}")) for j in range(2)])
            tmp = rot_f32("tmpD", 2, DM)
            pm = Rot([(psf[i], PSB[i]) for i in range(8)])
            ngrp = T // G
            loaded = {}

            def load(g):
                sl = slice(g * G, (g + 1) * G)
                a_, ba = oa.next(); b_, bb = obb.next(); sa_, bsa = sa.next(); sb_, bsb = sbb.next(); h_, bh = hs.next()
                dma(a_, oaT.rearrange("(c p) t -> p c t", p=128)[:, :, sl], [DB["oaT"]], [ba], ba)
                dma(b_, obT.rearrange("(c p) t -> p c t", p=128)[:, :, sl], [DB["obT"]], [bb], bb)
                dma(sa_, sgaT.rearrange("(c p) t -> p c t", p=128)[:, :, sl], [DB["sgaT"]], [bsa], bsa)
                dma(sb_, sgbT.rearrange("(c p) t -> p c t", p=128)[:, :, sl], [DB["sgbT"]], [bsb], bsb)
                dma(h_, h_src[sl, :].rearrange("(i p) d -> p i d", p=128), [b_src], [bh], bh)
                loaded[g] = (a_, ba, b_, bb, sa_, bsa, sb_, bsb, h_, bh)

            def part1(g):
                a_, ba, b_, bb, sa_, bsa, sb_, bsb, h_, bh = loaded.pop(g)
                mx, bmx = mxT.next()
                for ncb in range(8):
                    pa, bpa = pm.next(); pb, bpb = pm.next()
                    for kc in range(4):
                        mm(pa, Wa[:, kc, ncb * 128:(ncb + 1) * 128], a_[:, kc, :], kc == 0, kc == 3, [bW, ba], [bpa])
                    for kc in range(4):
                        mm(pb, Wb[:, kc, ncb * 128:(ncb + 1) * 128], b_[:, kc, :], kc == 0, kc == 3, [bW, bb], [bpb])
                    m1_, bm1 = m1.next(); m2_, bm2 = m2.next()
                    tt("vector", m1_, pa, sa_[:, ncb, :], ALU.mult, [bpa, bsa], [bm1])
                    tt("vector", m2_, pb, sb_[:, ncb, :], ALU.mult, [bpb, bsb], [bm2])
                    tt("gpsimd", mx[:, ncb, :], m1_, m2_, ALU.add, [bm1, bm2], [bmx])
                return (g, mx, bmx, h_, bh)

            def part2(ctx):
                g, mx, bmx, h_, bh = ctx
                for i in range(NT):
                    t0 = g * G + i * 128
                    halves = []
                    for half in range(2):
                        pp, bp = pm.next()
                        for kc in range(8):
                            mm(pp, mx[:, kc, i * 128:(i + 1) * 128], Wo[:, kc, half * 512:(half + 1) * 512], kc == 0, kc == 7,
                               [bmx, bW], [bp])
                        halves.append((pp, bp))
                    rs, brs = nrm.rstd([halves[0][0], halves[1][0]], [halves[0][1], halves[1][1]])
                    tm, btm = tmp.next()
                    for half in range(2):
                        stt(tm[:, half * 512:(half + 1) * 512], halves[half][0], rs, gbc[:, half * 512:(half + 1) * 512],
                            ALU.mult, ALU.mult, [halves[half][1], brs, bG], [btm])
                    tt("gpsimd", h_[:, i, :], h_[:, i, :], tm, ALU.add, [bh, btm], [bh])
                    dma(h_dst[t0:t0 + 128, :], h_[:, i, :], [bh], [b_dst], bh)

            load(0)
            if ngrp > 1: load(1)
            pend = part1(0)
            for g in range(ngrp):
                nxt = part1(g + 1) if g + 1 < ngrp else None
                if g + 2 < ngrp: load(g + 2)
                part2(pend)
                pend = nxt

        def phase_E(l, h_src, b_src, h_dst, b_dst):
            P.barrier(); A.reset(m0)
            G = 256; NT = 2
            W1 = A.bf(8 * DFF).rearrange("p (k n) -> p k n", k=8); W2 = A.bf(32 * DM).rearrange("p (k n) -> p k n", k=32)
            bW = P.buf("WE")
            load_w_bf(W1, w1_d[l], 8, DFF, bW); load_w_bf(W2, w2m_d[l], 32, DM, bW)
            g1 = A.f32(DM); g2 = A.f32(DM); bG = P.buf("gE")
            dma(g1, gains_d[l, 2], (), [bG], bG); dma(g2, gains_d[l, 3], (), [bG], bG)
            nrm = Norm(njunk=2)
            hs = Rot([(A.f32(NT * DM).rearrange("p (i d) -> p i d", i=NT), P.buf(f"hE{j}")) for j in range(2)])
            ub = rot_bf("ubE", 2, DM)
            uT = Rot([(A.bf(8 * G).rearrange("p (k t) -> p k t", k=8), P.buf(f"uTE{j}")) for j in range(2)])
            a1 = Rot([(A.bf(32 * G).rearrange("p (k t) -> p k t", k=32), P.buf(f"a1E{j}")) for j in range(1)])
            rl = rot_f32("rlE", 3, G)
            tmp = rot_f32("tmpE", 1, DM)
            pt = Rot([(psb[0], PSB[0]), (psb[1], PSB[1])])
            pm = Rot([(psf[i], PSB[i]) for i in range(2, 8)])
            ngrp = T // G
            loaded = {}

            def load(g):
                h_, bh = hs.next()
                dma(h_, h_src[g * G:(g + 1) * G, :].rearrange("(i p) d -> p i d", p=128), [b_src], [bh], bh)
                loaded[g] = (h_, bh)

            load(0)
            for g in range(ngrp):
                if g + 1 < ngrp: load(g + 1)
                h_, bh = loaded.pop(g)
                uTt, buT = uT.next()
                for i in range(NT):
                    rs, brs = nrm.rstd([h_[:, i, :]], [bh])
                    u, bu = ub.next()
                    stt(u, h_[:, i, :], rs, g1, ALU.mult, ALU.mult, [bh, brs, bG], [bu])
                    ptile, bpt = pt.next()
                    for kc in range(8):
                        tr(ptile[:, kc * 128:(kc + 1) * 128], u[:, kc * 128:(kc + 1) * 128], ident_bf, [bu, bC], [bpt])
                    cp("vector" if i % 2 == 0 else "scalar", uTt[:, :, i * 128:(i + 1) * 128],
                       ptile.rearrange("p (k t) -> p k t", k=8), [bpt], [buT])
                a1_, ba1 = a1.next()
                for fc in range(32):
                    pp, bp = pm.next()
                    for kc in range(8):
                        mm(pp[:, 0:G], W1[:, kc, fc * 128:(fc + 1) * 128], uTt[:, kc, :], kc == 0, kc == 7, [bW, buT], [bp])
                    r_, br = rl.next()
                    act(r_, pp[:, 0:G], AF.Relu, [bp], [br])
                    tt("vector" if fc % 2 == 0 else "gpsimd", a1_[:, fc, :], r_, r_, ALU.mult, [br], [ba1])
                for i in range(NT):
                    t0 = g * G + i * 128
                    halves = []
                    for half in range(2):
                        pp, bp = pm.next()
                        for fc in range(32):
                            mm(pp, a1_[:, fc, i * 128:(i + 1) * 128], W2[:, fc, half * 512:(half + 1) * 512], fc == 0, fc == 31,
                               [ba1, bW], [bp])
                        halves.append((pp, bp))
                    rs, brs = nrm.rstd([halves[0][0], halves[1][0]], [halves[0][1], halves[1][1]])
                    tm, btm = tmp.next()
                    for half in range(2):
                        stt(tm[:, half * 512:(half + 1) * 512], halves[half][0], rs, g2[:, half * 512:(half + 1) * 512],
                            ALU.mult, ALU.mult, [halves[half][1], brs, bG], [btm])
                    tt("gpsimd", h_[:, i, :], h_[:, i, :], tm, ALU.add, [bh, btm], [bh])
                    dma(h_dst[t0:t0 + 128, :], h_[:, i, :], [bh], [b_dst], bh)

        def phase_F(l, h_src, b_src, h_dst, b_dst):
            P.barrier(); A.reset(m0)
            Wp = A.bf(2 * DM).rearrange("p (k n) -> p k n", k=2); Wg = A.bf(8 * DM).rearrange("p (k n) -> p k n", k=8)
            bW = P.buf("WF")
            load_w_bf(Wp, wple_d[l], 2, DM, bW); load_w_bf(Wg, wpg_d[l], 8, DM, bW)
            g5 = A.f32(DM); bG = P.buf("gF")
            dma(g5, gains_d[l, 4], (), [bG], bG)
            nrm = Norm()
            hs = rot_f32("hF", 4, DM); ps_ = rot_f32("pF", 4, PLE)
            hb = rot_bf("hbF", 2, DM); pb = rot_bf("pbF", 2, PLE)
            hT = Rot([(A.bf(8 * 128).rearrange("p (k t) -> p k t", k=8), P.buf(f"hTF{j}")) for j in range(3)])
            pT = Rot([(A.bf(2 * 128).rearrange("p (k t) -> p k t", k=2), P.buf(f"pTF{j}")) for j in range(3)])
            sg = rot_f32("sgF", 2, DM); ee = rot_f32("eF", 2, DM); tmp = rot_f32("tmpF", 2, DM)
            pt = Rot([(psb[0], PSB[0]), (psb[1], PSB[1])])
            pm = Rot([(psf[i], PSB[i]) for i in range(2, 8)])
            ntile = T // 128
            loaded = {}

            def load(i):
                h_, bh = hs.next(); p_, bp = ps_.next()
                dma(h_, h_src[i * 128:(i + 1) * 128, :], [b_src], [bh], bh)
                dma(p_, p_d[l, i * 128:(i + 1) * 128, :], [DB["p"]], [bp], bp)
                loaded[i] = (h_, bh, p_, bp)

            def f1(i):
                h_, bh, p_, bpp = loaded.pop(i)
                hb_, bhb = hb.next(); pb_, bpb = pb.next()
                cp("vector", hb_, h_, [bh], [bhb]); cp("gpsimd", pb_, p_, [bpp], [bpb])
                ptile, bpt = pt.next()
                for kc in range(8):
                    tr(ptile[:, kc * 128:(kc + 1) * 128], hb_[:, kc * 128:(kc + 1) * 128], ident_bf, [bhb, bC], [bpt])
                hT_, bhT = hT.next()
                cp("scalar", hT_, ptile.rearrange("p (k t) -> p k t", k=8), [bpt], [bhT])
                ptile, bpt = pt.next()
                for kc in range(2):
                    tr(ptile[:, kc * 128:(kc + 1) * 128], pb_[:, kc * 128:(kc + 1) * 128], ident_bf, [bpb, bC], [bpt])
                pT_, bpT = pT.next()
                cp("vector", pT_, ptile[:, 0:256].rearrange("p (k t) -> p k t", k=2), [bpt], [bpT])
                return (i, h_, bh, hT_, bhT, pT_, bpT)

            def f2(ctx):
                i, h_, bh, hT_, bhT, pT_, bpT = ctx
                sg_, bsg = sg.next(); e_, be = ee.next()
                for half in range(2):
                    cs = slice(half * 512, (half + 1) * 512)
                    pg, bpg = pm.next()
                    for kc in range(8):
                        mm(pg, hT_[:, kc, :], Wg[:, kc, cs], kc == 0, kc == 7, [bhT, bW], [bpg])
                    act(sg_[:, cs], pg, AF.Sigmoid, [bpg], [bsg])
                    pe, bpe = pm.next()
                    for kc in range(2):
                        mm(pe, pT_[:, kc, :], Wp[:, kc, cs], kc == 0, kc == 1, [bpT, bW], [bpe])
                    tt("vector", e_[:, cs], pe, sg_[:, cs], ALU.mult, [bpe, bsg], [be])
                rs, brs = nrm.rstd([e_], [be])
                tm, btm = tmp.next()
                stt(tm, e_, rs, g5, ALU.mult, ALU.mult, [be, brs, bG], [btm])
                tt("gpsimd", h_, h_, tm, ALU.add, [bh, btm], [bh])
                dma(h_dst[i * 128:(i + 1) * 128, :], h_, [bh], [b_dst], bh)

            load(0)
            if ntile > 1: load(1)
            pend = f1(0)
            for i in range(ntile):
                nxt = f1(i + 1) if i + 1 < ntile else None
                if i + 2 < ntile: load(i + 2)
                f2(pend)
                pend = nxt

        cur, bcur = x_d, DB["x"]
        for l in range(layers):
            last = (l == layers - 1)
            if "A" in phases: phase_A(l, cur, bcur)
            if "B" in phases: phase_B(l)
            if "C" in phases: phase_C(l)
            if "D" in phases: phase_D(l, cur, bcur, hA, DB["hA"])
            if "E" in phases: phase_E(l, hA, DB["hA"], hB, DB["hB"])
            if "F" in phases:
                dst, bdst = (out_d, DB["out"]) if last else (hC, DB["hC"])
                phase_F(l, hB, DB["hB"], dst, bdst)
                cur, bcur = dst, bdst
        P.barrier()

        with nc.Block() as block:
            @block.tensor
            def _(h): P.replay("tensor", h)

            @block.scalar
            def _(h): P.replay("scalar", h)

            @block.vector
            def _(h): P.replay("vector", h)

            @block.gpsimd
            def _(h): P.replay("gpsimd", h)

            @block.sync
            def _(h): P.replay("sync", h)
    return nc


def _bucket_table():
    d = np.arange(256)
    dd = np.maximum(d, 1).astype(np.float32)
    large = 16 + (np.log(dd / np.float32(16)) / np.float32(np.log(8.0)) * np.float32(16)).astype(np.int32)
    large = np.minimum(large, 31)
    return np.where(d < 16, d, large).astype(np.int64)


def make_consts():
    c = np.zeros((128, NCONST), np.float32)
    i = np.arange(128)
    c[:, C_ID:C_ID + 128] = np.eye(128, dtype=np.float32)
    c[:, C_TRI:C_TRI + 128] = (i[:, None] <= i[None, :]).astype(np.float32)
    c[:, C_LNEG:C_LNEG + 128] = (i[:, None] <= i[None, :]).astype(np.float32) * (-1.0 / 16.0)
    c[:, C_UNEG:C_UNEG + 128] = (i[:, None] > i[None, :]).astype(np.float32) * (-1.0 / 16.0)
    c[:, C_CNEG:C_CNEG + 128] = np.where(i[None, :] <= i[:, None], 0.0, -1e30).astype(np.float32)
    c[:, C_POW2:C_POW2 + 32] = (0.5 ** np.arange(1, 33)).astype(np.float32)[None, :]
    c[:, C_ONESM:C_ONESM + 128] = 1.0 / 128.0
    c[:, C_ONES:C_ONES + 128] = 1.0
    return c


def make_bias_tiles(rel_bias):
    bt = _bucket_table()
    s = np.arange(128)[:, None]; t = np.arange(128)[None, :]
    out = np.zeros((128, 2048), np.float32)
    for h in range(8):
        for o in range(2):
            d = t - s + o * 128
            idx = bt[np.clip(d, 0, 255)]
            out[:, (h * 2 + o) * 128:(h * 2 + o + 1) * 128] = rel_bias[idx, h]
    cfar = np.broadcast_to(rel_bias[31, :][None, :], (128, 8)).astype(np.float32).copy()
    return out, cfar


def make_in_maps(inputs, S, NSEQ, ncores):
    f = lambda a: np.ascontiguousarray(np.asarray(a, dtype=np.float32))
    x = f(inputs["x"]); p = f(inputs["p"])
    gains = np.stack([f(inputs[k]) for k in ("ln_mix_pre", "ln_mix_post", "ln_mlp_pre", "ln_mlp_post", "ln_ple_post")], axis=1)
    gains = np.ascontiguousarray(np.broadcast_to(gains[:, :, None, :], (DEPTH, 5, 128, DM)))
    biasT, cfar = make_bias_tiles(f(inputs["rel_bias"]))
    shared = dict(
        w_in=f(inputs["w_in"]), gw2=f(inputs["gla_gate_w2"]), ggb=f(inputs["gla_gate_b"]).reshape(DEPTH, 1, 256),
        gnorm=f(inputs["gla_norm"]).reshape(DEPTH, 128, 1), w_a=f(inputs["w_branch_a"]), w_b=f(inputs["w_branch_b"]),
        w_o=f(inputs["w_out"]), w_1=f(inputs["w_mlp_in"]), w_2=f(inputs["w_mlp_out"]), w_ple=f(inputs["w_ple"]),
        w_pg=f(inputs["w_ple_gate"]), gains=gains, consts=make_consts(), biasT=biasT, cfar=cfar)
    maps = []
    for c in range(ncores):
        m = dict(shared)
        m["x"] = np.ascontiguousarray(x[c * NSEQ:(c + 1) * NSEQ, :S].reshape(NSEQ * S, DM))
        m["p"] = np.ascontiguousarray(p[:, c * NSEQ:(c + 1) * NSEQ, :S].reshape(DEPTH, NSEQ * S, PLE))
        maps.append(m)
    return maps


_NC_CACHE = {}


def kernel(**inputs):
    B, S, _ = inputs["x"].shape
    ncores = 8; NSEQ = B // ncores
    key = (S, NSEQ)
    if key not in _NC_CACHE:
        _NC_CACHE[key] = build(dict(S=S, NSEQ=NSEQ))
    nc = _NC_CACHE[key]
    maps = make_in_maps(inputs, S, NSEQ, ncores)
    res = run_bass_kernel_spmd(nc, maps, core_ids=list(range(ncores)))
    out = np.stack([r["out"].reshape(NSEQ, S, DM) for r in res.results], axis=0).reshape(B, S, DM)
    return out.astype(np.float32)
```

```python
import numpy as np
from contextlib import ExitStack
import concourse.bass as bass
import concourse.mybir as mybir
from concourse.bass_utils import run_bass_kernel_spmd

F32 = mybir.dt.float32
BF16 = mybir.dt.bfloat16
AF = mybir.ActivationFunctionType
ALU = mybir.AluOpType
AX = mybir.AxisListType

DM = 1024; DEPTH = 2; NIN = 5720; DFF = 4096; PLE = 256
O_GQ, O_GK, O_GV, O_GG, O_GLR, O_DQ, O_DK, O_DV, O_IQ, O_IK, O_IW, O_GA, O_GB = (
    0, 256, 512, 1024, 1536, 1552, 2064, 2576, 3088, 3600, 3664, 3672, 4696)
EPS = 1e-6
NIT = 16
ARENA = 52000
NEG = -30000.0
ACT_BISECT = False
C_ID, C_TRI, C_LNEG, C_UNEG, C_CNEG, C_POW2, C_ONESM, C_ONES = 0, 128, 256, 384, 512, 640, 672, 800
NCONST = 928


class Buf:
    __slots__ = ("name", "w", "r")

    def __init__(self, name):
        self.name = name; self.w = {}; self.r = {}


class Prog:
    ENGS = ("tensor", "scalar", "vector", "gpsimd", "sync")

    def __init__(self, nc, es):
        self.nc = nc; self.es = es
        self.q = {e: [] for e in self.ENGS}
        self.cnt = {e: 0 for e in self.ENGS}
        self.seen = {e: {} for e in self.ENGS}
        self.sem = {}; self.val = {}
        for e in self.ENGS:
            self.sem["E:" + e] = es.enter_context(nc.semaphore("pe_" + e)); self.val["E:" + e] = 0
        self.nb = 0; self.free = []; self.free_sw = []; self.bufsem = {}

    def buf(self, name):
        self.nb += 1
        return Buf(f"{name}_{self.nb}")

    def _waits(self, eng, R, W, is_dma=False):
        me = "E:" + eng
        skip_me = (eng == "tensor") and not is_dma
        need = {}
        for b in R:
            for k, v in b.w.items():
                if k == me and skip_me: continue
                if v > need.get(k, 0): need[k] = v
        for b in W:
            for k, v in b.w.items():
                if k == me and skip_me: continue
                if v > need.get(k, 0): need[k] = v
            for k, v in b.r.items():
                if k == me and skip_me: continue
                if v > need.get(k, 0): need[k] = v
        out = []; seen = self.seen[eng]
        for k, v in need.items():
            if seen.get(k, 0) >= v: continue
            seen[k] = v; out.append((k, v))
        return out

    def _mark(self, k, v, R, W):
        for b in W:
            b.w[k] = v; b.r = {}
        for b in R:
            b.r[k] = v

    def op(self, eng, fn, R=(), W=()):
        waits = self._waits(eng, R, W)
        k = "E:" + eng
        self.val[k] += 1
        self.q[eng].append((waits, fn, k))
        self._mark(k, self.val[k], R, W)

    def dma(self, eng, out, in_, R=(), W=(), sb=None):
        waits = self._waits(eng, R, W, is_dma=True)
        k = self.bufsem.get(sb.name)
        if k is None:
            pool = self.free_sw if eng == "gpsimd" else self.free
            if pool:
                k = pool.pop()
            else:
                k = f"D{'s' if eng == 'gpsimd' else 'h'}:{len(self.sem)}"
                self.sem[k] = self.es.enter_context(self.nc.semaphore(f"dsem{len(self.sem)}")); self.val[k] = 0
            self.bufsem[sb.name] = k
        self.val[k] += 16
        self.q[eng].append((waits, lambda h: h.dma_start(out=out, in_=in_), k))
        self._mark(k, self.val[k], R, W)

    def barrier(self):
        for e in self.ENGS:
            waits = []; seen = self.seen[e]
            for k, v in self.val.items():
                if k == "E:" + e or v == 0 or seen.get(k, 0) >= v: continue
                seen[k] = v; waits.append((k, v))
            self.q[e].append((waits, None, None))
        for k in self.bufsem.values():
            (self.free_sw if k.startswith("Ds") else self.free).append(k)
        self.bufsem = {}

    def replay(self, eng, h):
        for waits, fn, k in self.q[eng]:
            for sk, v in waits:
                h.wait_ge(self.sem[sk], v)
            if fn is None: continue
            ins = fn(h)
            ins.then_inc(self.sem[k], 16 if k[0] == "D" else 1)


class Arena:
    def __init__(self, ap, n):
        self.ap = ap; self.n = n; self.off = 0

    def mark(self): return self.off

    def reset(self, m): self.off = m

    def f32(self, cols, parts=128):
        o = self.off; self.off += cols
        assert self.off <= self.n, f"arena overflow {self.off}"
        return self.ap[0:parts, o:o + cols]

    def bf(self, cols, parts=128):
        n = (cols + 1) // 2
        o = self.off; self.off += n
        assert self.off <= self.n, f"arena overflow {self.off}"
        return self.ap[0:parts, o:o + n].bitcast(BF16)[:, 0:cols]


class Rot:
    def __init__(self, items):
        self.items = items; self.i = 0

    def next(self):
        it = self.items[self.i % len(self.items)]; self.i += 1
        return it


def build(cfg):
    S = cfg["S"]; NSEQ = cfg["NSEQ"]; T = S * NSEQ; TOPK = min(256, S // 4); NB = S // 128
    dbg = cfg.get("dbg", ()); phases = cfg.get("phases", "ABCDEF"); layers = cfg.get("layers", DEPTH)
    nc = bass.Bass("TRN2", target_bir_lowering=False)

    def din(name, shape, dt=F32):
        return nc.dram_tensor(name, list(shape), dt, kind="ExternalInput").ap()

    DB = {}

    def dscr(name, shape, dt):
        kind = "ExternalOutput" if name in dbg else "Internal"
        ap = nc.dram_tensor(name, list(shape), dt, kind=kind).ap()
        return ap

    x_d = din("x", [T, DM]); p_d = din("p", [DEPTH, T, PLE])
    win_d = din("w_in", [DEPTH, DM, NIN]); w2_d = din("gw2", [DEPTH, 16, 256]); gb_d = din("ggb", [DEPTH, 1, 256])
    gn_d = din("gnorm", [DEPTH, 128, 1]); wa_d = din("w_a", [DEPTH, 512, DM]); wb_d = din("w_b", [DEPTH, 512, DM])
    wo_d = din("w_o", [DEPTH, DM, DM]); w1_d = din("w_1", [DEPTH, DM, DFF]); w2m_d = din("w_2", [DEPTH, DFF, DM])
    wple_d = din("w_ple", [DEPTH, PLE, DM]); wpg_d = din("w_pg", [DEPTH, DM, DM])
    gains_d = din("gains", [DEPTH, 5, 128, DM]); cst_d = din("consts", [128, NCONST])
    bias_d = din("biasT", [128, 2048]); cfar_d = din("cfar", [128, 8])
    out_d = nc.dram_tensor("out", [T, DM], F32, kind="ExternalOutput").ap()

    gqT = dscr("gqT", [256, T], F32); gkT = dscr("gkT", [256, T], F32); gk = dscr("gk", [T, 256], F32)
    gv = dscr("gv", [T, 512], BF16); sgT = dscr("sgT", [512, T], BF16); glrT = dscr("glrT", [16, T], F32)
    dqT = dscr("dqT", [512, T], BF16); dkT = dscr("dkT", [512, T], BF16); dvx = dscr("dvx", [T, 520], BF16)
    iqT = dscr("iqT", [512, T], BF16); ikT = dscr("ikT", [64, T], BF16); iw = dscr("iw", [T, 8], F32)
    sgaT = dscr("sgaT", [DM, T], BF16); sgbT = dscr("sgbT", [DM, T], BF16)
    oaT = dscr("oaT", [512, T], BF16); obT = dscr("obT", [512, T], BF16)
    hA = dscr("hA", [T, DM], F32); hB = dscr("hB", [T, DM], F32); hC = dscr("hC", [T, DM], F32)
    SCR = dict(gqT=gqT, gkT=gkT, gk=gk, gv=gv, sgT=sgT, glrT=glrT, dqT=dqT, dkT=dkT, dvx=dvx, iqT=iqT, ikT=ikT,
               iw=iw, sgaT=sgaT, sgbT=sgbT, oaT=oaT, obT=obT, hA=hA, hB=hB, hC=hC)

    with ExitStack() as es:
        arena_t = es.enter_context(nc.sbuf_tensor("arena", [128, ARENA], F32))
        A = Arena(arena_t[:, :], ARENA)
        PS = [es.enter_context(nc.psum_tensor(f"ps{i}", [128, 512], F32)) for i in range(8)]
        P = Prog(nc, es)
        for n_ in list(SCR) + ["x", "p", "out"]:
            DB[n_] = P.buf("dram_" + n_)
        PSB = [P.buf(f"psum{i}") for i in range(8)]
        psf = [PS[i][:, :] for i in range(8)]
        psb = [PS[i][:, :].bitcast(BF16) for i in range(8)]

        def mm(out, lhsT, rhs, start, stop, R, W):
            P.op("tensor", lambda h: h.matmul(out, lhsT=lhsT, rhs=rhs, start=start, stop=stop), R, W)

        def tr(out, in_, ident, R, W):
            P.op("tensor", lambda h: h.transpose(out, in_, ident), R, W)

        def act(out, in_, func, R, W, scale=None, bias=None, accum=None):
            kw = {}
            if scale is not None: kw["scale"] = scale
            if bias is not None: kw["bias"] = bias
            if accum is not None: kw["accum_out"] = accum
            P.op("scalar", lambda h: h.activation(out=out, in_=in_, func=func, **kw), R, W)

        def ts(eng, out, in0, s1, s2, op0, op1, R, W, accum=None):
            kw = {}
            if op1 is not None: kw["op1"] = op1
            if accum is not None: kw["accum_out"] = accum
            P.op(eng, lambda h: h.tensor_scalar(out=out, in0=in0, scalar1=s1, scalar2=s2, op0=op0, **kw), R, W)

        def tt(eng, out, in0, in1, op, R, W):
            P.op(eng, lambda h: h.tensor_tensor(out=out, in0=in0, in1=in1, op=op), R, W)

        def stt(out, in0, scalar, in1, op0, op1, R, W):
            P.op("vector", lambda h: h.scalar_tensor_tensor(out=out, in0=in0, scalar=scalar, in1=in1, op0=op0, op1=op1), R, W)

        def cp(eng, out, in_, R, W):
            if eng == "scalar":
                act(out, in_, AF.Copy, R, W)
            else:
                P.op(eng, lambda h: h.tensor_copy(out=out, in_=in_), R, W)

        def red(out, in_, op, R, W):
            P.op("vector", lambda h: h.tensor_reduce(out=out, in_=in_, axis=AX.X, op=op), R, W)

        def recip(out, in_, R, W):
            P.op("vector", lambda h: h.reciprocal(out=out, in_=in_), R, W)

        def mset(eng, ap, val, W):
            P.op(eng, lambda h: h.memset(ap, val), (), W)

        def dma(out, in_, R, W, sb, eng="sync"):
            P.dma(eng, out, in_, R, W, sb)

        def rot_f32(name, n, cols, parts=128):
            return Rot([(A.f32(cols, parts), P.buf(f"{name}{j}")) for j in range(n)])

        def rot_bf(name, n, cols, parts=128):
            return Rot([(A.bf(cols, parts), P.buf(f"{name}{j}")) for j in range(n)])

        cst = A.f32(NCONST); bC = P.buf("cst")
        dma(cst, cst_d[:, :], (), [bC], bC)
        cfar = A.f32(8); bCF = P.buf("cfar")
        dma(cfar, cfar_d[:, :], (), [bCF], bCF)
        ident_bf = A.bf(128); biasT_bf = A.bf(2048); tri4 = A.f32(512)
        m_tmp = A.mark()
        btmp = A.f32(2048); bBT = P.buf("btmp")
        dma(btmp, bias_d[:, :], (), [bBT], bBT)
        cp("vector", ident_bf, cst[:, C_ID:C_ID + 128], [bC], [bC])
        cp("vector", biasT_bf, btmp, [bBT], [bC])
        for i in range(4):
            cp("vector", tri4[:, i * 128:(i + 1) * 128], cst[:, C_TRI:C_TRI + 128], [bC], [bC])
        A.reset(m_tmp)
        m0 = A.mark()
        ident_f = cst[:, C_ID:C_ID + 128]

        def load_w_bf(dst3, src2, kchunks, ncols, bW):
            for kc in range(kchunks):
                for c0 in range(0, ncols, 2048):
                    c1 = min(ncols, c0 + 2048)
                    dma(dst3[:, kc, c0:c1], src2[kc * 128:(kc + 1) * 128, c0:c1], (), [bW], bW, eng="gpsimd")

        class Norm:
            def __init__(self, nslots=4, njunk=4):
                self.st = rot_f32("nst", nslots, 4)
                self.junks = rot_bf("njunk", njunk, 1024)

            def rstd(self, srcs, R):
                st, bs = self.st.next()
                self.last_st = st
                col = 0
                for s_ap in srcs:
                    n = s_ap.shape[-1]
                    jk, bj = self.junks.next()
                    act(jk[:, 0:n], s_ap, AF.Square, R, [bj, bs], accum=st[:, col:col + 1])
                    col += 1
                if len(srcs) == 2:
                    tt("vector", st[:, 0:1], st[:, 0:1], st[:, 1:2], ALU.add, [bs], [bs])
                act(st[:, 2:3], st[:, 0:1], AF.Sqrt, [bs], [bs], scale=1.0 / DM, bias=EPS)
                recip(st[:, 3:4], st[:, 2:3], [bs], [bs])
                return st[:, 3:4], bs

        def phase_A(l, h_src, b_src):
            P.barrier(); A.reset(m0)
            G = 512; NT = 4
            Wt = A.bf(8 * NIN).rearrange("p (k n) -> p k n", k=8); bW = P.buf("Win")
            load_w_bf(Wt, win_d[l], 8, NIN, bW)
            gbc = A.f32(DM); bG = P.buf("gAin")
            dma(gbc, gains_d[l, 0], (), [bG], bG)
            nrm = Norm()
            hs = Rot([(A.f32(NT * DM).rearrange("p (i d) -> p i d", i=NT), P.buf(f"hA{j}")) for j in range(2)])
            ub = rot_bf("ubf", 2, DM)
            uT = Rot([(A.bf(8 * G).rearrange("p (k t) -> p k t", k=8), P.buf(f"uT{j}")) for j in range(2)])
            stf = rot_f32("stf", 4, 512); stb = rot_bf("stb", 6, 512)
            dvs = Rot([(A.bf(520).rearrange("p (h e) -> p h e", h=8), P.buf(f"dvs{j}")) for j in range(2)])
            for ap_, b_ in dvs.items:
                mset("vector", ap_, 1.0, [b_])
            pt = Rot([(psb[0], PSB[0]), (psb[1], PSB[1])])
            pm = Rot([(psf[i], PSB[i]) for i in range(2, 8)])
            FM = []
            for i in range(2): FM.append((O_GQ + 128 * i, 128, "gqT", 128 * i, "sc_f"))
            for i in range(2): FM.append((O_GK + 128 * i, 128, "gkT", 128 * i, "cp_f"))
            for i in range(4): FM.append((O_GG + 128 * i, 128, "sgT", 128 * i, "silu"))
            FM.append((O_GLR, 16, "glrT", 0, "cp_f"))
            for i in range(4): FM.append((O_DQ + 128 * i, 128, "dqT", 128 * i, "sc_b"))
            for i in range(4): FM.append((O_DK + 128 * i, 128, "dkT", 128 * i, "cp_b"))
            for i in range(4): FM.append((O_IQ + 128 * i, 128, "iqT", 128 * i, "cp_b"))
            FM.append((O_IK, 64, "ikT", 0, "cp_b"))
            for i in range(8): FM.append((O_GA + 128 * i, 128, "sgaT", 128 * i, "sig"))
            for i in range(8): FM.append((O_GB + 128 * i, 128, "sgbT", 128 * i, "sig"))
            ngrp = T // G
            loaded = {}

            def load(g):
                hb, bh = hs.next()
                dma(hb, h_src[g * G:(g + 1) * G, :].rearrange("(i p) d -> p i d", p=128), [b_src], [bh], bh)
                loaded[g] = (hb, bh)

            load(0)
            for g in range(ngrp):
                if g + 1 < ngrp: load(g + 1)
                hb, bh = loaded.pop(g)
                uTt, buT = uT.next()
                for i in range(NT):
                    rs, brs = nrm.rstd([hb[:, i, :]], [bh])
                    rs_dbg = (nrm.last_st, brs)
                    u, bu = ub.next()
                    stt(u, hb[:, i, :], rs, gbc, ALU.mult, ALU.mult, [bh, brs, bG], [bu])
                    ptile, bpt = pt.next()
                    for kc in range(8):
                        tr(ptile[:, kc * 128:(kc + 1) * 128], u[:, kc * 128:(kc + 1) * 128], ident_bf, [bu, bC], [bpt])
                    cp("vector" if i % 2 == 0 else "scalar", uTt[:, :, i * 128:(i + 1) * 128],
                       ptile.rearrange("p (k t) -> p k t", k=8), [bpt], [buT])
                if cfg.get("dumpA") and g == 0:
                    d1 = nc.dram_tensor("dbg_st", [128, 4], F32, kind="ExternalOutput").ap()
                    d2 = nc.dram_tensor("dbg_u", [128, DM], BF16, kind="ExternalOutput").ap()
                    d3 = nc.dram_tensor("dbg_uT", [128, 8 * G], BF16, kind="ExternalOutput").ap()
                    d4 = nc.dram_tensor("dbg_W", [128, 512], BF16, kind="ExternalOutput").ap()
                    d5 = nc.dram_tensor("dbg_g", [128, DM], F32, kind="ExternalOutput").ap()
                    bdd = P.buf("dbgd")
                    dma(d1, rs_dbg[0], [rs_dbg[1]], [bdd], rs_dbg[1])
                    dma(d2, u, [bu], [bdd], bu)
                    dma(d3, uTt.rearrange("p k t -> p (k t)"), [buT], [bdd], buT)
                    dma(d4, Wt[:, 0, 0:512], [bW], [bdd], bW)
                    dma(d5, gbc, [bG], [bdd], bG)
                for (c0, m, dst, r0, kind) in FM:
                    pp, bp = pm.next()
                    for kc in range(8):
                        mm(pp[0:m, :], Wt[:, kc, c0:c0 + m], uTt[:, kc, :], kc == 0, kc == 7, [bW, buT], [bp])
                    if kind in ("sc_f", "cp_f"):
                        sg_, bs_ = stf.next()
                    else:
                        sg_, bs_ = stb.next()
                    if kind in ("sc_f", "sc_b"):
                        ts("vector", sg_[0:m, :], pp[0:m, :], 0.125, None, ALU.mult, None, [bp], [bs_])
                    elif kind in ("cp_f", "cp_b"):
                        cp("vector", sg_[0:m, :], pp[0:m, :], [bp], [bs_])
                    elif kind == "silu":
                        act(sg_[0:m, :], pp[0:m, :], AF.Silu, [bp], [bs_])
                    else:
                        act(sg_[0:m, :], pp[0:m, :], AF.Sigmoid, [bp], [bs_])
                    dma(SCR[dst][r0:r0 + m, g * G:(g + 1) * G], sg_[0:m, :], [bs_], [DB[dst]], bs_)
                for i in range(NT):
                    t0 = g * G + i * 128
                    lh = lambda kc: uTt[:, kc, i * 128:(i + 1) * 128]
                    pp, bp = pm.next()
                    for kc in range(8):
                        mm(pp[:, 0:256], lh(kc), Wt[:, kc, O_GK:O_GK + 256], kc == 0, kc == 7, [bW, buT], [bp])
                    sg_, bs_ = stf.next()
                    cp("vector", sg_[:, 0:256], pp[:, 0:256], [bp], [bs_])
                    dma(gk[t0:t0 + 128, :], sg_[:, 0:256], [bs_], [DB["gk"]], bs_)
                    pp, bp = pm.next()
                    for kc in range(8):
                        mm(pp[:, 0:8], lh(kc), Wt[:, kc, O_IW:O_IW + 8], kc == 0, kc == 7, [bW, buT], [bp])
                    sg_, bs_ = stf.next()
                    cp("vector", sg_[:, 0:8], pp[:, 0:8], [bp], [bs_])
                    dma(iw[t0:t0 + 128, :], sg_[:, 0:8], [bs_], [DB["iw"]], bs_)
                    pp, bp = pm.next()
                    for kc in range(8):
                        mm(pp, lh(kc), Wt[:, kc, O_GV:O_GV + 512], kc == 0, kc == 7, [bW, buT], [bp])
                    sg_, bs_ = stb.next()
                    cp("scalar", sg_, pp, [bp], [bs_])
                    dma(gv[t0:t0 + 128, :], sg_, [bs_], [DB["gv"]], bs_)
                    pp, bp = pm.next()
                    for kc in range(8):
                        mm(pp, lh(kc), Wt[:, kc, O_DV:O_DV + 512], kc == 0, kc == 7, [bW, buT], [bp])
                    dv_, bdv = dvs.next()
                    cp("vector", dv_[:, :, 0:64], pp.rearrange("p (h e) -> p h e", h=8), [bp], [bdv])
                    dma(dvx[t0:t0 + 128, :], dv_.rearrange("p h e -> p (h e)"), [bdv], [DB["dvx"]], bdv)

        def phase_B(l):
            P.barrier(); A.reset(m0)
            w2p = A.f32(256); gno = A.f32(1); bK = P.buf("Bconst")
            mset("vector", w2p, 0.0, [bK])
            dma(w2p[0:16, :], w2_d[l], (), [bK], bK); dma(w2p[32:33, :], gb_d[l], (), [bK], bK); dma(gno, gn_d[l], (), [bK], bK)
            Lneg = cst[:, C_LNEG:C_LNEG + 128]; Uneg = cst[:, C_UNEG:C_UNEG + 128]
            onesm = cst[:, C_ONESM:C_ONESM + 128]

            def padded(name, n, cols, dt_bf, view=None):
                items = []
                for j_ in range(n):
                    ap_ = A.bf(cols) if dt_bf else A.f32(cols)
                    b_ = P.buf(f"{name}{j_}")
                    mset("vector", ap_, 0.0, [b_])
                    items.append((ap_, b_))
                return Rot(items)

            qT2 = padded("qT2", 2, 512, False); kT2 = padded("kT2", 2, 512, False)
            kk = rot_f32("kk", 2, 256); vv = rot_bf("vv", 6, 512)
            lr = padded("lr", 2, 128, False)
            for ap_, b_ in lr.items:
                mset("vector", ap_[32:33, :], 1.0, [b_])
            sg = Rot([(A.bf(512).rearrange("p (c t) -> p c t", c=4), P.buf(f"sgB{j}")) for j in range(6)])
            ee = rot_f32("ee", 2, 256)
            ll = padded("ll", 2, 320, False)
            ebT = rot_f32("ebT", 6, 512); enbT = rot_f32("enbT", 2, 512); ecs = rot_f32("ecs", 2, 256)
            qt = padded("qt", 6, 512, True); kt = padded("kt", 6, 512, True)
            kh = padded("kh", 6, 320, True)
            att = rot_bf("att", 4, 512); sq = rot_f32("sq", 4, 512); sd = rot_f32("sd", 4, 512)
            o1 = rot_f32("o1", 4, 512)
            oas = Rot([(A.bf(512).rearrange("p (c t) -> p c t", c=4), P.buf(f"oas{j}")) for j in range(4)])
            Sst = [(A.f32(512), P.buf(f"S{s}")) for s in range(NSEQ)]
            Sbf = [padded(f"Sb{s}", 2, 512, True) for s in range(NSEQ)]
            cur_sbf = {}
            for s in range(NSEQ):
                mset("vector", Sst[s][0], 0.0, [Sst[s][1]])
                cur_sbf[s] = Sbf[s].next()
            px = (psf[0], PSB[0]); ppc = (psf[0][:, 256:512], PSB[0]); ppb = (psf[1], PSB[1])

            def stage_a(n, s):
                t0 = s * S + n * 128
                q2, bq2 = qT2.next(); k2, bk2 = kT2.next(); kk_, bkk = kk.next(); vv_, bvv = vv.next()
                lr_, blr = lr.next(); sg_, bsg = sg.next()
                dma(q2[0:64, :].rearrange("p (h t) -> p h t", h=4), gqT.rearrange("(h d) t -> d h t", d=64)[:, :, t0:t0 + 128],
                    [DB["gqT"]], [bq2], bq2)
                dma(k2[0:64, :].rearrange("p (h t) -> p h t", h=4), gkT.rearrange("(h d) t -> d h t", d=64)[:, :, t0:t0 + 128],
                    [DB["gkT"]], [bk2], bk2)
                dma(kk_, gk[t0:t0 + 128, :], [DB["gk"]], [bkk], bkk)
                dma(vv_, gv[t0:t0 + 128, :], [DB["gv"]], [bvv], bvv)
                dma(lr_[0:16, :], glrT[:, t0:t0 + 128], [DB["glrT"]], [blr], blr)
                dma(sg_, sgT.rearrange("(c p) t -> p c t", p=128)[:, :, t0:t0 + 128], [DB["sgT"]], [bsg], bsg)
                mm(px[0][:, 0:256], lr_, w2p, True, True, [blr, bK], [px[1]])
                e_, be = ee.next(); l_, bl = ll.next()
                act(e_, px[0][:, 0:256], AF.Exp, [px[1]], [be], scale=-1.0)
                act(l_[:, 0:256], e_, AF.Ln, [be], [bl], bias=1.0)
                for hh in range(4):
                    mm(ppb[0][:, hh * 128:(hh + 1) * 128], l_[:, hh * 64:hh * 64 + 128], Lneg, True, True, [bl, bC], [ppb[1]])
                mm(ppc[0][:, 0:256], Uneg, l_[:, 0:256], True, True, [bl, bC], [ppc[1]])
                eb, beb = ebT.next(); enb, benb = enbT.next(); ec, bec = ecs.next()
                act(eb[0:64, :], ppb[0][0:64, :], AF.Exp, [ppb[1]], [beb])
                act(enb[0:64, :], ppb[0][0:64, :], AF.Exp, [ppb[1]], [benb], scale=-1.0)
                act(ec, ppc[0][:, 0:256], AF.Exp, [ppc[1]], [bec])
                qt_, bqt = qt.next(); kt_, bkt = kt.next(); kh_, bkh = kh.next()
                tt("vector", qt_[0:64, :], q2[0:64, :], eb[0:64, :], ALU.mult, [bq2, beb], [bqt])
                tt("vector", kt_[0:64, :], k2[0:64, :], enb[0:64, :], ALU.mult, [bk2, benb], [bkt])
                tt("gpsimd", kh_[:, 0:256], kk_, ec, ALU.mult, [bkk, bec], [bkh])
                return (n, s, t0, qt_, bqt, kt_, bkt, kh_, bkh, vv_, bvv, sg_, bsg, eb, beb)

            def stage_b_group(ctxs):
                ns = len(ctxs)
                bank = lambda s_, r_: (psf[2 + 3 * s_ + r_], PSB[2 + 3 * s_ + r_])
                ats = []
                for (n, s, t0, qt_, bqt, kt_, bkt, kh_, bkh, vv_, bvv, sg_, bsg, eb, beb) in ctxs:
                    patt = bank(s, 0)
                    for hh in range(4):
                        mm(patt[0][:, hh * 128:(hh + 1) * 128], kt_[:, hh * 128:(hh + 1) * 128], qt_[:, hh * 128:(hh + 1) * 128],
                           True, True, [bkt, bqt], [patt[1]])
                for (n, s, t0, qt_, bqt, kt_, bkt, kh_, bkh, vv_, bvv, sg_, bsg, eb, beb) in ctxs:
                    patt = bank(s, 0)
                    at_, bat = att.next()
                    tt("vector", at_, patt[0], tri4, ALU.mult, [patt[1], bC], [bat])
                    ats.append((at_, bat))
                for ci, (n, s, t0, qt_, bqt, kt_, bkt, kh_, bkh, vv_, bvv, sg_, bsg, eb, beb) in enumerate(ctxs):
                    ppo = bank(s, 1); at_, bat = ats[ci]
                    sb_, bsb = cur_sbf[s]
                    for hh in range(4):
                        mm(ppo[0][:, hh * 128:(hh + 1) * 128], vv_[:, hh * 128:(hh + 1) * 128], at_[:, hh * 128:(hh + 1) * 128],
                           True, False, [bvv, bat], [ppo[1]])
                        mm(ppo[0][:, hh * 128:(hh + 1) * 128], sb_[:, hh * 128:(hh + 1) * 128], qt_[:, hh * 128:(hh + 1) * 128],
                           False, True, [bsb, bqt], [ppo[1]])
                for (n, s, t0, qt_, bqt, kt_, bkt, kh_, bkh, vv_, bvv, sg_, bsg, eb, beb) in ctxs:
                    ppS = bank(s, 0)
                    for hh in range(4):
                        mm(ppS[0][:, hh * 128:(hh + 1) * 128], kh_[:, hh * 64:hh * 64 + 128], vv_[:, hh * 128:(hh + 1) * 128],
                           True, True, [bkh, bvv], [ppS[1]])
                sqs = []
                for (n, s, t0, qt_, bqt, kt_, bkt, kh_, bkh, vv_, bvv, sg_, bsg, eb, beb) in ctxs:
                    ppo = bank(s, 1)
                    sq_, bsq = sq.next()
                    act(sq_, ppo[0], AF.Square, [ppo[1]], [bsq])
                    sqs.append((sq_, bsq))
                for (n, s, t0, qt_, bqt, kt_, bkt, kh_, bkh, vv_, bvv, sg_, bsg, eb, beb) in ctxs:
                    ppS = bank(s, 0)
                    S_, bS = Sst[s]
                    for hh in range(4):
                        cs_ = slice(hh * 128, (hh + 1) * 128)
                        stt(S_[0:64, cs_], S_[0:64, cs_], eb[0:64, hh * 128 + 127:hh * 128 + 128], ppS[0][0:64, cs_],
                            ALU.mult, ALU.add, [bS, beb, ppS[1]], [bS])
                    sb_, bsb = Sbf[s].next()
                    cp("gpsimd", sb_[0:64, :], S_[0:64, :], [bS], [bsb])
                    cur_sbf[s] = (sb_, bsb)
                for ci, (n, s, t0, qt_, bqt, kt_, bkt, kh_, bkh, vv_, bvv, sg_, bsg, eb, beb) in enumerate(ctxs):
                    ppm = bank(s, 2); sq_, bsq = sqs[ci]
                    mm(ppm[0], onesm, sq_, True, True, [bC, bsq], [ppm[1]])
                for (n, s, t0, qt_, bqt, kt_, bkt, kh_, bkh, vv_, bvv, sg_, bsg, eb, beb) in ctxs:
                    ppo = bank(s, 1); ppm = bank(s, 2)
                    sd_, bsd = sd.next(); o1_, bo1 = o1.next(); oa_, boa = oas.next()
                    act(sd_, ppm[0], AF.Sqrt, [ppm[1]], [bsd], bias=EPS)
                    recip(sd_, sd_, [bsd], [bsd])
                    stt(o1_, ppo[0], gno[:, 0:1], sd_, ALU.mult, ALU.mult, [ppo[1], bK, bsd], [bo1])
                    tt("gpsimd", oa_.rearrange("p c t -> p (c t)"), o1_, sg_.rearrange("p c t -> p (c t)"), ALU.mult,
                       [bo1, bsg], [boa])
                    dma(oaT.rearrange("(c p) t -> p c t", p=128)[:, :, t0:t0 + 128], oa_, [boa], [DB["oaT"]], boa)

            pend = [stage_a(0, s) for s in range(NSEQ)]
            for n in range(NB):
                nxt = [stage_a(n + 1, s) for s in range(NSEQ)] if n + 1 < NB else None
                stage_b_group(pend)
                pend = nxt

        def phase_C(l):
            P.barrier(); A.reset(m0)
            dk_s = A.bf(8 * S).rearrange("p (c t) -> p c t", c=8); bdk = P.buf("dk_s")
            dv_s = A.bf(NB * 520).rearrange("p (k e) -> p k e", k=NB); bdv = P.buf("dv_s")
            ik2 = A.bf(S); bik = P.buf("ik2")
            mset("vector", dk_s[64:128], 0.0, [bdk]); mset("vector", ik2[64:128, :], 0.0, [bik])
            acc = A.f32(S); bacc = P.buf("acc")
            junk = A.bf(S); bjunk = P.buf("junkC")
            madd = rot_bf("madd", 2, S)
            rr = rot_f32("rr", 5, 512); PT = rot_bf("PT", 4, 512)
            iqb = Rot([(A.bf(1024).rearrange("p (c t) -> p c t", c=8), P.buf(f"iqb{j}")) for j in range(2)])
            dqb = Rot([(A.bf(1024).rearrange("p (c t) -> p c t", c=8), P.buf(f"dqb{j}")) for j in range(3)])
            for ap_, b_ in iqb.items + dqb.items:
                mset("vector", ap_[64:128], 0.0, [b_])
            iwb = rot_f32("iwb", 2, 8)
            ob = rot_bf("ob", 2, 512)
            obs = Rot([(A.bf(512).rearrange("p (c t) -> p c t", c=4), P.buf(f"obs{j}")) for j in range(2)])
            stat = rot_f32("statC", 2, 8 + 2 * NIT)
            recs = rot_f32("recC", 2, 8)
            statA = rot_f32("statA", 2, 2 * NIT + 4)
            junkA = A.bf(S); bjunkA = P.buf("junkA")
            blk_count = [0]
            cneg = cst[:, C_CNEG:C_CNEG + 128]; pow2 = cst[:, C_POW2:C_POW2 + NIT]
            pi = Rot([(psf[0], PSB[0]), (psf[1], PSB[1])])
            pl = Rot([(psf[2], PSB[2]), (psf[3], PSB[3]), (psf[4], PSB[4])])
            po = [(psf[5], PSB[5]), (psf[6], PSB[6])]
            ptr = (psb[7], PSB[7])
            cur_seq = {"s1": -1, "s2": -1}

            def stage1(s, j):
                s0 = s * S
                if cur_seq["s1"] != s:
                    cur_seq["s1"] = s
                    dma(ik2[0:64, :], ikT[:, s0:s0 + S], [DB["ikT"]], [bik], bik)
                t0 = s0 + j * 128; nk = j + 1; NK = nk * 128
                iq_, biq = iqb.next(); dq_, bdq = dqb.next(); iw_, biw = iwb.next()
                dma(iq_[0:64], iqT.rearrange("(h d) t -> d h t", d=64)[:, :, t0:t0 + 128], [DB["iqT"]], [biq], biq)
                dma(dq_[0:64], dqT.rearrange("(h d) t -> d h t", d=64)[:, :, t0:t0 + 128], [DB["dqT"]], [bdq], bdq)
                dma(iw_, iw[t0:t0 + 128, :], [DB["iw"]], [biw], biw)
                for hh in range(8):
                    for c4 in range(0, NK, 512):
                        w = min(512, NK - c4)
                        pp, bp = pi.next()
                        mm(pp[:, 0:w], iq_[:, hh, :], ik2[:, c4:c4 + w], True, True, [biq, bik], [bp])
                        r_, br = rr.next()
                        act(r_[:, 0:w], pp[:, 0:w], AF.Relu, [bp], [br])
                        if hh == 0:
                            ts("vector", acc[:, c4:c4 + w], r_[:, 0:w], iw_[:, 0:1], None, ALU.mult, None, [br, biw], [bacc])
                        else:
                            stt(acc[:, c4:c4 + w], r_[:, 0:w], iw_[:, hh:hh + 1], acc[:, c4:c4 + w], ALU.mult, ALU.add,
                                [br, biw, bacc], [bacc])
                st, bst = stat.next()
                lo = st[:, 0:1]; hi = st[:, 1:2]; w0 = st[:, 2:3]; mid = st[:, 3:4]; cnt = st[:, 4:5]; step = st[:, 5:6]
                wtab = st[:, 8:8 + NIT]; twt = st[:, 8 + NIT:8 + 2 * NIT]
                red(hi, acc[:, 0:NK], ALU.max, [bacc], [bst])
                red(lo, acc[:, 0:NK], ALU.min, [bacc], [bst])
                tt("vector", w0, hi, lo, ALU.subtract, [bst], [bst])
                ts("vector", wtab, pow2, w0, None, ALU.mult, None, [bC, bst], [bst])
                tt("vector", acc[:, j * 128:(j + 1) * 128], acc[:, j * 128:(j + 1) * 128], cneg, ALU.add, [bacc, bC], [bacc])
                ma, bma = madd.next()
                rc, brc = recs.next()
                deferred = []
                use_act = ACT_BISECT and (blk_count[0] % 2 == 1) and nk >= 3
                blk_count[0] += 1
                if not use_act:
                    if NK > TOPK:
                        ts("vector", twt, wtab, 2.0, None, ALU.mult, None, [bst], [bst])
                        tt("vector", mid, lo, wtab[:, 0:1], ALU.add, [bst], [bst])
                        for k in range(NIT):
                            ts("vector", junk[:, 0:NK], acc[:, 0:NK], mid, None, ALU.is_ge, ALU.add, [bacc, bst], [bjunk, bst], accum=cnt)
                            if k < NIT - 1:
                                stt(step, cnt, TOPK - 0.5, twt[:, k + 1:k + 2], ALU.is_ge, ALU.mult, [bst], [bst])
                                stt(mid, step, wtab[:, k + 1:k + 2], mid, ALU.subtract, ALU.add, [bst], [bst])
                            else:
                                stt(step, cnt, TOPK - 0.5, wtab[:, k:k + 1], ALU.is_ge, ALU.mult, [bst], [bst])
                                stt(lo, mid, wtab[:, k:k + 1], step, ALU.subtract, ALU.add, [bst], [bst])
                    ts("vector", ma[:, 0:NK], acc[:, 0:NK], lo, NEG, ALU.is_lt, ALU.mult, [bacc, bst], [bma])
                else:
                    sa, bsa = statA.next()
                    nwt = sa[:, 0:NIT]; hwt = sa[:, NIT:2 * NIT]; nmid = sa[:, 2 * NIT:2 * NIT + 1]
                    sgs = sa[:, 2 * NIT + 1:2 * NIT + 2]; sfl = sa[:, 2 * NIT + 2:2 * NIT + 3]; stp = sa[:, 2 * NIT + 3:2 * NIT + 4]
                    ts("vector", nwt, wtab, -1.0, None, ALU.mult, None, [bst], [bsa])
                    ts("vector", hwt, wtab, 0.5, None, ALU.mult, None, [bst], [bsa])

                    def it(k, lo=lo, NK=NK, nwt=nwt, hwt=hwt, nmid=nmid, sgs=sgs, sfl=sfl, stp=stp, bst=bst, bsa=bsa):
                        act(nmid, lo, AF.Identity, [bst, bsa], [bsa], scale=-1.0, bias=nwt[:, k:k + 1])
                        act(junkA[:, 0:NK], acc[:, 0:NK], AF.Sign, [bacc, bsa], [bjunkA, bsa], bias=nmid, accum=sgs)
                        act(sfl, sgs, AF.Sign, [bsa], [bsa], bias=float(NK - 2 * TOPK + 1))
                        act(stp, sfl, AF.Identity, [bsa], [bsa], scale=hwt[:, k:k + 1], bias=hwt[:, k:k + 1])
                        act(lo, lo, AF.Identity, [bst, bsa], [bst], bias=stp)
                    for k in range(NIT):
                        deferred.append(lambda k=k: it(k))
                    deferred.append(lambda lo=lo, NK=NK, ma=ma, bma=bma, bst=bst:
                                    ts("vector", ma[:, 0:NK], acc[:, 0:NK], lo, NEG, ALU.is_lt, ALU.mult, [bacc, bst], [bma]))
                return (s, j, dq_, bdq, ma, bma, rc, brc, deferred)

            def stage2(ctx, inter):
                s, j, dq_, bdq, ma, bma, rec, bst, _d = ctx
                per_head = (len(inter) + 7) // 8
                s0 = s * S; t0 = s0 + j * 128; nk = j + 1
                if cur_seq["s2"] != s:
                    cur_seq["s2"] = s
                    dma(dk_s[0:64], dkT.rearrange("(h d) t -> d h t", d=64)[:, :, s0:s0 + S], [DB["dkT"]], [bdk], bdk)
                    dma(dv_s, dvx[s0:s0 + S, :].rearrange("(k p) e -> p k e", p=128), [DB["dvx"]], [bdv], bdv)
                for hh in range(8):
                    pob, bpo = po[hh // 4]
                    pcol = (hh % 4) * 65
                    for c4 in range(0, nk, 4):
                        kbs = list(range(c4, min(c4 + 4, nk)))
                        pp, bp = pl.next()
                        for idx, kb in enumerate(kbs):
                            sl = pp[:, idx * 128:(idx + 1) * 128]
                            near = kb >= j - 1
                            mm(sl, dk_s[:, hh, kb * 128:(kb + 1) * 128], dq_[:, hh, :], True, False, [bdk, bdq], [bp])
                            mm(sl, ma[:, kb * 128:(kb + 1) * 128], ident_bf, False, not near, [bma, bC], [bp])
                            if near:
                                bi = hh * 2 + (0 if kb == j else 1)
                                mm(sl, ident_bf, biasT_bf[:, bi * 128:(bi + 1) * 128], False, True, [bC], [bp])
                        nfar = sum(1 for kb in kbs if kb < j - 1)
                        pt_, bpt = PT.next()
                        if nfar > 0:
                            act(pt_[:, 0:nfar * 128], pp[:, 0:nfar * 128], AF.Exp, [bp, bCF], [bpt], bias=cfar[:, hh:hh + 1])
                        if nfar < len(kbs):
                            act(pt_[:, nfar * 128:len(kbs) * 128], pp[:, nfar * 128:len(kbs) * 128], AF.Exp, [bp], [bpt])
                        for idx, kb in enumerate(kbs):
                            mm(pob[:, pcol:pcol + 65], pt_[:, idx * 128:(idx + 1) * 128], dv_s[:, kb, hh * 65:(hh + 1) * 65],
                               kb == 0, kb == j, [bpt, bdv], [bpo])
                    for _ in range(per_head):
                        if inter: inter.pop(0)()
                while inter: inter.pop(0)()
                for half in range(2):
                    pob, bpo = po[half]
                    recip(rec[:, half * 4:(half + 1) * 4].rearrange("p (h o) -> p h o", o=1),
                          pob[:, 0:260].rearrange("p (h e) -> p h e", e=65)[:, :, 64:65], [bpo], [bst])
                ob_, bob = ob.next()
                for hh in range(8):
                    pob, bpo = po[hh // 4]
                    pcol = (hh % 4) * 65
                    if hh % 2 == 0:
                        ts("vector", ob_[:, hh * 64:(hh + 1) * 64], pob[:, pcol:pcol + 64], rec[:, hh:hh + 1], None, ALU.mult, None,
                           [bpo, bst], [bob])
                    else:
                        act(ob_[:, hh * 64:(hh + 1) * 64], pob[:, pcol:pcol + 64], AF.Copy, [bpo, bst], [bob], scale=rec[:, hh:hh + 1])
                for c in range(4):
                    tr(ptr[0][:, c * 128:(c + 1) * 128], ob_[:, c * 128:(c + 1) * 128], ident_bf, [bob, bC], [ptr[1]])
                os_, bos = obs.next()
                cp("scalar", os_, ptr[0][:, 0:512].rearrange("p (c t) -> p c t", c=4), [ptr[1]], [bos])
                dma(obT.rearrange("(c p) t -> p c t", p=128)[:, :, t0:t0 + 128], os_, [bos], [DB["obT"]], bos)

            blocks = [(s, j) for s in range(NSEQ) for j in range(NB)]
            pend = stage1(*blocks[0])
            while pend[-1]: pend[-1].pop(0)()
            for bi_ in range(len(blocks)):
                nxt = stage1(*blocks[bi_ + 1]) if bi_ + 1 < len(blocks) else None
                stage2(pend, nxt[-1] if nxt is not None else [])
                pend = nxt

        def phase_D(l, h_src, b_src, h_dst, b_dst):
            P.barrier(); A.reset(m0)
            G = 512; NT = 4
            Wa = A.bf(4 * DM).rearrange("p (k n) -> p k n", k=4); Wb = A.bf(4 * DM).rearrange("p (k n) -> p k n", k=4)
            Wo = A.bf(8 * DM).rearrange("p (k n) -> p k n", k=8); bW = P.buf("WD")
            load_w_bf(Wa, wa_d[l], 4, DM, bW); load_w_bf(Wb, wb_d[l], 4, DM, bW); load_w_bf(Wo, wo_d[l], 8, DM, bW)
            gbc = A.f32(DM); bG = P.buf("gD")
            dma(gbc, gains_d[l, 1], (), [bG], bG)
            nrm = Norm()
            oa = Rot([(A.bf(4 * G).rearrange("p (c t) -> p c t", c=4), P.buf(f"oaD{j}")) for j in range(2)])
            obb = Rot([(A.bf(4 * G).rearrange("p (c t) -> p c t", c=4), P.buf(f"obD{j}")) for j in range(2)])
            sa = Rot([(A.bf(8 * G).rearrange("p (c t) -> p c t", c=8), P.buf(f"saD{j}")) for j in range(2)])
            sbb = Rot([(A.bf(8 * G).rearrange("p (c t) -> p c t", c=8), P.buf(f"sbD{j}")) for j in range(2)])
            hs = Rot([(A.f32(NT * DM).rearrange("p (i d) -> p i d", i=NT), P.buf(f"hD{j}")) for j in range(3)])
            m1 = rot_f32("m1", 2, G); m2 = rot_f32("m2", 2, G)
            mxT = Rot([(A.bf(8 * G).rearrange("p (k t) -> p k t", k=8), P.buf(f"mxT{j}")) for j in range(2)])
            tmp = rot_f32("tmpD", 2, DM)
            pm = Rot([(psf[i], PSB[i]) for i in range(8)])
            ngrp = T // G
            loaded = {}

            def load(g):
                sl = slice(g * G, (g + 1) * G)
                a_, ba = oa.next(); b_, bb = obb.next(); sa_, bsa = sa.next(); sb_, bsb = sbb.next(); h_, bh = hs.next()
                dma(a_, oaT.rearrange("(c p) t -> p c t", p=128)[:, :, sl], [DB["oaT"]], [ba], ba)
                dma(b_, obT.rearrange("(c p) t -> p c t", p=128)[:, :, sl], [DB["obT"]], [bb], bb)
                dma(sa_, sgaT.rearrange("(c p) t -> p c t", p=128)[:, :, sl], [DB["sgaT"]], [bsa], bsa)
                dma(sb_, sgbT.rearrange("(c p) t -> p c t", p=128)[:, :, sl], [DB["sgbT"]], [bsb], bsb)
                dma(h_, h_src[sl, :].rearrange("(i p) d -> p i d", p=128), [b_src], [bh], bh)
                loaded[g] = (a_, ba, b_, bb, sa_, bsa, sb_, bsb, h_, bh)

            def part1(g):
                a_, ba, b_, bb, sa_, bsa, sb_, bsb, h_, bh = loaded.pop(g)
                mx, bmx = mxT.next()
                for ncb in range(8):
                    pa, bpa = pm.next(); pb, bpb = pm.next()
                    for kc in range(4):
                        mm(pa, Wa[:, kc, ncb * 128:(ncb + 1) * 128], a_[:, kc, :], kc == 0, kc == 3, [bW, ba], [bpa])
                    for kc in range(4):
                        mm(pb, Wb[:, kc, ncb * 128:(ncb + 1) * 128], b_[:, kc, :], kc == 0, kc == 3, [bW, bb], [bpb])
                    m1_, bm1 = m1.next(); m2_, bm2 = m2.next()
                    tt("vector", m1_, pa, sa_[:, ncb, :], ALU.mult, [bpa, bsa], [bm1])
                    tt("vector", m2_, pb, sb_[:, ncb, :], ALU.mult, [bpb, bsb], [bm2])
                    tt("gpsimd", mx[:, ncb, :], m1_, m2_, ALU.add, [bm1, bm2], [bmx])
                return (g, mx, bmx, h_, bh)

            def part2(ctx):
                g, mx, bmx, h_, bh = ctx
                for i in range(NT):
                    t0 = g * G + i * 128
                    halves = []
                    for half in range(2):
                        pp, bp = pm.next()
                        for kc in range(8):
                            mm(pp, mx[:, kc, i * 128:(i + 1) * 128], Wo[:, kc, half * 512:(half + 1) * 512], kc == 0, kc == 7,
                               [bmx, bW], [bp])
                        halves.append((pp, bp))
                    rs, brs = nrm.rstd([halves[0][0], halves[1][0]], [halves[0][1], halves[1][1]])
                    tm, btm = tmp.next()
                    for half in range(2):
                        stt(tm[:, half * 512:(half + 1) * 512], halves[half][0], rs, gbc[:, half * 512:(half + 1) * 512],
                            ALU.mult, ALU.mult, [halves[half][1], brs, bG], [btm])
                    tt("gpsimd", h_[:, i, :], h_[:, i, :], tm, ALU.add, [bh, btm], [bh])
                    dma(h_dst[t0:t0 + 128, :], h_[:, i, :], [bh], [b_dst], bh)

            load(0)
            if ngrp > 1: load(1)
            pend = part1(0)
            for g in range(ngrp):
                nxt = part1(g + 1) if g + 1 < ngrp else None
                if g + 2 < ngrp: load(g + 2)
                part2(pend)
                pend = nxt

        def phase_E(l, h_src, b_src, h_dst, b_dst):
            P.barrier(); A.reset(m0)
            G = 256; NT = 2
            W1 = A.bf(8 * DFF).rearrange("p (k n) -> p k n", k=8); W2 = A.bf(32 * DM).rearrange("p (k n) -> p k n", k=32)
            bW = P.buf("WE")
            load_w_bf(W1, w1_d[l], 8, DFF, bW); load_w_bf(W2, w2m_d[l], 32, DM, bW)
            g1 = A.f32(DM); g2 = A.f32(DM); bG = P.buf("gE")
            dma(g1, gains_d[l, 2], (), [bG], bG); dma(g2, gains_d[l, 3], (), [bG], bG)
            nrm = Norm(njunk=2)
            hs = Rot([(A.f32(NT * DM).rearrange("p (i d) -> p i d", i=NT), P.buf(f"hE{j}")) for j in range(2)])
            ub = rot_bf("ubE", 2, DM)
            uT = Rot([(A.bf(8 * G).rearrange("p (k t) -> p k t", k=8), P.buf(f"uTE{j}")) for j in range(2)])
            a1 = Rot([(A.bf(32 * G).rearrange("p (k t) -> p k t", k=32), P.buf(f"a1E{j}")) for j in range(1)])
            rl = rot_f32("rlE", 3, G)
            tmp = rot_f32("tmpE", 1, DM)
            pt = Rot([(psb[0], PSB[0]), (psb[1], PSB[1])])
            pm = Rot([(psf[i], PSB[i]) for i in range(2, 8)])
            ngrp = T // G
            loaded = {}

            def load(g):
                h_, bh = hs.next()
                dma(h_, h_src[g * G:(g + 1) * G, :].rearrange("(i p) d -> p i d", p=128), [b_src], [bh], bh)
                loaded[g] = (h_, bh)

            load(0)
            for g in range(ngrp):
                if g + 1 < ngrp: load(g + 1)
                h_, bh = loaded.pop(g)
                uTt, buT = uT.next()
                for i in range(NT):
                    rs, brs = nrm.rstd([h_[:, i, :]], [bh])
                    u, bu = ub.next()
                    stt(u, h_[:, i, :], rs, g1, ALU.mult, ALU.mult, [bh, brs, bG], [bu])
                    ptile, bpt = pt.next()
                    for kc in range(8):
                        tr(ptile[:, kc * 128:(kc + 1) * 128], u[:, kc * 128:(kc + 1) * 128], ident_bf, [bu, bC], [bpt])
                    cp("vector" if i % 2 == 0 else "scalar", uTt[:, :, i * 128:(i + 1) * 128],
                       ptile.rearrange("p (k t) -> p k t", k=8), [bpt], [buT])
                a1_, ba1 = a1.next()
                for fc in range(32):
                    pp, bp = pm.next()
                    for kc in range(8):
                        mm(pp[:, 0:G], W1[:, kc, fc * 128:(fc + 1) * 128], uTt[:, kc, :], kc == 0, kc == 7, [bW, buT], [bp])
                    r_, br = rl.next()
                    act(r_, pp[:, 0:G], AF.Relu, [bp], [br])
                    tt("vector" if fc % 2 == 0 else "gpsimd", a1_[:, fc, :], r_, r_, ALU.mult, [br], [ba1])
                for i in range(NT):
                    t0 = g * G + i * 128
                    halves = []
                    for half in range(2):
                        pp, bp = pm.next()
                        for fc in range(32):
                            mm(pp, a1_[:, fc, i * 128:(i + 1) * 128], W2[:, fc, half * 512:(half + 1) * 512], fc == 0, fc == 31,
                               [ba1, bW], [bp])
                        halves.append((pp, bp))
                    rs, brs = nrm.rstd([halves[0][0], halves[1][0]], [halves[0][1], halves[1][1]])
                    tm, btm = tmp.next()
                    for half in range(2):
                        stt(tm[:, half * 512:(half + 1) * 512], halves[half][0], rs, g2[:, half * 512:(half + 1) * 512],
                            ALU.mult, ALU.mult, [halves[half][1], brs, bG], [btm])
                    tt("gpsimd", h_[:, i, :], h_[:, i, :], tm, ALU.add, [bh, btm], [bh])
                    dma(h_dst[t0:t0 + 128, :], h_[:, i, :], [bh], [b_dst], bh)

        def phase_F(l, h_src, b_src, h_dst, b_dst):
            P.barrier(); A.reset(m0)
            Wp = A.bf(2 * DM).rearrange("p (k n) -> p k n", k=2); Wg = A.bf(8 * DM).rearrange("p (k n) -> p k n", k=8)
            bW = P.buf("WF")
            load_w_bf(Wp, wple_d[l], 2, DM, bW); load_w_bf(Wg, wpg_d[l], 8, DM, bW)
            g5 = A.f32(DM); bG = P.buf("gF")
            dma(g5, gains_d[l, 4], (), [bG], bG)
            nrm = Norm()
            hs = rot_f32("hF", 4, DM); ps_ = rot_f32("pF", 4, PLE)
            hb = rot_bf("hbF", 2, DM); pb = rot_bf("pbF", 2, PLE)
            hT = Rot([(A.bf(8 * 128).rearrange("p (k t) -> p k t", k=8), P.buf(f"hTF{j}")) for j in range(3)])
            pT = Rot([(A.bf(2 * 128).rearrange("p (k t) -> p k t", k=2), P.buf(f"pTF{j}")) for j in range(3)])
            sg = rot_f32("sgF", 2, DM); ee = rot_f32("eF", 2, DM); tmp = rot_f32("tmpF", 2, DM)
            pt = Rot([(psb[0], PSB[0]), (psb[1], PSB[1])])
            pm = Rot([(psf[i], PSB[i]) for i in range(2, 8)])
            ntile = T // 128
            loaded = {}

            def load(i):
                h_, bh = hs.next(); p_, bp = ps_.next()
                dma(h_, h_src[i * 128:(i + 1) * 128, :], [b_src], [bh], bh)
                dma(p_, p_d[l, i * 128:(i + 1) * 128, :], [DB["p"]], [bp], bp)
                loaded[i] = (h_, bh, p_, bp)

            def f1(i):
                h_, bh, p_, bpp = loaded.pop(i)
                hb_, bhb = hb.next(); pb_, bpb = pb.next()
                cp("vector", hb_, h_, [bh], [bhb]); cp("gpsimd", pb_, p_, [bpp], [bpb])
                ptile, bpt = pt.next()
                for kc in range(8):
                    tr(ptile[:, kc * 128:(kc + 1) * 128], hb_[:, kc * 128:(kc + 1) * 128], ident_bf, [bhb, bC], [bpt])
                hT_, bhT = hT.next()
                cp("scalar", hT_, ptile.rearrange("p (k t) -> p k t", k=8), [bpt], [bhT])
                ptile, bpt = pt.next()
                for kc in range(2):
                    tr(ptile[:, kc * 128:(kc + 1) * 128], pb_[:, kc * 128:(kc + 1) * 128], ident_bf, [bpb, bC], [bpt])
                pT_, bpT = pT.next()
                cp("vector", pT_, ptile[:, 0:256].rearrange("p (k t) -> p k t", k=2), [bpt], [bpT])
                return (i, h_, bh, hT_, bhT, pT_, bpT)

            def f2(ctx):
                i, h_, bh, hT_, bhT, pT_, bpT = ctx
                sg_, bsg = sg.next(); e_, be = ee.next()
                for half in range(2):
                    cs = slice(half * 512, (half + 1) * 512)
                    pg, bpg = pm.next()
                    for kc in range(8):
                        mm(pg, hT_[:, kc, :], Wg[:, kc, cs], kc == 0, kc == 7, [bhT, bW], [bpg])
                    act(sg_[:, cs], pg, AF.Sigmoid, [bpg], [bsg])
                    pe, bpe = pm.next()
                    for kc in range(2):
                        mm(pe, pT_[:, kc, :], Wp[:, kc, cs], kc == 0, kc == 1, [bpT, bW], [bpe])
                    tt("vector", e_[:, cs], pe, sg_[:, cs], ALU.mult, [bpe, bsg], [be])
                rs, brs = nrm.rstd([e_], [be])
                tm, btm = tmp.next()
                stt(tm, e_, rs, g5, ALU.mult, ALU.mult, [be, brs, bG], [btm])
                tt("gpsimd", h_, h_, tm, ALU.add, [bh, btm], [bh])
                dma(h_dst[i * 128:(i + 1) * 128, :], h_, [bh], [b_dst], bh)

            load(0)
            if ntile > 1: load(1)
            pend = f1(0)
            for i in range(ntile):
                nxt = f1(i + 1) if i + 1 < ntile else None
                if i + 2 < ntile: load(i + 2)
                f2(pend)
                pend = nxt

        cur, bcur = x_d, DB["x"]
        for l in range(layers):
            last = (l == layers - 1)
            if "A" in phases: phase_A(l, cur, bcur)
            if "B" in phases: phase_B(l)
            if "C" in phases: phase_C(l)
            if "D" in phases: phase_D(l, cur, bcur, hA, DB["hA"])
            if "E" in phases: phase_E(l, hA, DB["hA"], hB, DB["hB"])
            if "F" in phases:
                dst, bdst = (out_d, DB["out"]) if last else (hC, DB["hC"])
                phase_F(l, hB, DB["hB"], dst, bdst)
                cur, bcur = dst, bdst
        P.barrier()

        with nc.Block() as block:
            @block.tensor
            def _(h): P.replay("tensor", h)

            @block.scalar
            def _(h): P.replay("scalar", h)

            @block.vector
            def _(h): P.replay("vector", h)

            @block.gpsimd
            def _(h): P.replay("gpsimd", h)

            @block.sync
            def _(h): P.replay("sync", h)
    return nc


def _bucket_table():
    d = np.arange(256)
    dd = np.maximum(d, 1).astype(np.float32)
    large = 16 + (np.log(dd / np.float32(16)) / np.float32(np.log(8.0)) * np.float32(16)).astype(np.int32)
    large = np.minimum(large, 31)
    return np.where(d < 16, d, large).astype(np.int64)


def make_consts():
    c = np.zeros((128, NCONST), np.float32)
    i = np.arange(128)
    c[:, C_ID:C_ID + 128] = np.eye(128, dtype=np.float32)
    c[:, C_TRI:C_TRI + 128] = (i[:, None] <= i[None, :]).astype(np.float32)
    c[:, C_LNEG:C_LNEG + 128] = (i[:, None] <= i[None, :]).astype(np.float32) * (-1.0 / 16.0)
    c[:, C_UNEG:C_UNEG + 128] = (i[:, None] > i[None, :]).astype(np.float32) * (-1.0 / 16.0)
    c[:, C_CNEG:C_CNEG + 128] = np.where(i[None, :] <= i[:, None], 0.0, -1e30).astype(np.float32)
    c[:, C_POW2:C_POW2 + 32] = (0.5 ** np.arange(1, 33)).astype(np.float32)[None, :]
    c[:, C_ONESM:C_ONESM + 128] = 1.0 / 128.0
    c[:, C_ONES:C_ONES + 128] = 1.0
    return c


def make_bias_tiles(rel_bias):
    bt = _bucket_table()
    s = np.arange(128)[:, None]; t = np.arange(128)[None, :]
    out = np.zeros((128, 2048), np.float32)
    for h in range(8):
        for o in range(2):
            d = t - s + o * 128
            idx = bt[np.clip(d, 0, 255)]
            out[:, (h * 2 + o) * 128:(h * 2 + o + 1) * 128] = rel_bias[idx, h]
    cfar = np.broadcast_to(rel_bias[31, :][None, :], (128, 8)).astype(np.float32).copy()
    return out, cfar


def make_in_maps(inputs, S, NSEQ, ncores):
    f = lambda a: np.ascontiguousarray(np.asarray(a, dtype=np.float32))
    x = f(inputs["x"]); p = f(inputs["p"])
    gains = np.stack([f(inputs[k]) for k in ("ln_mix_pre", "ln_mix_post", "ln_mlp_pre", "ln_mlp_post", "ln_ple_post")], axis=1)
    gains = np.ascontiguousarray(np.broadcast_to(gains[:, :, None, :], (DEPTH, 5, 128, DM)))
    biasT, cfar = make_bias_tiles(f(inputs["rel_bias"]))
    shared = dict(
        w_in=f(inputs["w_in"]), gw2=f(inputs["gla_gate_w2"]), ggb=f(inputs["gla_gate_b"]).reshape(DEPTH, 1, 256),
        gnorm=f(inputs["gla_norm"]).reshape(DEPTH, 128, 1), w_a=f(inputs["w_branch_a"]), w_b=f(inputs["w_branch_b"]),
        w_o=f(inputs["w_out"]), w_1=f(inputs["w_mlp_in"]), w_2=f(inputs["w_mlp_out"]), w_ple=f(inputs["w_ple"]),
        w_pg=f(inputs["w_ple_gate"]), gains=gains, consts=make_consts(), biasT=biasT, cfar=cfar)
    maps = []
    for c in range(ncores):
        m = dict(shared)
        m["x"] = np.ascontiguousarray(x[c * NSEQ:(c + 1) * NSEQ, :S].reshape(NSEQ * S, DM))
        m["p"] = np.ascontiguousarray(p[:, c * NSEQ:(c + 1) * NSEQ, :S].reshape(DEPTH, NSEQ * S, PLE))
        maps.append(m)
    return maps


_NC_CACHE = {}


def kernel(**inputs):
    B, S, _ = inputs["x"].shape
    ncores = 8; NSEQ = B // ncores
    key = (S, NSEQ)
    if key not in _NC_CACHE:
        _NC_CACHE[key] = build(dict(S=S, NSEQ=NSEQ))
    nc = _NC_CACHE[key]
    maps = make_in_maps(inputs, S, NSEQ, ncores)
    res = run_bass_kernel_spmd(nc, maps, core_ids=list(range(ncores)))
    out = np.stack([r["out"].reshape(NSEQ, S, DM) for r in res.results], axis=0).reshape(B, S, DM)
    return out.astype(np.float32)
```
